# Optimizing a Trainium2 kernel written in Bass

```python
import math
import jax
import jax.numpy as jnp
from jax import lax
import numpy as np

D_MODEL = 1024
BATCH = 4
SEQ = 8192
DEPTH = 2

GLA_HEADS = 4
GLA_DK = 64
GLA_DV = 128
GLA_RANK = 16
GLA_TAU = 16.0
DIFF_HEADS = 4
DIFF_DH = 64
ROT_DIM = DIFF_DH // 4
ROPE_THETA = 500000.0
Q_BLOCK = 128
GDN_HEADS = 4
GDN_DK = 128
GDN_DV = 128
CONV_W = 4
CHUNK = 64
N_BRANCH = 3
D_FF = 2816
N_EXPERTS = 8
TOP_K = 2
D_EXPERT = 3584
EPS = 1e-6
MAX_POS_OFFSET = 4096

GLA_QK = GLA_HEADS * GLA_DK
GLA_V = GLA_HEADS * GLA_DV
DIFF_QK = 2 * DIFF_HEADS * DIFF_DH
DIFF_V = DIFF_HEADS * 2 * DIFF_DH
GDN_QK = GDN_HEADS * GDN_DK
GDN_V = GDN_HEADS * GDN_DV
BRANCH_W = GLA_V
IN_SIZES = (GLA_QK, GLA_QK, GLA_V, GLA_V, GLA_RANK, GLA_RANK,
            DIFF_QK, DIFF_QK, DIFF_V,
            GDN_QK, GDN_QK, GDN_V, GDN_V, GDN_HEADS, GDN_HEADS, GDN_HEADS, GDN_HEADS,
            N_BRANCH * D_MODEL)
IN_SPLITS = tuple(int(s) for s in np.cumsum(IN_SIZES)[:-1])
IN_COLS = int(sum(IN_SIZES))
N_DENSE = (DEPTH + 1) // 2
N_MOE = DEPTH // 2

kernel_name = "hybrid_gla_diffattn_gdn_moe_encoder"


def rmsnorm(x, w):
    xf = x.astype(jnp.float32)
    y = xf * lax.rsqrt(jnp.mean(xf * xf, axis=-1, keepdims=True) + EPS)
    return (y * w.astype(jnp.float32)).astype(x.dtype)


def l2norm(x):
    return x * lax.rsqrt(jnp.sum(x * x, axis=-1, keepdims=True) + EPS)


def to_heads(t, n_heads):
    b, l, _ = t.shape
    return t.reshape(b, l, n_heads, -1).transpose(0, 2, 1, 3).astype(jnp.float32)


def from_heads(t):
    return t.transpose(0, 2, 1, 3)


def flip_seq(t):
    return jnp.flip(t, axis=2)


def partial_rope(x, pos):
    inv = ROPE_THETA ** (-jnp.arange(0, ROT_DIM, 2, dtype=jnp.float32) / ROT_DIM)
    ang = pos.astype(jnp.float32)[..., None] * inv
    cos = jnp.cos(ang)[:, :, None, :]
    sin = jnp.sin(ang)[:, :, None, :]
    xr = x[..., :ROT_DIM].astype(jnp.float32)
    x1, x2 = xr[..., :ROT_DIM // 2], xr[..., ROT_DIM // 2:]
    rot = jnp.concatenate([x1 * cos - x2 * sin, x2 * cos + x1 * sin], axis=-1).astype(x.dtype)
    return jnp.concatenate([rot, x[..., ROT_DIM:]], axis=-1)


def short_conv(x, w):
    return lax.conv_general_dilated(x, w[:, None, :].astype(x.dtype), window_strides=(1,), padding='SAME',
                                    dimension_numbers=('NWC', 'WIO', 'NWC'), feature_group_count=x.shape[-1])


def gla_chunked(q, k, v, g):
    B, H, L, dk = q.shape
    dv = v.shape[-1]
    n = L // CHUNK
    q = q.reshape(B, H, n, CHUNK, dk)
    k = k.reshape(B, H, n, CHUNK, dk)
    v = v.reshape(B, H, n, CHUNK, dv)
    b = jnp.cumsum(g.reshape(B, H, n, CHUNK, dk), axis=3)
    b_mid = b[:, :, :, CHUNK // 2 - 1:CHUNK // 2, :]
    b_last = b[:, :, :, -1:, :]
    incl = jnp.tril(jnp.ones((CHUNK, CHUNK), dtype=bool))
    a = jnp.einsum('bhnid,bhnjd->bhnij', q * jnp.exp(b - b_mid), k * jnp.exp(b_mid - b))
    a = jnp.where(incl, a, 0.0)
    o_intra = jnp.einsum('bhnij,bhnjv->bhniv', a, v)
    kv = jnp.einsum('bhncd,bhncv->bhndv', k * jnp.exp(b_last - b), v)
    decay = jnp.exp(b_last[:, :, :, 0, :])

    def step(s, xs):
        d_n, kv_n = xs
        return d_n[..., None] * s + kv_n, s

    s0 = jnp.zeros((B, H, dk, dv), jnp.float32)
    _, s_prev = lax.scan(step, s0, (jnp.moveaxis(decay, 2, 0), jnp.moveaxis(kv, 2, 0)))
    s_prev = jnp.moveaxis(s_prev, 0, 2)
    o_inter = jnp.einsum('bhncd,bhndv->bhncv', q * jnp.exp(b), s_prev)
    return (o_intra + o_inter).reshape(B, H, L, dv)


def gdn_chunked(q, k, v, g, beta):
    B, H, L, dk = q.shape
    dv = v.shape[-1]
    n = L // CHUNK
    q = q.reshape(B, H, n, CHUNK, dk)
    k = k.reshape(B, H, n, CHUNK, dk)
    v = v.reshape(B, H, n, CHUNK, dv)
    beta = beta.reshape(B, H, n, CHUNK)
    b = jnp.cumsum(g.reshape(B, H, n, CHUNK), axis=-1)
    incl = jnp.tril(jnp.ones((CHUNK, CHUNK), dtype=bool))
    strict = jnp.tril(jnp.ones((CHUNK, CHUNK), dtype=bool), k=-1)
    gamma = jnp.exp(jnp.where(incl, b[..., :, None] - b[..., None, :], -jnp.inf))
    kk = jnp.einsum('bhnid,bhnjd->bhnij', k, k)
    m = jnp.eye(CHUNK, dtype=jnp.float32) + jnp.where(strict, beta[..., :, None] * kk * gamma, 0.0)
    rhs = jnp.concatenate([v * beta[..., None], k * (beta * jnp.exp(b))[..., None]], axis=-1)
    sol = lax.linalg.triangular_solve(m, rhs, left_side=True, lower=True, unit_diagonal=True)
    u, w = sol[..., :dv], sol[..., dv:]
    attn = jnp.einsum('bhnid,bhnjd->bhnij', q, k) * gamma
    q_dec = q * jnp.exp(b)[..., None]
    k_end = k * jnp.exp(b[..., -1:] - b)[..., None]
    dec_last = jnp.exp(b[..., -1])

    def step(s, xs):
        u_n, w_n, q_n, k_n, a_n, d_n = xs
        v_new = u_n - jnp.einsum('bhcd,bhdv->bhcv', w_n, s)
        o_n = jnp.einsum('bhcd,bhdv->bhcv', q_n, s) + jnp.einsum('bhij,bhjv->bhiv', a_n, v_new)
        s = d_n[..., None, None] * s + jnp.einsum('bhcd,bhcv->bhdv', k_n, v_new)
        return s, o_n

    xs = tuple(jnp.moveaxis(t, 2, 0) for t in (u, w, q_dec, k_end, attn, dec_last))
    s0 = jnp.zeros((B, H, dk, dv), jnp.float32)
    _, o = lax.scan(step, s0, xs)
    return jnp.moveaxis(o, 0, 2).reshape(B, H, L, dv)


def diff_attention(q, k, v, lam):
    B, L, H2, d = q.shape
    H = H2 // 2
    scale = d ** -0.5
    qb = q.reshape(B, L // Q_BLOCK, Q_BLOCK, H2, d).transpose(1, 0, 3, 2, 4)
    kt = k.transpose(0, 2, 1, 3)
    vt = v.transpose(0, 2, 1, 3)

    def block(q_blk):
        s = jnp.einsum('bhqd,bhkd->bhqk', q_blk, kt).astype(jnp.float32) * scale
        p = jax.nn.softmax(s, axis=-1).reshape(B, H, 2, Q_BLOCK, L)
        a = p[:, :, 0] - lam * p[:, :, 1]
        return jnp.einsum('bhqk,bhkv->bhqv', a.astype(v.dtype), vt)

    o = lax.map(block, qb)
    return o.transpose(1, 0, 3, 2, 4).reshape(B, L, H, 2 * d)


def hybrid_mixer(h, pos, layer, w_in, gla_gate_up, gla_gate_bias, gla_norm, diff_lambda, diff_norm,
                 gdn_conv, gdn_A_log, gdn_dt_bias, gdn_norm, w_branch, w_out):
    f32 = jnp.float32
    B, L, _ = h.shape
    proj = h @ w_in
    (gq, gk, gv, gr, gwf, gwb, dq, dk, dv, nq, nk, nv, nz, nbf, nbb, naf, nab, mg) = jnp.split(proj, IN_SPLITS, axis=-1)

    q = to_heads(gq, GLA_HEADS) * (GLA_DK ** -0.5)
    k = to_heads(gk, GLA_HEADS)
    v = to_heads(gv, GLA_HEADS)

    def gla_log_gate(w_down, d):
        z = w_down.astype(f32) @ gla_gate_up[d].astype(f32) + gla_gate_bias[d].astype(f32)
        return to_heads(jax.nn.log_sigmoid(z) / GLA_TAU, GLA_HEADS)

    o = gla_chunked(q, k, v, gla_log_gate(gwf, 0)) + flip_seq(
        gla_chunked(flip_seq(q), flip_seq(k), flip_seq(v), flip_seq(gla_log_gate(gwb, 1))))
    o = rmsnorm(from_heads(o), gla_norm).reshape(B, L, GLA_V)
    y_gla = (o * jax.nn.silu(gr.astype(f32))).astype(h.dtype)

    qd = partial_rope(dq.reshape(B, L, 2 * DIFF_HEADS, DIFF_DH), pos)
    kd = partial_rope(dk.reshape(B, L, 2 * DIFF_HEADS, DIFF_DH), pos)
    vd = dv.reshape(B, L, DIFF_HEADS, 2 * DIFF_DH)
    lam_init = 0.8 - 0.6 * math.exp(-0.3 * layer)
    lp = diff_lambda.astype(f32)
    lam = jnp.exp(jnp.sum(lp[0] * lp[1])) - jnp.exp(jnp.sum(lp[2] * lp[3])) + lam_init
    od = diff_attention(qd, kd, vd, lam)
    y_diff = (rmsnorm(od, diff_norm).astype(f32) * (1.0 - lam_init)).reshape(B, L, DIFF_V).astype(h.dtype)

    qkv = jax.nn.silu(short_conv(jnp.concatenate([nq, nk, nv], axis=-1), gdn_conv))
    cq, ck, cv = jnp.split(qkv, [GDN_QK, 2 * GDN_QK], axis=-1)
    q = l2norm(to_heads(cq, GDN_HEADS)) * (GDN_DK ** -0.5)
    k = l2norm(to_heads(ck, GDN_HEADS))
    v = to_heads(cv, GDN_HEADS)

    def gdn_gates(b_logit, a_logit, d):
        beta = jax.nn.sigmoid(b_logit.astype(f32))
        g = -jnp.exp(gdn_A_log[d].astype(f32)) * jax.nn.softplus(a_logit.astype(f32) + gdn_dt_bias[d].astype(f32))
        return g.transpose(0, 2, 1), beta.transpose(0, 2, 1)

    g_f, beta_f = gdn_gates(nbf, naf, 0)
    g_b, beta_b = gdn_gates(nbb, nab, 1)
    o = gdn_chunked(q, k, v, g_f, beta_f) + flip_seq(
        gdn_chunked(flip_seq(q), flip_seq(k), flip_seq(v), flip_seq(g_b), flip_seq(beta_b)))
    z = nz.reshape(B, L, GDN_HEADS, GDN_DV).astype(f32)
    y_gdn = (rmsnorm(from_heads(o), gdn_norm) * jax.nn.silu(z)).reshape(B, L, GDN_V).astype(h.dtype)

    ys = jnp.stack([y_gla, y_diff, y_gdn], axis=2)
    branch = jnp.einsum('blnc,ncd->blnd', ys, w_branch)
    gates = jax.nn.sigmoid(mg.reshape(B, L, N_BRANCH, D_MODEL).astype(f32)).astype(h.dtype)
    merged = jnp.sum(gates * branch, axis=2)
    return merged @ w_out


def swiglu(h, w1, w3, w2):
    return (jax.nn.silu(h @ w1) * (h @ w3)) @ w2


def moe_swiglu(h, router_w, w1, w3, w2):
    B, L, D = h.shape
    t = h.reshape(B * L, D)
    logits = (t @ router_w).astype(jnp.float32)
    top_val, top_idx = lax.top_k(logits, TOP_K)
    top_w = jax.nn.softmax(top_val, axis=-1)
    combine = jnp.einsum('tk,tke->te', top_w, jax.nn.one_hot(top_idx, N_EXPERTS, dtype=jnp.float32)).astype(t.dtype)
    out = jnp.zeros_like(t)
    for e in range(N_EXPERTS):
        out = out + combine[:, e:e + 1] * swiglu(t, w1[e], w3[e], w2[e])
    return out.reshape(B, L, D)


def setup_inputs(seed: int = 0) -> dict:
    key = jax.random.key(seed)
    ks = jax.random.split(key, 32)
    f32 = jnp.float32

    def nrm(k, shape, scale):
        return jax.random.normal(k, shape, f32) * scale

    x = nrm(ks[0], (BATCH, SEQ, D_MODEL), 1.0)
    c = nrm(ks[1], (BATCH, D_MODEL), 1.0)
    positions = (jnp.arange(SEQ, dtype=jnp.int32)[None, :]
                 + jax.random.randint(ks[2], (BATCH, 1), 0, MAX_POS_OFFSET, dtype=jnp.int32))
    adaln_w = nrm(ks[3], (DEPTH, D_MODEL, 6 * D_MODEL), 0.5 * D_MODEL ** -0.5)
    adaln_b = nrm(ks[4], (DEPTH, 6 * D_MODEL), 0.02)
    norm_w = 1.0 + nrm(ks[5], (DEPTH, 4, D_MODEL), 0.05)
    w_in = nrm(ks[6], (DEPTH, D_MODEL, IN_COLS), D_MODEL ** -0.5)
    gla_gate_up = nrm(ks[7], (DEPTH, 2, GLA_RANK, GLA_QK), GLA_RANK ** -0.5)
    gla_gate_bias = nrm(ks[8], (DEPTH, 2, GLA_QK), 0.1)
    gla_norm = 1.0 + nrm(ks[9], (DEPTH, GLA_DV), 0.05)
    diff_lambda = nrm(ks[10], (DEPTH, 4, DIFF_DH), 0.1)
    diff_norm = 1.0 + nrm(ks[11], (DEPTH, 2 * DIFF_DH), 0.05)
    gdn_conv = nrm(ks[12], (DEPTH, CONV_W, 2 * GDN_QK + GDN_V), CONV_W ** -0.5)
    gdn_A_log = jnp.log(jax.random.uniform(ks[13], (DEPTH, 2, GDN_HEADS), f32, 1.0, 16.0))
    dt = jnp.exp(jax.random.uniform(ks[14], (DEPTH, 2, GDN_HEADS), f32, math.log(1e-3), math.log(1e-1)))
    gdn_dt_bias = dt + jnp.log(-jnp.expm1(-dt))
    gdn_norm = 1.0 + nrm(ks[15], (DEPTH, GDN_DV), 0.05)
    w_branch = nrm(ks[16], (DEPTH, N_BRANCH, BRANCH_W, D_MODEL), BRANCH_W ** -0.5)
    w_out = nrm(ks[17], (DEPTH, D_MODEL, D_MODEL), D_MODEL ** -0.5)
    ffn_w1 = nrm(ks[18], (N_DENSE, D_MODEL, D_FF), D_MODEL ** -0.5)
    ffn_w3 = nrm(ks[19], (N_DENSE, D_MODEL, D_FF), D_MODEL ** -0.5)
    ffn_w2 = nrm(ks[20], (N_DENSE, D_FF, D_MODEL), D_FF ** -0.5)
    router_w = nrm(ks[21], (N_MOE, D_MODEL, N_EXPERTS), D_MODEL ** -0.5)
    moe_w1 = nrm(ks[22], (N_MOE, N_EXPERTS, D_MODEL, D_EXPERT), D_MODEL ** -0.5)
    moe_w3 = nrm(ks[23], (N_MOE, N_EXPERTS, D_MODEL, D_EXPERT), D_MODEL ** -0.5)
    moe_w2 = nrm(ks[24], (N_MOE, N_EXPERTS, D_EXPERT, D_MODEL), D_EXPERT ** -0.5)
    return {"x": x, "c": c, "positions": positions, "adaln_w": adaln_w, "adaln_b": adaln_b,
            "norm_w": norm_w, "w_in": w_in, "gla_gate_up": gla_gate_up, "gla_gate_bias": gla_gate_bias,
            "gla_norm": gla_norm, "diff_lambda": diff_lambda, "diff_norm": diff_norm, "gdn_conv": gdn_conv,
            "gdn_A_log": gdn_A_log, "gdn_dt_bias": gdn_dt_bias, "gdn_norm": gdn_norm, "w_branch": w_branch,
            "w_out": w_out, "ffn_w1": ffn_w1, "ffn_w3": ffn_w3, "ffn_w2": ffn_w2, "router_w": router_w,
            "moe_w1": moe_w1, "moe_w3": moe_w3, "moe_w2": moe_w2}


def reference(x, c, positions, adaln_w, adaln_b, norm_w, w_in, gla_gate_up, gla_gate_bias, gla_norm,
              diff_lambda, diff_norm, gdn_conv, gdn_A_log, gdn_dt_bias, gdn_norm, w_branch, w_out,
              ffn_w1, ffn_w3, ffn_w2, router_w, moe_w1, moe_w3, moe_w2):
    mod = jnp.einsum('bd,lde->lbe', jax.nn.silu(c), adaln_w) + adaln_b[:, None, :]
    for layer in range(DEPTH):
        sh1, sc1, g1, sh2, sc2, g2 = jnp.split(mod[layer][:, None, :].astype(x.dtype), 6, axis=-1)
        h = rmsnorm(x, norm_w[layer, 0]) * (1.0 + sc1) + sh1
        y = hybrid_mixer(h, positions, layer, w_in[layer], gla_gate_up[layer], gla_gate_bias[layer],
                         gla_norm[layer], diff_lambda[layer], diff_norm[layer], gdn_conv[layer],
                         gdn_A_log[layer], gdn_dt_bias[layer], gdn_norm[layer], w_branch[layer], w_out[layer])
        x = x + g1 * rmsnorm(y, norm_w[layer, 1])
        h = rmsnorm(x, norm_w[layer, 2]) * (1.0 + sc2) + sh2
        if layer % 2 == 0:
            y = swiglu(h, ffn_w1[layer // 2], ffn_w3[layer // 2], ffn_w2[layer // 2])
        else:
            y = moe_swiglu(h, router_w[layer // 2], moe_w1[layer // 2], moe_w3[layer // 2], moe_w2[layer // 2])
        x = x + g2 * rmsnorm(y, norm_w[layer, 3])
    return x
```

```python
from contextlib import ExitStack
import math
import numpy as np
import concourse.bass as bass
import concourse.mybir as mybir
from concourse.bass_utils import run_bass_kernel_spmd

F32 = mybir.dt.float32
BF16 = mybir.dt.bfloat16
I32 = mybir.dt.int32
AF = mybir.ActivationFunctionType
ALU = mybir.AluOpType
AX = mybir.AxisListType

D = 1024
T = 8192
KT = D // 128
EPS = 1e-6
D_FF = 2816
N_EXP = 8
D_EXP = 3584
ST = 2048
NSUB = ST // 128

SEM_CHUNK = 10 ** 9


class Sched:
    ENG = ("pe", "act", "dve", "pool", "sp")
    ENGMAP = {"pe": "tensor", "act": "scalar", "dve": "vector", "pool": "gpsimd", "sp": "sync"}
    DMA_RETIRE = 4000
    MAX_INFLIGHT = 8

    def __init__(self, nc, stack):
        self.nc = nc
        self.stack = stack
        self.q = {e: [] for e in self.ENG}
        self.cnt = {e: 0 for e in self.ENG}
        self.eng_sems = {e: [] for e in self.ENG}
        self.phys = []
        self.free = []
        self.key2phys = {}
        self.writers = {}
        self.readers = {}
        self.seen = {e: {} for e in self.ENG}
        self.nsem = 0
        self.ninstr = 0
        self.inflight = {}
        self.ninflight = {}

    def _new_sem(self, name):
        self.nsem += 1
        return self.stack.enter_context(self.nc.semaphore(name))

    def _eng_sem(self, e, idx):
        k = (idx - 1) // SEM_CHUNK
        while len(self.eng_sems[e]) <= k:
            self.eng_sems[e].append(self._new_sem(f"s_{e}_{len(self.eng_sems[e])}"))
        return self.eng_sems[e][k], (idx - 1) % SEM_CHUNK + 1

    def _phys_of(self, key, eng):
        p = self.key2phys.get(key)
        if p is None:
            fl = [i for i in self.free if self.phys[i][2] == eng]
            if fl:
                p = fl[-1]
                self.free.remove(p)
            else:
                p = len(self.phys)
                self.phys.append([self._new_sem(f"d_{p}"), 0, eng])
            self.key2phys[key] = p
        assert self.phys[p][2] == eng, (key, eng)
        return p

    def _unit_wait(self, unit, idx):
        if unit[0] == "e":
            return self._eng_sem(unit[1], idx)
        return self.phys[unit[1]][0], idx * 16

    def _collect(self, eng, reads, writes):
        need = {}

        def add(d):
            for u, i in d.items():
                if need.get(u, 0) < i:
                    need[u] = i
        for b in reads:
            add(self.writers.get(b, {}))
        for b in writes:
            add(self.writers.get(b, {}))
            add(self.readers.get(b, {}))
        return self._filter(eng, need)

    def _filter(self, eng, need):
        waits = []
        seen = self.seen[eng]
        for u, i in need.items():
            if u == ("e", "pe") and eng == "pe":
                continue
            if u[0] == "d":
                i = self.phys[u[1]][1]
            if seen.get(u, 0) >= i:
                continue
            seen[u] = i
            waits.append(self._unit_wait(u, i))
        return waits

    def op(self, eng, fn, reads=(), writes=()):
        waits = self._collect(eng, reads, writes)
        self.cnt[eng] += 1
        idx = self.cnt[eng]
        sem, _ = self._eng_sem(eng, idx)
        self.q[eng].append((waits, fn, sem, 1))
        u = ("e", eng)
        for b in reads:
            self.readers.setdefault(b, {})[u] = idx
        for b in writes:
            self.writers.setdefault(b, {})[u] = idx
        self.ninstr += 1

    def dma(self, eng, key, fn, reads=(), writes=()):
        waits = self._collect(eng, reads, writes)
        out = self.inflight.setdefault(eng, set())
        if self.ninflight.get(eng, 0) >= self.MAX_INFLIGHT:
            waits = waits + self._filter(eng, {("d", p_): self.phys[p_][1] for p_ in out})
            out.clear()
            self.ninflight[eng] = 0
        p = self._phys_of(key, eng)
        out.add(p)
        self.ninflight[eng] = self.ninflight.get(eng, 0) + 1
        self.phys[p][1] += 1
        idx = self.phys[p][1]
        self.q[eng].append((waits, fn, self.phys[p][0], 16))
        u = ("d", p)
        for b in reads:
            self.readers.setdefault(b, {})[u] = idx
        for b in writes:
            self.writers.setdefault(b, {})[u] = idx
        self.ninstr += 1

    def barrier(self):
        need = {("e", e): c for e, c in self.cnt.items() if c > 0}
        for p, (sem, c, _q) in enumerate(self.phys):
            if c > 0:
                need[("d", p)] = c
        for e in self.ENG:
            waits = self._filter(e, dict(need))
            self.q[e].append((waits, None, None, 0))
        self.writers = {}
        self.readers = {}
        self.key2phys = {}
        self.inflight = {}
        self.ninflight = {}
        self.free = [p for p, (sem, c, _q) in enumerate(self.phys) if c < self.DMA_RETIRE]

    def emit(self):
        nc = self.nc
        with nc.Block() as block:
            for e in self.ENG:
                items = self.q[e]
                if not items:
                    continue

                def body(engine, items=items):
                    for waits, fn, sem, inc in items:
                        for (ws, wv) in waits:
                            engine.wait_ge(ws, wv)
                        if fn is not None:
                            fn().then_inc(sem, inc)
                getattr(block, self.ENGMAP[e])(body)
        self.q = {e: [] for e in self.ENG}


class Ctx:
    def __init__(self, nc, stack):
        self.nc = nc
        self.S = Sched(nc, stack)
        self.stack = stack
        self.uid = 0

    def sb(self, st, name, shape, dt):
        self.uid += 1
        return st.enter_context(self.nc.sbuf_tensor(f"{name}_{self.uid}", shape, dt))

    def ps(self, st, name, shape, dt=F32):
        self.uid += 1
        return st.enter_context(self.nc.psum_tensor(f"{name}_{self.uid}", shape, dt))

    def mm(self, out, lhsT, rhs, start, stop, r, w):
        nc = self.nc
        self.S.op("pe", lambda: nc.tensor.matmul(out, lhsT, rhs, start=start, stop=stop), r, w)

    def tr(self, out, in_, ident, r, w):
        nc = self.nc
        self.S.op("pe", lambda: nc.tensor.transpose(out, in_, ident), r, w)

    def act(self, out, in_, func, r, w, bias=None, scale=1.0, accum_out=None):
        nc = self.nc
        kw = {}
        if bias is not None:
            kw["bias"] = bias
        if accum_out is not None:
            kw["accum_out"] = accum_out
        self.S.op("act", lambda: nc.scalar.activation(out=out, in_=in_, func=func, scale=scale, **kw), r, w)

    def _veng(self, eng):
        return self.nc.vector if eng == "dve" else self.nc.gpsimd

    def tt(self, eng, out, in0, in1, op, r, w):
        e = self._veng(eng)
        self.S.op(eng, lambda: e.tensor_tensor(out=out, in0=in0, in1=in1, op=op), r, w)

    def ts(self, eng, out, in0, s1, s2, op0, op1, r, w):
        e = self._veng(eng)
        if op1 is None:
            self.S.op(eng, lambda: e.tensor_scalar(out=out, in0=in0, scalar1=s1, scalar2=None, op0=op0), r, w)
        else:
            self.S.op(eng, lambda: e.tensor_scalar(out=out, in0=in0, scalar1=s1, scalar2=s2, op0=op0, op1=op1), r, w)

    def stt(self, eng, out, in0, scalar, in1, op0, op1, r, w):
        e = self._veng(eng)
        self.S.op(eng, lambda: e.scalar_tensor_tensor(out=out, in0=in0, scalar=scalar, in1=in1, op0=op0, op1=op1), r, w)

    def cp(self, eng, out, in_, r, w):
        nc = self.nc
        if eng == "act":
            self.S.op("act", lambda: nc.scalar.copy(out=out, in_=in_), r, w)
        else:
            e = self._veng(eng)
            self.S.op(eng, lambda: e.tensor_copy(out=out, in_=in_), r, w)

    def memset(self, eng, ap, val, w):
        e = self._veng(eng)
        self.S.op(eng, lambda: e.memset(ap, val), (), w)

    def reduce(self, eng, out, in_, op, r, w):
        e = self._veng(eng)
        self.S.op(eng, lambda: e.tensor_reduce(out=out, in_=in_, axis=AX.X, op=op), r, w)

    def dma(self, q, key, out, in_, r, w):
        nc = self.nc
        e = {"sp": nc.sync, "pool": nc.gpsimd, "act": nc.scalar}[q]
        self.S.dma(q, key, lambda: e.dma_start(out=out, in_=in_), r, w)

    def recip(self, out, in_, r, w):
        nc = self.nc
        self.S.op("dve", lambda: nc.vector.reciprocal(out=out, in_=in_), r, w)

    def rstd(self, out, in_, scale, key, eps=EPS):
        self.act(out, in_, AF.Sqrt, [key], [key], bias=eps, scale=scale)
        self.recip(out, out, [key], [key])

    def phase_end(self):
        self.S.barrier()
        self.S.emit()


def host_consts():
    c = {}
    c["ident"] = np.eye(128, dtype=np.float32)
    return c


class Glob:
    pass


def setup_globals(cx, st, dr):
    nc = cx.nc
    g = Glob()
    g.ident = cx.sb(st, "ident", [128, 128], F32)
    g.identb = cx.sb(st, "identb", [128, 128], BF16)
    g.ones32 = cx.sb(st, "ones32", [128, 128], F32)
    g.onesb = cx.sb(st, "onesb", [128, 128], BF16)
    g.zeros32 = cx.sb(st, "zeros32", [128, 128], F32)
    cx.dma("sp", "g_ident", g.ident[:], dr["ident"], [], ["ident"])
    cx.cp("dve", g.identb[:], g.ident[:], ["ident"], ["identb"])
    cx.memset("pool", g.ones32[:], 1.0, ["ones32"])
    cx.memset("pool", g.onesb[:], 1.0, ["onesb"])
    cx.memset("pool", g.zeros32[:], 0.0, ["zeros32"])
    g.mod = cx.sb(st, "mod", [128, 6, D], F32)
    return g


def phase_mod(cx, g, dr, layer):
    nc = cx.nc
    with ExitStack() as st:
        cT = cx.sb(st, "cT", [128, KT], F32)
        cs = cx.sb(st, "cs", [128, KT], F32)
        CB = cx.sb(st, "CB", [128, KT, 128], BF16)
        bias = cx.sb(st, "abias", [1, 6 * D], BF16)
        nwb = cx.sb(st, "nwb", [128, 4, D], F32)
        wbuf = [cx.sb(st, f"aw{i}", [128, KT, 512], BF16) for i in range(2)]
        pm = [cx.ps(st, f"pm{i}", [128, 512]) for i in range(2)]
        cx.dma("sp", "m_c", cT[:], dr["cT"], [], ["cT"])
        cx.act(cs[:], cT[:], AF.Silu, ["cT"], ["cs"])
        for kt in range(KT):
            cx.act(CB[:, kt, :], g.zeros32[:], AF.Identity, ["cs", "zeros32"], ["CB"], bias=cs[:, kt:kt + 1])
        cx.dma("pool", "m_b", bias[:].rearrange("o (c n) -> o c n", n=512),
               dr["adaln_b"][layer:layer + 1, :].rearrange("o (c n) -> o c n", n=512), [], ["abias"])
        cx.dma("sp", "m_nw", nwb[:].rearrange("p a d -> p (a d)"),
               dr["norm_w"][layer:layer + 1].rearrange("o a d -> o (a d)")[0].partition_broadcast(128), [], ["nwb"])
        wv = dr["adaln_w"][layer].rearrange("(kt p) n -> p kt n", p=128)
        for ch in range(12):
            wb = wbuf[ch % 2]
            wk = f"aw{ch % 2}"
            cx.dma("pool", wk, wb[:], wv[:, :, ch * 512:(ch + 1) * 512], [], [wk])
            p = pm[ch % 2]
            pk = f"pm{ch % 2}"
            for kt in range(KT):
                cx.mm(p[:], CB[:, kt, :], wb[:, kt, :], kt == 0, False, ["CB", wk], [pk])
            cx.mm(p[:], g.onesb[0:1, :], bias[0:1, ch * 512:(ch + 1) * 512], False, True, ["onesb", "abias"], [pk])
            j, half = ch // 2, ch % 2
            slot = {0: 1, 1: 0, 2: 2, 3: 4, 4: 3, 5: 5}[j]
            cx.cp("dve", g.mod[:, slot, half * 512:(half + 1) * 512], p[:], [pk], [("mod", slot)])
        for slot, nwi in ((0, 0), (3, 2)):
            cx.stt("dve", g.mod[:, slot, :], g.mod[:, slot, :], 1.0, nwb[:, nwi, :], ALU.add, ALU.mult,
                   [("mod", slot), "nwb"], [("mod", slot)])
        for slot, nwi in ((2, 1), (5, 3)):
            cx.tt("dve", g.mod[:, slot, :], g.mod[:, slot, :], nwb[:, nwi, :], ALU.mult,
                  [("mod", slot), "nwb"], [("mod", slot)])
        cx.phase_end()


class HTMaker:
    def __init__(self, cx, st, g, tag):
        self.cx, self.g, self.tag = cx, g, tag
        self.xt = [cx.sb(st, f"{tag}xt{i}", [128, D], F32) for i in range(2)]
        self.h = [cx.sb(st, f"{tag}h{i}", [128, D], F32) for i in range(2)]
        self.junk = cx.sb(st, f"{tag}junk", [128, D], BF16)
        self.ss = [cx.sb(st, f"{tag}ss{i}", [128, 2], F32) for i in range(2)]
        self.pT = [cx.ps(st, f"{tag}pT{i}", [128, 4, 128]) for i in range(2)]
        self.n = 0

    def run(self, x_rows, aslot, shslot, hT, hTkey, col0):
        cx, g, tag = self.cx, self.g, self.tag
        i = self.n % 2
        self.n += 1
        xt, h, ss = self.xt[i], self.h[i], self.ss[i]
        kx, kh, ks = f"{tag}xt{i}", f"{tag}h{i}", f"{tag}ss{i}"
        cx.dma("sp", kx, xt[:], x_rows, [], [kx])
        cx.act(self.junk[:], xt[:], AF.Square, [kx], [ks], accum_out=ss[:, 0:1])
        cx.rstd(ss[:, 1:2], ss[:, 0:1], 1.0 / D, ks)
        cx.stt("dve", h[:], xt[:], ss[:, 1:2], g.mod[:, aslot, :], ALU.mult, ALU.mult, [kx, ks, ("mod", aslot)], [kh])
        cx.tt("pool", h[:], h[:], g.mod[:, shslot, :], ALU.add, [kh, ("mod", shslot)], [kh])
        for half in range(2):
            p = self.pT[half]
            pk = f"{tag}pT{half}"
            for q in range(4):
                kt = half * 4 + q
                cx.tr(p[:, q, :], h[:, kt * 128:(kt + 1) * 128], g.ident[:], [kh, "ident"], [pk])
            cx.cp("act", hT[:, half * 4:(half + 1) * 4, col0:col0 + 128], p[:], [pk], [hTkey])


def phase_ffn(cx, g, dr, x_in, x_out, experts, router=None, tok_range=None):
    nc = cx.nc
    if tok_range is None:
        tok_range = (0, T)
    F = experts[0][0].shape[1]
    FG = 256
    NG = F // FG
    with ExitStack() as st:
        hm = HTMaker(cx, st, g, "f")
        hT = cx.sb(st, "f_hT", [128, KT, ST], BF16)
        acc = cx.sb(st, "f_acc", [128, NSUB, D], F32)
        actT = cx.sb(st, "f_actT", [128, 2, ST], BF16)
        w1b = [cx.sb(st, f"f_w1_{i}", [128, KT, FG], BF16) for i in range(2)]
        w3b = [cx.sb(st, f"f_w3_{i}", [128, KT, FG], BF16) for i in range(2)]
        w2b = [cx.sb(st, f"f_w2_{i}", [128, 2, D], BF16) for i in range(2)]
        sil = [cx.sb(st, f"f_sil{i}", [128, 512], BF16) for i in range(2)]
        pu1 = [cx.ps(st, f"f_pu1_{i}", [128, 512]) for i in range(2)]
        pu3 = [cx.ps(st, f"f_pu3_{i}", [128, 512]) for i in range(2)]
        py = [cx.ps(st, f"f_py{i}", [128, 512]) for i in range(2)]
        if router is not None:
            wr = cx.sb(st, "f_wr", [128, KT, N_EXP], BF16)
            comb = cx.sb(st, "f_comb", [128, NSUB, N_EXP], F32)
            rt = cx.sb(st, "f_rt", [128, 8, N_EXP], F32)
            rs = cx.sb(st, "f_rs", [128, 8], F32)
            cx.dma("pool", "f_wr", wr[:], router.rearrange("(kt p) n -> p kt n", p=128), [], ["f_wr"])
        yo = [cx.sb(st, f"f_yo{i}", [128, D], F32) for i in range(2)]
        xr = [cx.sb(st, f"f_xr{i}", [128, D], F32) for i in range(2)]
        fs = [cx.sb(st, f"f_fs{i}", [128, 2], F32) for i in range(2)]
        gcount = 0
        for t0 in range(tok_range[0], tok_range[1], ST):
            for s in range(NSUB):
                hm.run(x_in[t0 + s * 128:t0 + (s + 1) * 128, :], 3, 4, hT, "f_hT", s * 128)
            if router is not None:
                for s in range(NSUB):
                    pr = py[s % 2]
                    pk = f"f_py{s % 2}"
                    for kt in range(KT):
                        cx.mm(pr[:, 0:N_EXP], hT[:, kt, s * 128:(s + 1) * 128], wr[:, kt, :], kt == 0, kt == KT - 1,
                              ["f_hT", "f_wr"], [pk])
                    L, EQ, L2, SEL, EX = (rt[:, i, :] for i in range(5))
                    cx.cp("dve", L, pr[:, 0:N_EXP], [pk], ["f_rt"])
                    cx.reduce("dve", rs[:, 0:1], L, ALU.max, ["f_rt"], ["f_rs"])
                    cx.ts("dve", EQ, L, rs[:, 0:1], None, ALU.is_equal, None, ["f_rt", "f_rs"], ["f_rt"])
                    cx.stt("dve", L2, EQ, -1e30, L, ALU.mult, ALU.add, ["f_rt"], ["f_rt"])
                    cx.reduce("dve", rs[:, 1:2], L2, ALU.max, ["f_rt"], ["f_rs"])
                    cx.ts("dve", SEL, L, rs[:, 1:2], None, ALU.is_ge, None, ["f_rt", "f_rs"], ["f_rt"])
                    cx.ts("dve", rs[:, 2:3], rs[:, 0:1], -1.0, None, ALU.mult, None, ["f_rs"], ["f_rs"])
                    cx.act(EX, L, AF.Exp, ["f_rt", "f_rs"], ["f_rt"], bias=rs[:, 2:3])
                    cx.tt("dve", EX, EX, SEL, ALU.mult, ["f_rt"], ["f_rt"])
                    cx.reduce("dve", rs[:, 3:4], EX, ALU.add, ["f_rt"], ["f_rs"])
                    cx.S.op("dve", (lambda o=rs[:, 4:5], i_=rs[:, 3:4]: nc.vector.reciprocal(out=o, in_=i_)), ["f_rs"], ["f_rs"])
                    cx.ts("dve", comb[:, s, :], EX, rs[:, 4:5], None, ALU.mult, None, ["f_rt", "f_rs"], ["f_comb"])
            first = True
            for e, (w1, w3, w2) in enumerate(experts):
                w1v = w1.rearrange("(kt p) n -> p kt n", p=128)
                w3v = w3.rearrange("(kt p) n -> p kt n", p=128)
                w2v = w2.rearrange("(f p) n -> p f n", p=128)
                for gi in range(NG):
                    b = gcount % 2
                    gcount += 1
                    k1, k3, k2 = f"f_w1_{b}", f"f_w3_{b}", f"f_w2_{b}"
                    cx.dma("pool", k1, w1b[b][:], w1v[:, :, gi * FG:(gi + 1) * FG], [], [k1])
                    cx.dma("pool", k3, w3b[b][:], w3v[:, :, gi * FG:(gi + 1) * FG], [], [k3])
                    cx.dma("pool", k2, w2b[b][:], w2v[:, gi * 2:(gi + 1) * 2, :], [], [k2])
                    for ch in range(ST // 512):
                        for f in range(2):
                            pb = (ch * 2 + f) % 2
                            for kt in range(KT):
                                cx.mm(pu1[pb][:], w1b[b][:, kt, f * 128:(f + 1) * 128], hT[:, kt, ch * 512:(ch + 1) * 512],
                                      kt == 0, kt == KT - 1, [k1, "f_hT"], [f"f_pu1{pb}"])
                            for kt in range(KT):
                                cx.mm(pu3[pb][:], w3b[b][:, kt, f * 128:(f + 1) * 128], hT[:, kt, ch * 512:(ch + 1) * 512],
                                      kt == 0, kt == KT - 1, [k3, "f_hT"], [f"f_pu3{pb}"])
                            sb_ = sil[pb]
                            sk = f"f_sil{pb}"
                            cx.act(sb_[:], pu1[pb][:], AF.Silu, [f"f_pu1{pb}"], [sk])
                            cx.tt("dve", actT[:, f, ch * 512:(ch + 1) * 512], sb_[:], pu3[pb][:], ALU.mult,
                                  [sk, f"f_pu3{pb}"], [("f_actT", ch)])
                    for s in range(NSUB):
                        for half in range(2):
                            p = py[(s * 2 + half) % 2]
                            pk = f"f_py{(s * 2 + half) % 2}"
                            for f in range(2):
                                cx.mm(p[:], actT[:, f, s * 128:(s + 1) * 128], w2b[b][:, f, half * 512:(half + 1) * 512],
                                      f == 0, f == 1, [("f_actT", s // 4), k2], [pk])
                            a = acc[:, s, half * 512:(half + 1) * 512]
                            ak = ("f_acc", s)
                            if router is None:
                                if first:
                                    cx.cp("dve", a, p[:], [pk], [ak])
                                else:
                                    cx.tt("dve", a, a, p[:], ALU.add, [pk, ak], [ak])
                            else:
                                if first:
                                    cx.ts("dve", a, p[:], comb[:, s, e:e + 1], None, ALU.mult, None, [pk, "f_comb"], [ak])
                                else:
                                    cx.stt("dve", a, p[:], comb[:, s, e:e + 1], a, ALU.mult, ALU.add, [pk, "f_comb", ak], [ak])
                    first = False
            for s in range(NSUB):
                i = s % 2
                ky, kx, kf = f"f_yo{i}", f"f_xr{i}", f"f_fs{i}"
                rows = slice(t0 + s * 128, t0 + (s + 1) * 128)
                cx.dma("sp", kx, xr[i][:], x_in[rows, :], [], [kx])
                cx.act(yo[i][:], acc[:, s, :], AF.Square, [("f_acc", s)], [ky, kf], accum_out=fs[i][:, 0:1])
                cx.rstd(fs[i][:, 1:2], fs[i][:, 0:1], 1.0 / D, kf)
                cx.stt("dve", yo[i][:], acc[:, s, :], fs[i][:, 1:2], g.mod[:, 5, :], ALU.mult, ALU.mult,
                       [("f_acc", s), kf, ("mod", 5)], [ky])
                cx.tt("pool", yo[i][:], yo[i][:], xr[i][:], ALU.add, [ky, kx], [ky])
                cx.dma("sp", ky, x_out[rows, :], yo[i][:], [ky], [("xout", s)])
        cx.phase_end()


OFF = 2
TP = T + 6
FM_GQ, FM_GK, FM_GR, FM_GW, FM_DQ, FM_DQS, FM_DK, FM_DKS, FM_NC, FM_NZ, FM_MG = 0, 2, 4, 8, 9, 13, 17, 21, 25, 37, 41
NFM = 65
NFMG = 17
TM_GV, TM_DV, TM_GL = 0, 512, 1024
NTM = 1040

IN_SIZES = (256, 256, 512, 512, 16, 16, 512, 512, 512, 512, 512, 512, 512, 4, 4, 4, 4, 3072)
IN_OFF = np.concatenate([[0], np.cumsum(IN_SIZES)]).astype(int)
(C_GQ, C_GK, C_GV, C_GR, C_GWF, C_GWB, C_DQ, C_DK, C_DV, C_NQ, C_NK, C_NV, C_NZ, C_NBF, C_NBB, C_NAF, C_NAB, C_MG) = IN_OFF[:-1]


def pack_w_in(w):
    z = lambda n: np.zeros((D, n), np.float32)
    rng = lambda a, n: w[:, a:a + n]
    swap = np.arange(512)
    for hd in range(8):
        for i in range(16):
            swap[hd * 64 + i] = hd * 64 + (i + 8 if i < 8 else i - 8)
    fm = [rng(C_GQ, 256), rng(C_GK, 256), rng(C_GR, 512),
          rng(C_GWF, 16), z(16), rng(C_GWB, 16), z(80),
          rng(C_DQ, 512), rng(C_DQ, 512)[:, swap], rng(C_DK, 512), rng(C_DK, 512)[:, swap],
          rng(C_NQ, 512), rng(C_NK, 512), rng(C_NV, 512), rng(C_NZ, 512), rng(C_MG, 3072), z(NFMG * 512 - NFM * 128)]
    fm = np.concatenate(fm, axis=1)
    assert fm.shape[1] == NFMG * 512
    tm = np.concatenate([rng(C_GV, 512), rng(C_DV, 512), rng(C_NAF, 4), rng(C_NAB, 4), rng(C_NBF, 4), rng(C_NBB, 4)], axis=1)
    assert tm.shape[1] == NTM
    return np.ascontiguousarray(fm), np.ascontiguousarray(tm)


def fm_evac_kind(tile):
    if tile < FM_GK:
        return ("scale", 0.125)
    if FM_GR <= tile < FM_GW:
        return ("act", AF.Silu)
    if FM_NZ <= tile < FM_MG:
        return ("act", AF.Silu)
    if tile >= FM_MG:
        return ("act", AF.Sigmoid)
    return ("copy", None)


def phase_proj(cx, g, dr, x_in, layer, sc):
    nc = cx.nc
    with ExitStack() as st:
        hm = HTMaker(cx, st, g, "p")
        hT = cx.sb(st, "p_hT", [128, KT, ST], BF16)
        wb = [cx.sb(st, f"p_w{i}", [128, KT, 512], BF16) for i in range(2)]
        stg = [cx.sb(st, f"p_stg{i}", [128, ST], BF16) for i in range(2)]
        tstg = [cx.sb(st, f"p_tstg{i}", [128, 512], BF16) for i in range(2)]
        gstg = [cx.sb(st, f"p_gstg{i}", [128, 16], F32) for i in range(2)]
        zpad = cx.sb(st, "p_zpad", [128, 8], BF16)
        pp = [cx.ps(st, f"p_pp{i}", [128, 512]) for i in range(4)]
        wfm = dr["w_fm"][layer].rearrange("(kt p) n -> p kt n", p=128)
        wtm = dr["w_tm"][layer].rearrange("(kt p) n -> p kt n", p=128)
        cx.memset("pool", zpad[:], 0.0, ["p_zpad"])
        for tile in range(FM_NC, FM_NC + 12):
            cx.dma("sp", "p_zp", sc["FM"][tile, :, 0:OFF], zpad[:, 0:OFF], ["p_zpad"], [])
            cx.dma("sp", "p_zp", sc["FM"][tile, :, OFF + T:TP], zpad[:, 0:TP - OFF - T], ["p_zpad"], [])
        wcount = 0
        pcount = 0
        scount = 0
        for si in range(T // ST):
            t0 = si * ST
            for s in range(NSUB):
                hm.run(x_in[t0 + s * 128:t0 + (s + 1) * 128, :], 0, 1, hT, "p_hT", s * 128)
            for gi in range(NFMG):
                b = wcount % 2
                wcount += 1
                wk = f"p_w{b}"
                cx.dma("pool", wk, wb[b][:], wfm[:, :, gi * 512:(gi + 1) * 512], [], [wk])
                for q in range(4):
                    tile = gi * 4 + q
                    if tile >= NFM:
                        break
                    sb_ = scount % 2
                    scount += 1
                    sk = f"p_stg{sb_}"
                    kind, arg = fm_evac_kind(tile)
                    for ch in range(ST // 512):
                        pb = pcount % 4
                        pcount += 1
                        pk = f"p_pp{pb}"
                        for kt in range(KT):
                            cx.mm(pp[pb][:], wb[b][:, kt, q * 128:(q + 1) * 128], hT[:, kt, ch * 512:(ch + 1) * 512],
                                  kt == 0, kt == KT - 1, [wk, "p_hT"], [pk])
                        o = stg[sb_][:, ch * 512:(ch + 1) * 512]
                        if kind == "act":
                            cx.act(o, pp[pb][:], arg, [pk], [sk])
                        elif kind == "scale":
                            cx.ts("dve", o, pp[pb][:], arg, None, ALU.mult, None, [pk], [sk])
                        else:
                            cx.cp("dve", o, pp[pb][:], [pk], [sk])
                    cx.dma("sp", sk, sc["FM"][tile, :, OFF + t0:OFF + t0 + ST], stg[sb_][:], [sk], [])
            for (c0, ncol) in ((TM_GV, 512), (TM_DV, 512), (TM_GL, 16)):
                b = wcount % 2
                wcount += 1
                wk = f"p_w{b}"
                cx.dma("pool", wk, wb[b][:, :, 0:ncol], wtm[:, :, c0:c0 + ncol], [], [wk])
                for s in range(NSUB):
                    pb = pcount % 4
                    pcount += 1
                    pk = f"p_pp{pb}"
                    for kt in range(KT):
                        cx.mm(pp[pb][:, 0:ncol], hT[:, kt, s * 128:(s + 1) * 128], wb[b][:, kt, 0:ncol],
                              kt == 0, kt == KT - 1, [wk, "p_hT"], [pk])
                    rows = slice(t0 + s * 128, t0 + (s + 1) * 128)
                    if ncol == 512:
                        tb = s % 2
                        tk = f"p_tstg{tb}"
                        cx.cp("dve" if s % 2 else "act", tstg[tb][:], pp[pb][:], [pk], [tk])
                        cx.dma("sp", tk, sc["TM"][rows, c0:c0 + 512], tstg[tb][:], [tk], [])
                    else:
                        tb = s % 2
                        tk = f"p_gstg{tb}"
                        cx.cp("dve", gstg[tb][:], pp[pb][:, 0:16], [pk], [tk])
                        cx.dma("sp", tk, sc["GL"][rows, :], gstg[tb][:], [tk], [])
        cx.phase_end()


def rot_consts():
    inv = np.zeros((128, 1), np.float32)
    sgn = np.zeros((128, 1), np.float32)
    for p in range(128):
        i = p % 64
        if i < 16:
            inv[p, 0] = 500000.0 ** (-(2 * (i % 8)) / 16.0)
            sgn[p, 0] = -1.0 if i < 8 else 1.0
    return inv, sgn


def phase_diff(cx, g, dr, layer, sc):
    nc = cx.nc
    lam_init = 0.8 - 0.6 * math.exp(-0.3 * layer)
    TWO_PI = 2.0 * math.pi
    CH = 2048
    with ExitStack() as st:
        Ct = cx.sb(st, "d_C", [128, T], BF16)
        St = cx.sb(st, "d_S", [128, T], BF16)
        inv = cx.sb(st, "d_inv", [128, 1], F32)
        sgn = cx.sb(st, "d_sgn", [128, 1], F32)
        pi_ = cx.sb(st, "d_pi", [128, CH], I32)
        ang = cx.sb(st, "d_ang", [128, CH], F32)
        tf = cx.sb(st, "d_tf", [128, CH], F32)
        ti = cx.sb(st, "d_ti", [128, CH], I32)
        fx = cx.sb(st, "d_fx", [128, CH], F32)
        cx.dma("sp", "d_c0", inv[:], dr["rot_inv"], [], ["d_inv"])
        cx.dma("sp", "d_c1", sgn[:], dr["rot_sgn"], [], ["d_sgn"])

        def reduced_sin(out_bf, shift, kout, scale_ap=None):
            cx.ts("dve", tf[:], ang[:], shift, 1.0 / TWO_PI, ALU.add, ALU.mult, ["d_ang"], ["d_tf"])
            cx.cp("dve", ti[:], tf[:], ["d_tf"], ["d_ti"])
            cx.cp("dve", tf[:], ti[:], ["d_ti"], ["d_tf"])
            cx.stt("dve", tf[:], tf[:], -TWO_PI, ang[:], ALU.mult, ALU.add, ["d_tf", "d_ang"], ["d_tf"])
            cx.ts("dve", tf[:], tf[:], shift, None, ALU.add, None, ["d_tf"], ["d_tf"])
            cx.ts("dve", fx[:], tf[:], math.pi, -TWO_PI, ALU.is_gt, ALU.mult, ["d_tf"], ["d_fx"])
            cx.tt("dve", tf[:], tf[:], fx[:], ALU.add, ["d_tf", "d_fx"], ["d_tf"])
            cx.ts("dve", fx[:], tf[:], -math.pi, TWO_PI, ALU.is_lt, ALU.mult, ["d_tf"], ["d_fx"])
            cx.tt("dve", tf[:], tf[:], fx[:], ALU.add, ["d_tf", "d_fx"], ["d_tf"])
            cx.ts("dve", tf[:], tf[:], -math.pi, math.pi, ALU.max, ALU.min, ["d_tf"], ["d_tf"])
            if scale_ap is None:
                cx.act(out_bf, tf[:], AF.Sin, ["d_tf"], [kout])
            else:
                cx.act(out_bf, tf[:], AF.Sin, ["d_tf", "d_sgn"], [kout], scale=scale_ap)

        for c in range(T // CH):
            cx.dma("sp", "d_pi", pi_[:], dr["positions"][0, c * CH:(c + 1) * CH].partition_broadcast(128), [], ["d_pi"])
            cx.cp("dve", ang[:], pi_[:], ["d_pi"], ["d_ang"])
            cx.ts("dve", ang[:], ang[:], inv[:, 0:1], None, ALU.mult, None, ["d_ang", "d_inv"], ["d_ang"])
            reduced_sin(Ct[:, c * CH:(c + 1) * CH], math.pi / 2.0, "d_C")
            reduced_sin(St[:, c * CH:(c + 1) * CH], 0.0, "d_S", scale_ap=sgn[:, 0:1])

        lp = cx.sb(st, "d_lp", [128, 4, 64], F32)
        pr = cx.sb(st, "d_pr", [128, 2, 64], F32)
        lc = cx.sb(st, "d_lc", [128, 8], F32)
        nwc = cx.sb(st, "d_nwc", [128, 1], F32)
        cx.dma("sp", "d_c2", lp[:].rearrange("p a b -> p (a b)"),
               dr["diff_lambda"][layer:layer + 1].rearrange("o a b -> o (a b)")[0].partition_broadcast(128), [], ["d_lp"])
        cx.dma("sp", "d_c3", nwc[:], dr["diff_norm"][layer].rearrange("(p o) -> p o", o=1), [], ["d_nwc"])
        cx.tt("dve", pr[:, 0, :], lp[:, 0, :], lp[:, 1, :], ALU.mult, ["d_lp"], ["d_pr"])
        cx.tt("dve", pr[:, 1, :], lp[:, 2, :], lp[:, 3, :], ALU.mult, ["d_lp"], ["d_pr"])
        cx.reduce("dve", lc[:, 0:1], pr[:, 0, :], ALU.add, ["d_pr"], ["d_lc"])
        cx.reduce("dve", lc[:, 1:2], pr[:, 1, :], ALU.add, ["d_pr"], ["d_lc"])
        cx.act(lc[:, 2:4], lc[:, 0:2], AF.Exp, ["d_lc"], ["d_lc"])
        cx.tt("dve", lc[:, 4:5], lc[:, 3:4], lc[:, 2:3], ALU.subtract, ["d_lc"], ["d_lc"])
        cx.ts("dve", lc[:, 4:5], lc[:, 4:5], -lam_init, None, ALU.add, None, ["d_lc"], ["d_lc"])
        cx.ts("dve", nwc[:], nwc[:], 1.0 - lam_init, None, ALU.mult, None, ["d_nwc"], ["d_nwc"])

        qb = cx.sb(st, "d_q", [128, T], BF16)
        kb = cx.sb(st, "d_k", [128, T], BF16)
        xb = cx.sb(st, "d_x", [128, T], BF16)
        vt = cx.sb(st, "d_v", [128, T // 128, 128], BF16)
        pbuf = [cx.sb(st, f"d_p{i}", [128, 512], BF16) for i in range(3)]
        osb = [cx.sb(st, f"d_os{i}", [128, 512], F32) for i in range(2)]
        rl = cx.sb(st, "d_rl", [128, 512], F32)
        od = cx.sb(st, "d_od", [128, 512], F32)
        sq = cx.sb(st, "d_sq", [128, 512], BF16)
        yst = [cx.sb(st, f"d_y{i}", [128, 512], BF16) for i in range(2)]
        pS = [cx.ps(st, f"d_pS{i}", [128, 512]) for i in range(3)]
        pO = cx.ps(st, "d_pO", [128, 512])
        pL = cx.ps(st, "d_pL", [128, 512])
        pN = cx.ps(st, "d_pN", [128, 512])
        NK = T // 128
        ycount = 0
        for ht in range(4):
            for (dst, dk_, t_main, t_sw) in ((qb, "d_q", FM_DQ + ht, FM_DQS + ht), (kb, "d_k", FM_DK + ht, FM_DKS + ht)):
                cx.dma("sp", dk_, dst[:], sc["FM"][t_main, :, OFF:OFF + T], [], [dk_])
                cx.dma("sp", "d_x", xb[:], sc["FM"][t_sw, :, OFF:OFF + T], [], ["d_x"])
                cx.tt("dve", dst[:], dst[:], Ct[:], ALU.mult, [dk_, "d_C"], [dk_])
                cx.tt("pool", xb[:], xb[:], St[:], ALU.mult, ["d_x", "d_S"], ["d_x"])
                cx.tt("dve", dst[:], dst[:], xb[:], ALU.add, [dk_, "d_x"], [dk_])
            cx.dma("sp", "d_v", vt[:], sc["TM"][:, TM_DV + ht * 128:TM_DV + (ht + 1) * 128].rearrange("(n p) c -> p n c", p=128),
                   [], ["d_v"])
            for qc in range(T // 512):
                qs_ = slice(qc * 512, (qc + 1) * 512)
                for s in range(2):
                    rs_ = slice(s * 64, (s + 1) * 64)

                    def smm(kt, i):
                        cx.mm(pS[i % 3][:], kb[rs_, kt * 128:(kt + 1) * 128], qb[rs_, qs_], True, True,
                              ["d_k", "d_q"], [f"d_pS{i % 3}"])
                    base = (qc * 2 + s) * NK
                    smm(0, base)
                    for kt in range(NK):
                        i = base + kt
                        if kt + 1 < NK:
                            smm(kt + 1, i + 1)
                        pb_ = pbuf[i % 3]
                        pk = f"d_p{i % 3}"
                        cx.act(pb_[:], pS[i % 3][:], AF.Exp, [f"d_pS{i % 3}"], [pk], scale=0.125)
                        cx.mm(pO[:], vt[:, kt, :], pb_[:], kt == 0, kt == NK - 1, ["d_v", pk], ["d_pO"])
                        cx.mm(pL[:], g.onesb[:], pb_[:], kt == 0, kt == NK - 1, ["onesb", pk], ["d_pL"])
                    cx.recip(rl[:], pL[:], ["d_pL"], ["d_rl"])
                    cx.tt("dve", osb[s][:], pO[:], rl[:], ALU.mult, ["d_pO", "d_rl"], [f"d_os{s}"])
                cx.stt("dve", od[:], osb[1][:], lc[:, 4:5], osb[0][:], ALU.mult, ALU.add, ["d_os0", "d_os1", "d_lc"], ["d_od"])
                cx.act(sq[:], od[:], AF.Square, ["d_od"], ["d_sq"])
                cx.mm(pN[:], g.onesb[:], sq[:], True, True, ["onesb", "d_sq"], ["d_pN"])
                cx.act(rl[:], pN[:], AF.Sqrt, ["d_pN"], ["d_rl"], bias=EPS, scale=1.0 / 128)
                cx.recip(rl[:], rl[:], ["d_rl"], ["d_rl"])
                yb = yst[ycount % 2]
                yk = f"d_y{ycount % 2}"
                ycount += 1
                cx.stt("dve", yb[:], od[:], nwc[:, 0:1], rl[:], ALU.mult, ALU.mult, ["d_od", "d_nwc", "d_rl"], [yk])
                cx.dma("sp", yk, sc["YT"][1, ht, :, qs_], yb[:], [yk], [])
        cx.phase_end()


class Rot:
    def __init__(self, cx, st, name, shape, dt, n, psum=False):
        mk = cx.ps if psum else cx.sb
        self.t = [mk(st, f"{name}{i}", shape, dt) for i in range(n)]
        self.k = [f"{name}#{i}" for i in range(n)]
        self.i = -1

    def next(self):
        self.i += 1
        j = self.i % len(self.t)
        return self.t[j], self.k[j]

    def cur(self):
        j = self.i % len(self.t)
        return self.t[j], self.k[j]


def chunk_masks():
    j = np.arange(128)[:, None]
    i = np.arange(128)[None, :]
    same = (j // 64) == (i // 64)
    mf = (same & (j <= i)).astype(np.float32)
    mb = (same & (j >= i)).astype(np.float32)
    reset = np.ones((128, 512), np.float32)
    reset[:, ::64] = 0.0
    return mf, mb, reset


def phase_gla(cx, g, dr, layer, sc):
    nc = cx.nc
    NB = T // 512
    with ExitStack() as st:
        gu = cx.sb(st, "a_gu", [64, 256], BF16)
        nb = cx.sb(st, "a_nb", [128, 4], F32)
        nwc = cx.sb(st, "a_nwc", [128, 1], F32)
        mk = [cx.sb(st, f"a_mk{i}", [128, 128], F32) for i in range(2)]
        reset = cx.sb(st, "a_reset", [128, 512], F32)
        S = cx.sb(st, "a_S", [128, 128], BF16)
        cx.dma("pool", "a_c0", gu[0:16, :], dr["gla_gate_up"][layer, 0], [], ["a_gu"])
        cx.dma("pool", "a_c0", gu[32:48, :], dr["gla_gate_up"][layer, 1], [], ["a_gu"])
        for d in range(2):
            for rt in range(2):
                cx.dma("sp", "a_c1", nb[:, d * 2 + rt:d * 2 + rt + 1],
                       dr["gla_gate_bias"][layer, d, rt * 128:(rt + 1) * 128].rearrange("(p o) -> p o", o=1), [], ["a_nb"])
        cx.ts("dve", nb[:], nb[:], -1.0, None, ALU.mult, None, ["a_nb"], ["a_nb"])
        cx.dma("sp", "a_c2", nwc[:], dr["gla_norm"][layer].rearrange("(p o) -> p o", o=1), [], ["a_nwc"])
        cx.dma("sp", "a_c3", mk[0][:], dr["mask_f"], [], ["a_mk0"])
        cx.dma("sp", "a_c3", mk[1][:], dr["mask_b"], [], ["a_mk1"])
        cx.dma("sp", "a_c4", reset[:], dr["reset64"], [], ["a_reset"])

        gw = Rot(cx, st, "a_gw", [64, 512], BF16, 2)
        qk = Rot(cx, st, "a_qk", [128, 2, 512], BF16, 2)
        vtm = Rot(cx, st, "a_v", [128, 4, 256], BF16, 2)
        f32r = {n: Rot(cx, st, f"a_{n}", [128, 512], F32, 2) for n in ("e", "sp", "Bp", "Dm", "E3")}
        E12 = Rot(cx, st, "a_E12", [128, 512], F32, 2)
        tot = Rot(cx, st, "a_tot", [128, 8, 1], F32, 2)
        bfr = {n: Rot(cx, st, f"a_{n}", [128, 512], BF16, 2) for n in ("qt", "kt", "qd", "ke")}
        ketm = Rot(cx, st, "a_ketm", [128, 128], BF16, 2)
        Am = Rot(cx, st, "a_Am", [128, 128], BF16, 3)
        ofw = Rot(cx, st, "a_ofw", [128, 512], F32, 4)
        grb = Rot(cx, st, "a_gr", [128, 512], BF16, 4)
        osb = Rot(cx, st, "a_o", [128, 512], F32, 2)
        sq = Rot(cx, st, "a_sq", [128, 512], BF16, 2)
        rl = Rot(cx, st, "a_rl", [128, 512], F32, 2)
        yb = Rot(cx, st, "a_y", [128, 512], BF16, 2)
        pZ = cx.ps(st, "a_pZ", [128, 512])
        pN = pZ
        pO = Rot(cx, st, "a_pO", [128, 512], F32, 3, psum=True)
        pA = Rot(cx, st, "a_pA", [128, 128], F32, 2, psum=True)
        pKV = cx.ps(st, "a_pKV", [128, 128])
        pT = cx.ps(st, "a_pT", [128, 2, 128], BF16)

        for d in range(2):
            mid, last = (31, 63) if d == 0 else (32, 0)
            blocks = list(range(NB)) if d == 0 else list(range(NB - 1, -1, -1))
            for rt in range(2):
                cx.memset("dve", S[:], 0.0, ["a_S"])
                prep = {}

                def gates(b):
                    c0 = b * 512
                    cols = slice(OFF + c0, OFF + c0 + 512)
                    gwt, gwk = gw.next()
                    cx.dma("sp", gwk, gwt[:], sc["FM"][FM_GW, 0:64, cols], [], [gwk])
                    qkt, qkk = qk.next()
                    cx.dma("sp", qkk, qkt[:, 0, :], sc["FM"][FM_GQ + rt, :, cols], [], [qkk])
                    cx.dma("sp", qkk, qkt[:, 1, :], sc["FM"][FM_GK + rt, :, cols], [], [qkk])
                    vt, vk = vtm.next()
                    cx.dma("sp", vk, vt[:], sc["TM"][c0:c0 + 512, TM_GV + rt * 256:TM_GV + (rt + 1) * 256]
                           .rearrange("(n p) c -> p n c", p=128), [], [vk])
                    r0 = d * 32
                    cx.mm(pZ[:], gu[r0:r0 + 16, rt * 128:(rt + 1) * 128], gwt[r0:r0 + 16, :], True, True, ["a_gu", gwk], ["a_pZ"])
                    e, ek = f32r["e"].next()
                    cx.act(e[:], pZ[:], AF.Exp, ["a_pZ", "a_nb"], [ek], bias=nb[:, d * 2 + rt:d * 2 + rt + 1], scale=-1.0)
                    sp, spk = f32r["sp"].next()
                    cx.act(sp[:], e[:], AF.Ln, [ek], [spk], bias=1.0)
                    Bp, Bk = f32r["Bp"].next()
                    nc_ = nc
                    cx.S.op("dve", (lambda o=Bp[:], r_=reset[:], s_=sp[:]: nc_.vector.tensor_tensor_scan(o, r_, s_, 0.0, ALU.mult, ALU.add)),
                            ["a_reset", spk], [Bk])
                    B3 = Bp[:].rearrange("p (c j) -> p c j", j=64)
                    if d == 1:
                        Dm_, Dk_ = f32r["Dm"].next()
                        cx.tt("pool", Dm_[:], sp[:], Bp[:], ALU.subtract, [spk, Bk], [Dk_])
                        tot_, totk_ = tot.next()
                        cx.cp("dve", tot_[:], B3[:, :, 63:64], [Bk], [totk_])
                        cx.tt("dve", B3, Dm_[:].rearrange("p (c j) -> p c j", j=64), tot_[:].to_broadcast([128, 8, 64]),
                              ALU.add, [Dk_, totk_], [Bk])
                    Dm, Dk = f32r["Dm"].next()
                    cx.tt("dve", Dm[:].rearrange("p (c j) -> p c j", j=64), B3, B3[:, :, mid:mid + 1].to_broadcast([128, 8, 64]),
                          ALU.subtract, [Bk], [Dk])
                    E3, E3k = f32r["E3"].next()
                    cx.act(E3[:], Bp[:], AF.Exp, [Bk], [E3k], scale=-1.0 / 16)
                    qt, qtk = bfr["qt"].next()
                    kt_, ktk = bfr["kt"].next()
                    qd, qdk = bfr["qd"].next()
                    ke, kek = bfr["ke"].next()
                    Ea, Eak = E12.next()
                    cx.act(Ea[:], Dm[:], AF.Exp, [Dk], [Eak], scale=-1.0 / 16)
                    cx.tt("pool", qt[:], qkt[:, 0, :], Ea[:], ALU.mult, [qkk, Eak], [qtk])
                    Eb, Ebk = E12.next()
                    cx.act(Eb[:], Dm[:], AF.Exp, [Dk], [Ebk], scale=1.0 / 16)
                    cx.tt("pool", kt_[:], qkt[:, 1, :], Eb[:], ALU.mult, [qkk, Ebk], [ktk])
                    cx.tt("dve", qd[:], qkt[:, 0, :], E3[:], ALU.mult, [qkk, E3k], [qdk])
                    Dl, Dlk = f32r["Dm"].next()
                    cx.tt("dve", Dl[:].rearrange("p (c j) -> p c j", j=64), B3, B3[:, :, last:last + 1].to_broadcast([128, 8, 64]),
                          ALU.subtract, [Bk], [Dlk])
                    Ec, Eck = E12.next()
                    cx.act(Ec[:], Dl[:], AF.Exp, [Dlk], [Eck], scale=1.0 / 16)
                    cx.tt("pool", ke[:], qkt[:, 1, :], Ec[:], ALU.mult, [qkk, Eck], [kek])
                    ex = {}
                    if d == 1:
                        ex["ofw"] = []
                        ex["gr"] = []
                        for hh in range(2):
                            h = rt * 2 + hh
                            o_, ok_ = ofw.next()
                            cx.dma("sp", ok_, o_[:], sc["OFW"][h, :, c0:c0 + 512], [], [ok_])
                            ex["ofw"].append((o_, ok_))
                            g_, gk_ = grb.next()
                            cx.dma("sp", gk_, g_[:], sc["FM"][FM_GR + h, :, cols], [], [gk_])
                            ex["gr"].append((g_, gk_))
                    prep[b] = dict(vt=vt, vk=vk, E3=E3, E3k=E3k, qt=qt, qtk=qtk, kt=kt_, ktk=ktk, qd=qd, qdk=qdk, ke=ke, kek=kek, **ex)

                def tiles(b):
                    P = prep.pop(b)
                    c0 = b * 512
                    po = []
                    for hh in range(2):
                        po.append(pO.next())
                    torder = range(4) if d == 0 else range(3, -1, -1)
                    for tt_ in torder:
                        ts_ = slice(tt_ * 128, (tt_ + 1) * 128)
                        kb_, kbk = ketm.next()
                        tb = ketm.i % 2
                        cx.tr(pT[:, tb, :], P["ke"][:, ts_], g.identb[:], [P["kek"], "identb"], ["a_pT"])
                        cx.cp("act", kb_[:], pT[:, tb, :], ["a_pT"], [kbk])
                        for hh in range(2):
                            rows = slice(hh * 64, (hh + 1) * 64)
                            am, amk = Am.next()
                            pa_, pak_ = pA.next()
                            cx.mm(pa_[:], P["kt"][rows, ts_], P["qt"][rows, ts_], True, True, [P["ktk"], P["qtk"]], [pak_])
                            cx.tt("dve", am[:], pa_[:], mk[d][:], ALU.mult, [pak_, f"a_mk{d}"], [amk])
                            pot, pok = po[hh]
                            cx.mm(pot[:, ts_], P["vt"][:, tt_, hh * 128:(hh + 1) * 128], am[:], True, False, [P["vk"], amk], [pok])
                            corder = (0, 1) if d == 0 else (1, 0)
                            for ci, c in enumerate(corder):
                                cs = slice(tt_ * 128 + c * 64, tt_ * 128 + (c + 1) * 64)
                                cx.mm(pot[:, cs], S[rows, :], P["qd"][rows, cs], False, ci == 1, ["a_S", P["qdk"]], [pok])
                                crow = slice(c * 64, (c + 1) * 64)
                                cx.mm(pKV[rows, :], kb_[crow, hh * 64:(hh + 1) * 64], P["vt"][crow, tt_, hh * 128:(hh + 1) * 128],
                                      True, True, [kbk, P["vk"]], ["a_pKV"])
                                dcol = tt_ * 128 + c * 64 + last
                                cx.stt("dve", S[rows, :], S[rows, :], P["E3"][rows, dcol:dcol + 1], pKV[rows, :], ALU.mult, ALU.add,
                                       ["a_S", P["E3k"], "a_pKV"], ["a_S"])
                    for hh in range(2):
                        h = rt * 2 + hh
                        pot, pok = po[hh]
                        o_, ok_ = osb.next()
                        if d == 0:
                            cx.cp("act", o_[:], pot[:], [pok], [ok_])
                            cx.dma("sp", ok_, sc["OFW"][h, :, c0:c0 + 512], o_[:], [ok_], [])
                        else:
                            f_, fk_ = P["ofw"][hh]
                            cx.tt("dve", o_[:], pot[:], f_[:], ALU.add, [pok, fk_], [ok_])
                            s_, sk_ = sq.next()
                            cx.act(s_[:], o_[:], AF.Square, [ok_], [sk_])
                            cx.mm(pN[:], g.onesb[:], s_[:], True, True, ["onesb", sk_], ["a_pZ"])
                            r_, rk_ = rl.next()
                            cx.act(r_[:], pN[:], AF.Sqrt, ["a_pZ"], [rk_], bias=EPS, scale=1.0 / 128)
                            cx.recip(r_[:], r_[:], [rk_], [rk_])
                            cx.stt("dve", o_[:], o_[:], nwc[:, 0:1], r_[:], ALU.mult, ALU.mult, [ok_, "a_nwc", rk_], [ok_])
                            y_, yk_ = yb.next()
                            g_, gk_ = P["gr"][hh]
                            cx.tt("pool", y_[:], o_[:], g_[:], ALU.mult, [ok_, gk_], [yk_])
                            cx.dma("sp", yk_, sc["YT"][0, h, :, c0:c0 + 512], y_[:], [yk_], [])

                gates(blocks[0])
                for bi, b in enumerate(blocks):
                    if bi + 1 < len(blocks):
                        gates(blocks[bi + 1])
                    tiles(b)
            if d == 0:
                cx.S.barrier()
        cx.phase_end()


def gdn_masks():
    j = np.arange(128)[:, None]
    i = np.arange(128)[None, :]
    same = (j // 64) == (i // 64)
    sm_f = (same & (j < i)).astype(np.float32)
    sm_b = (same & (j > i)).astype(np.float32)
    m_same = same.astype(np.float32)
    m_c0 = np.broadcast_to((j < 64), (128, 128)).astype(np.float32)
    m_c1 = np.broadcast_to((j >= 64), (128, 128)).astype(np.float32)
    return sm_f, sm_b, m_same, np.ascontiguousarray(m_c0), np.ascontiguousarray(m_c1)


GDN_STAGES = 4
GDN_SB_STEPS = 10 ** 6
GDN_AUX = "dve"


def phase_gdn(cx, g, dr, layer, sc):
    nc = cx.nc
    NT = T // 128
    HALF = NT // 2
    with ExitStack() as st0:
        msk = {}
        for nm in ("mask_f", "mask_b", "sm_f", "sm_b", "m_same", "m_c0", "m_c1"):
            msk[nm] = cx.sb(st0, f"n_{nm}", [128, 128], F32)
            cx.dma("sp", f"n_{nm}", msk[nm][:], dr[nm], [], [f"n_{nm}"])
        GLs = cx.sb(st0, "n_GL", [128, NT, 16], F32)
        gt = {nm: cx.sb(st0, f"n_{nm}", [128, NT, 8], F32) for nm in
              ("g", "beta", "Bc", "eB", "eEnd", "dec0", "dec1", "nbeta", "bEB", "tmp")}
        dtb = cx.sb(st0, "n_dtb", [128, 8], F32)
        nA = cx.sb(st0, "n_nA", [128, 8], F32)
        nwc = cx.sb(st0, "n_nwc", [128, 1], F32)
        with ExitStack() as st:
            pg = cx.ps(st, "n_pg", [128, NT, 4])
            cx.dma("sp", "n_GL", GLs[:], sc["GL"].rearrange("(n p) c -> p n c", p=128), [], ["n_GL"])
            cx.dma("sp", "n_c0", dtb[:], dr["gdn_dt_bias"][layer:layer + 1].rearrange("o a b -> o (a b)")[0].partition_broadcast(128), [], ["n_dtb"])
            cx.dma("sp", "n_c1", nA[:], dr["gdn_A_log"][layer:layer + 1].rearrange("o a b -> o (a b)")[0].partition_broadcast(128), [], ["n_nA"])
            cx.dma("sp", "n_c2", nwc[:], dr["gdn_norm"][layer].rearrange("(p o) -> p o", o=1), [], ["n_nwc"])
            cx.act(nA[:], nA[:], AF.Exp, ["n_nA"], ["n_nA"])
            cx.ts("dve", nA[:], nA[:], -1.0, None, ALU.mult, None, ["n_nA"], ["n_nA"])
            bc = lambda t: t[:].unsqueeze(1).to_broadcast([128, NT, 8])
            cx.tt("dve", gt["tmp"][:], GLs[:, :, 0:8], bc(dtb), ALU.add, ["n_GL", "n_dtb"], ["n_tmp"])
            cx.act(gt["tmp"][:], gt["tmp"][:], AF.Exp, ["n_tmp"], ["n_tmp"])
            cx.act(gt["tmp"][:], gt["tmp"][:], AF.Ln, ["n_tmp"], ["n_tmp"], bias=1.0)
            cx.tt("dve", gt["g"][:], gt["tmp"][:], bc(nA), ALU.mult, ["n_tmp", "n_nA"], ["n_g"])
            cx.act(gt["beta"][:], GLs[:, :, 8:16], AF.Sigmoid, ["n_GL"], ["n_beta"])
            cx.ts("dve", gt["nbeta"][:], gt["beta"][:], -1.0, None, ALU.mult, None, ["n_beta"], ["n_nbeta"])
            for d in range(2):
                cs = slice(d * 4, (d + 1) * 4)
                um = msk["mask_f"] if d == 0 else msk["mask_b"]
                for (mm_, dst) in ((um, "Bc"), (msk["m_same"], "eEnd"), (msk["m_c0"], "dec0"), (msk["m_c1"], "dec1")):
                    cx.mm(pg[:], mm_[:], gt["g"][:, :, cs], True, True, [f"n_{'mask_f' if d == 0 else 'mask_b'}", "n_m_same", "n_m_c0", "n_m_c1", "n_g"], ["n_pg"])
                    cx.cp("dve", gt[dst][:, :, cs], pg[:], ["n_pg"], [f"n_{dst}"])
            cx.tt("dve", gt["eEnd"][:], gt["eEnd"][:], gt["Bc"][:], ALU.subtract, ["n_eEnd", "n_Bc"], ["n_eEnd"])
            cx.act(gt["eEnd"][:], gt["eEnd"][:], AF.Exp, ["n_eEnd"], ["n_eEnd"])
            cx.act(gt["eB"][:], gt["Bc"][:], AF.Exp, ["n_Bc"], ["n_eB"])
            cx.act(gt["dec0"][:], gt["dec0"][:], AF.Exp, ["n_dec0"], ["n_dec0"])
            cx.act(gt["dec1"][:], gt["dec1"][:], AF.Exp, ["n_dec1"], ["n_dec1"])
            cx.tt("dve", gt["bEB"][:], gt["beta"][:], gt["eB"][:], ALU.mult, ["n_beta", "n_eB"], ["n_bEB"])
            cx.S.barrier()
            cx.S.emit()
        if GDN_STAGES < 2:
            return

        with ExitStack() as st:
            cw = cx.sb(st, "n_cw", [4, 1536], F32)
            cwT = cx.sb(st, "n_cwT", [128, 12, 4], F32)
            dw = cx.sb(st, "n_dw", [128, 12, 4, 128], BF16)
            xin = Rot(cx, st, "n_xin", [128, TP], BF16, 2)
            sS = Rot(cx, st, "n_s", [128, 512], F32, 2)
            sQ = Rot(cx, st, "n_sq", [128, 512], BF16, 2)
            rR = Rot(cx, st, "n_r", [128, 512], F32, 2)
            stg = Rot(cx, st, "n_stg", [128, 512], BF16, 3)
            tms = Rot(cx, st, "n_tms", [128, 4, 128], BF16, 3)
            pc = Rot(cx, st, "n_pc", [128, 512], F32, 2, psum=True)
            pn = Rot(cx, st, "n_pn", [128, 512], F32, 2, psum=True)
            pt = Rot(cx, st, "n_pt", [128, 4, 128], F32, 2, psum=True)
            pw_ = cx.ps(st, "n_pw", [128, 12, 4])
            cx.dma("sp", "n_cw", cw[:], dr["gdn_conv"][layer], [], ["n_cw"])
            for ct in range(12):
                cx.tr(pw_[:, ct, :], cw[0:4, ct * 128:(ct + 1) * 128], g.ident[0:4, 0:4], ["n_cw", "ident"], ["n_pw"])
            cx.cp("dve", cwT[:], pw_[:], ["n_pw"], ["n_cwT"])
            for ct in range(12):
                for w in range(4):
                    cx.ts(GDN_AUX if (ct + w) % 2 else "dve", dw[:, ct, w, :], g.identb[:], cwT[:, ct, w:w + 1], None, ALU.mult, None,
                          ["identb", "n_cwT"], ["n_dw"])
            for h in range(4):
                for kind in range(3):
                    ct = kind * 4 + h
                    xt, xk = xin.next()
                    cx.dma("sp", xk, xt[:], sc["FM"][FM_NC + ct], [], [xk])
                    if kind < 2:
                        dst = sc["GQT"] if kind == 0 else sc["GKT"]
                        scale = (128 ** -0.5) if kind == 0 else 1.0
                        for ch in range(T // 512):
                            t0 = ch * 512
                            p_, pk_ = pc.next()
                            for w in range(4):
                                cx.mm(p_[:], dw[:, ct, w, :], xt[:, OFF - 1 + w + t0:OFF - 1 + w + t0 + 512], w == 0, w == 3, ["n_dw", xk], [pk_])
                            s_, sk_ = sS.next()
                            cx.act(s_[:], p_[:], AF.Silu, [pk_], [sk_])
                            q_, qk_ = sQ.next()
                            cx.tt(GDN_AUX, q_[:], s_[:], s_[:], ALU.mult, [sk_], [qk_])
                            n_, nk_ = pn.next()
                            cx.mm(n_[:], g.onesb[:], q_[:], True, True, ["onesb", qk_], [nk_])
                            r_, rk_ = rR.next()
                            cx.act(r_[:], n_[:], AF.Sqrt, [nk_], [rk_], bias=EPS, scale=1.0)
                            cx.recip(r_[:], r_[:], [rk_], [rk_])
                            o_, ok_ = stg.next()
                            cx.stt("dve", o_[:], s_[:], scale, r_[:], ALU.mult, ALU.mult, [sk_, rk_], [ok_])
                            cx.dma("sp", ok_, dst[h, :, t0:t0 + 512], o_[:], [ok_], [])
                            if kind == 1:
                                t_, tk_ = pt.next()
                                for q in range(4):
                                    cx.mm(t_[:, q, :], o_[:, q * 128:(q + 1) * 128], g.identb[:], True, True, [ok_, "identb"], [tk_])
                                m_, mk_ = tms.next()
                                cx.cp("act", m_[:], t_[:], [tk_], [mk_])
                                cx.dma("sp", mk_, sc["GKM"][h, t0:t0 + 512, :].rearrange("(n p) c -> p n c", p=128), m_[:], [mk_], [])
                    else:
                        for ch in range(T // 512):
                            t0 = ch * 512
                            t_, tk_ = pt.next()
                            for q in range(4):
                                for w in range(4):
                                    c0 = OFF - 1 + w + t0 + q * 128
                                    cx.mm(t_[:, q, :], xt[:, c0:c0 + 128], dw[:, ct, w, :], w == 0, w == 3, [xk, "n_dw"], [tk_])
                            m_, mk_ = tms.next()
                            cx.act(m_[:], t_[:], AF.Silu, [tk_], [mk_])
                            cx.dma("sp", mk_, sc["GVM"][h, t0:t0 + 512, :].rearrange("(n p) c -> p n c", p=128), m_[:], [mk_], [])
            cx.S.barrier()
            cx.S.emit()
        if GDN_STAGES < 3:
            return

        NI = min(8, NT)
        with ExitStack() as st:
            bank = [cx.ps(st, f"n_bk{i}", [128, 128]) for i in range(NI)]
            bkk = [f"n_bk{i}" for i in range(NI)]
            qT8 = Rot(cx, st, "n_q8", [128, NI * 128], BF16, 2)
            kT8 = Rot(cx, st, "n_k8", [128, NI * 128], BF16, 2)
            km8 = Rot(cx, st, "n_km8", [128, NI, 128], BF16, 2)
            vm8 = Rot(cx, st, "n_vm8", [128, NI, 128], BF16, 2)
            outp = Rot(cx, st, "n_out", [128, NI, 4, 128], BF16, 2)

            def ibuf(nm, dt):
                ts_ = [cx.sb(st, f"n_{nm}{i}", [128, 128], dt) for i in range(NI)]
                return ts_, [f"n_{nm}#{i}" for i in range(NI)]
            Lg, Lgk = ibuf("Lg", F32)
            Ge, Gek = ibuf("Ge", F32)
            GM, GMk = ibuf("GM", F32)
            GI, GIk = ibuf("GI", F32)
            Tm = [ibuf(f"Tm{j}", BF16) for j in range(2)]
            Ym = [ibuf(f"Ym{j}", BF16) for j in range(2)]
            Xm, Xk = ibuf("X", BF16)
            at, atk = ibuf("at", BF16)
            bv, bvk = ibuf("bv", BF16)
            rw, rwk = ibuf("rw", BF16)
            for h in range(4):
                for d in range(2):
                    col = d * 4 + h
                    LM, LMk = (msk["mask_f"], "n_mask_f") if d == 0 else (msk["mask_b"], "n_mask_b")
                    SM, SMk = (msk["sm_b"], "n_sm_b") if d == 0 else (msk["sm_f"], "n_sm_f")
                    for n0 in range(0, NT, NI):
                        q8, q8k = qT8.next()
                        k8, k8k = kT8.next()
                        km, kmk = km8.next()
                        vm, vmk = vm8.next()
                        op_, opk = outp.next()
                        tsl = slice(n0 * 128, (n0 + NI) * 128)
                        cx.dma("sp", q8k, q8[:], sc["GQT"][h, :, tsl], [], [q8k])
                        cx.dma("sp", k8k, k8[:], sc["GKT"][h, :, tsl], [], [k8k])
                        cx.dma("sp", kmk, km[:], sc["GKM"][h, tsl, :].rearrange("(n p) c -> p n c", p=128), [], [kmk])
                        cx.dma("sp", vmk, vm[:], sc["GVM"][h, tsl, :].rearrange("(n p) c -> p n c", p=128), [], [vmk])
                        R_ = range(NI)
                        tl = lambda i: slice(i * 128, (i + 1) * 128)
                        for i in R_:
                            cx.ts("dve", Lg[i][:], LM[:], gt["g"][:, n0 + i, col:col + 1], None, ALU.mult, None, [LMk, "n_g"], [Lgk[i]])
                        for i in R_:
                            cx.mm(bank[i][:], Lg[i][:], SM[:], True, True, [Lgk[i], SMk], [bkk[i]])
                        for i in R_:
                            cx.act(Ge[i][:], bank[i][:], AF.Exp, [bkk[i]], [Gek[i]])
                        for i in R_:
                            cx.tt(GDN_AUX, GM[i][:], Ge[i][:], SM[:], ALU.mult, [Gek[i], SMk], [GMk[i]])
                        for i in R_:
                            cx.tt(GDN_AUX, GI[i][:], GM[i][:], g.ident[:], ALU.add, [GMk[i], "ident"], [GIk[i]])
                        for i in R_:
                            cx.mm(bank[i][:], k8[:, tl(i)], k8[:, tl(i)], True, True, [k8k], [bkk[i]])
                        T0, T0k = Tm[0]
                        for i in R_:
                            cx.stt("dve", T0[i][:], bank[i][:], gt["nbeta"][:, n0 + i, col:col + 1], GM[i][:], ALU.mult, ALU.mult,
                                   [bkk[i], "n_nbeta", GMk[i]], [T0k[i]])
                        for i in R_:
                            cx.mm(bank[i][:], q8[:, tl(i)], k8[:, tl(i)], True, True, [q8k, k8k], [bkk[i]])
                        for i in R_:
                            cx.tt("dve", at[i][:], bank[i][:], GI[i][:], ALU.mult, [bkk[i], GIk[i]], [atk[i]])
                        for i in R_:
                            cx.mm(bank[i][:], at[i][:], g.identb[:], True, True, [atk[i], "identb"], [bkk[i]])
                        for i in R_:
                            cx.cp("act", op_[:, i, 2, :], bank[i][:], [bkk[i]], [opk])
                        Y0, Y0k = Ym[0]
                        for i in R_:
                            cx.mm(bank[i][:], T0[i][:], g.identb[:], True, True, [T0k[i], "identb"], [bkk[i]])
                        for i in R_:
                            cx.cp("act", Y0[i][:], bank[i][:], [bkk[i]], [Y0k[i]])
                        for i in R_:
                            cx.tt(GDN_AUX, Xm[i][:], Y0[i][:], g.identb[:], ALU.add, [Y0k[i], "identb"], [Xk[i]])
                        cur = 0
                        for it in range(5):
                            Tc, Tck = Tm[cur]
                            Yc, Yck = Ym[cur]
                            Tn, Tnk = Tm[1 - cur]
                            Yn, Ynk = Ym[1 - cur]
                            for i in R_:
                                cx.mm(bank[i][:], Yc[i][:], Tc[i][:], True, True, [Yck[i], Tck[i]], [bkk[i]])
                            for i in R_:
                                cx.cp("act", Tn[i][:], bank[i][:], [bkk[i]], [Tnk[i]])
                            if it < 4:
                                for i in R_:
                                    cx.mm(bank[i][:], Tc[i][:], Yc[i][:], True, True, [Yck[i], Tck[i]], [bkk[i]])
                                for i in R_:
                                    cx.cp("dve", Yn[i][:], bank[i][:], [bkk[i]], [Ynk[i]])
                            for i in R_:
                                cx.mm(bank[i][:], Tn[i][:], Xm[i][:], True, True, [Tnk[i], Xk[i]], [bkk[i]])
                            for i in R_:
                                cx.tt("dve", Xm[i][:], Xm[i][:], bank[i][:], ALU.add, [Xk[i], bkk[i]], [Xk[i]])
                            cur = 1 - cur
                        for i in R_:
                            cx.ts(GDN_AUX, bv[i][:], vm[:, i, :], gt["beta"][:, n0 + i, col:col + 1], None, ALU.mult, None, [vmk, "n_beta"], [bvk[i]])
                        for i in R_:
                            cx.ts(GDN_AUX, rw[i][:], km[:, i, :], gt["bEB"][:, n0 + i, col:col + 1], None, ALU.mult, None, [kmk, "n_bEB"], [rwk[i]])
                        for i in R_:
                            cx.ts(GDN_AUX, op_[:, i, 3, :], km[:, i, :], gt["eEnd"][:, n0 + i, col:col + 1], None, ALU.mult, None, [kmk, "n_eEnd"], [opk])
                        for i in R_:
                            cx.mm(bank[i][:], Xm[i][:], bv[i][:], True, True, [Xk[i], bvk[i]], [bkk[i]])
                        for i in R_:
                            cx.cp("act", op_[:, i, 0, :], bank[i][:], [bkk[i]], [opk])
                        for i in R_:
                            cx.mm(bank[i][:], rw[i][:], Xm[i][:], True, True, [rwk[i], Xk[i]], [bkk[i]])
                        for i in R_:
                            cx.cp("dve", op_[:, i, 1, :], bank[i][:], [bkk[i]], [opk])
                        cx.dma("sp", opk, sc["GA"][h, d, n0:n0 + NI].rearrange("n p m c -> p n m c"), op_[:], [opk], [])
            cx.S.barrier()
            cx.S.emit()
        if GDN_STAGES < 4:
            return

        with ExitStack() as st:
            chains = [(h, d) for h in range(4) for d in range(2)]
            bank = [cx.ps(st, f"n_sbk{i}", [128, 4, 128]) for i in range(8)]
            bkk = [f"n_sbk{i}" for i in range(8)]
            Sst = [cx.sb(st, f"n_S{i}", [128, 128], BF16) for i in range(8)]
            Sk = [f"n_S{i}" for i in range(8)]
            ga = [Rot(cx, st, f"n_ga{i}_", [128, 4, 128], BF16, 2) for i in range(8)]
            qt = [Rot(cx, st, f"n_qt{i}_", [128, 128], BF16, 2) for i in range(8)]
            vn = [cx.sb(st, f"n_vn{i}", [128, 128], BF16) for i in range(8)]
            vnk = [f"n_vn{i}" for i in range(8)]
            oi = [cx.sb(st, f"n_oi{i}", [128, 128], F32) for i in range(8)]
            oik = [f"n_oi{i}" for i in range(8)]
            oo = [Rot(cx, st, f"n_oo{i}_", [128, 128], F32, 2) for i in range(8)]
            par = [Rot(cx, st, f"n_par{i}_", [128, 128], F32, 2) for i in range(8)]
            nz = [Rot(cx, st, f"n_nz{i}_", [128, 128], BF16, 2) for i in range(8)]
            on = [cx.sb(st, f"n_on{i}", [128, 128], BF16) for i in range(8)]
            onk = [f"n_on{i}" for i in range(8)]
            sj = [cx.sb(st, f"n_sj{i}", [128, 128], BF16) for i in range(8)]
            ssq = [cx.sb(st, f"n_ss{i}", [128, 2], F32) for i in range(8)]
            ssk = [f"n_ss{i}" for i in range(8)]
            yy = [Rot(cx, st, f"n_yy{i}_", [128, 128], BF16, 2) for i in range(8)]
            for i in range(8):
                cx.memset("pool", Sst[i][:], 0.0, [Sk[i]])
            for s in range(min(NT, GDN_SB_STEPS)):
                second = s >= HALF
                cur = []
                for ci, (h, d) in enumerate(chains):
                    n = s if d == 0 else NT - 1 - s
                    par2 = s % 2
                    ga_, gak = ga[ci].next()
                    cx.dma("sp", f"n_ga_{par2}", ga_[:], sc["GA"][h, d, n], [], [gak])
                    q_, qk_ = qt[ci].next()
                    cx.dma("sp", f"n_qt_{par2}", q_[:], sc["GQT"][h, :, n * 128:(n + 1) * 128], [], [qk_])
                    ex = None
                    if second:
                        p_, pk_ = par[ci].next()
                        cx.dma("sp", f"n_par_{par2}", p_[:], sc["GO"][h, n * 128:(n + 1) * 128, :], [("GO", h, n)], [pk_])
                        z_, zk_ = nz[ci].next()
                        cx.dma("sp", f"n_nz_{par2}", z_[:], sc["FM"][FM_NZ + h, :, OFF + n * 128:OFF + (n + 1) * 128], [], [zk_])
                        ex = (p_, pk_, z_, zk_)
                    cur.append((h, d, n, ga_, gak, q_, qk_, ex))
                for step in range(2):
                    for ci, (h, d, n, ga_, gak, q_, qk_, ex) in enumerate(cur):
                        c = step if d == 0 else 1 - step
                        cr = slice(c * 64, (c + 1) * 64)
                        col = d * 4 + h
                        B = bank[ci]
                        cx.mm(B[cr, 0, :], ga_[:, 1, cr], Sst[ci][:], True, True, [gak, Sk[ci]], [bkk[ci]])
                        cx.mm(B[cr, 1, :], q_[:, cr], Sst[ci][:], True, True, [qk_, Sk[ci]], [bkk[ci]])
                    for ci, (h, d, n, ga_, gak, q_, qk_, ex) in enumerate(cur):
                        c = step if d == 0 else 1 - step
                        cr = slice(c * 64, (c + 1) * 64)
                        cx.tt("dve", vn[ci][cr, :], ga_[cr, 0, :], bank[ci][cr, 0, :], ALU.subtract, [gak, bkk[ci]], [vnk[ci]])
                    for ci, (h, d, n, ga_, gak, q_, qk_, ex) in enumerate(cur):
                        c = step if d == 0 else 1 - step
                        cr = slice(c * 64, (c + 1) * 64)
                        B = bank[ci]
                        cx.mm(B[cr, 2, :], ga_[cr, 2, cr], vn[ci][cr, :], True, True, [gak, vnk[ci]], [bkk[ci]])
                        cx.mm(B[:, 3, :], ga_[cr, 3, :], vn[ci][cr, :], True, True, [gak, vnk[ci]], [bkk[ci]])
                    for ci, (h, d, n, ga_, gak, q_, qk_, ex) in enumerate(cur):
                        c = step if d == 0 else 1 - step
                        col = d * 4 + h
                        dec = gt["dec0"] if c == 0 else gt["dec1"]
                        cx.stt("dve", Sst[ci][:], Sst[ci][:], dec[:, n, col:col + 1], bank[ci][:, 3, :], ALU.mult, ALU.add,
                               [Sk[ci], f"n_dec{c}", bkk[ci]], [Sk[ci]])
                for ci, (h, d, n, ga_, gak, q_, qk_, ex) in enumerate(cur):
                    col = d * 4 + h
                    cx.ts("dve", oi[ci][:], bank[ci][:, 1, :], gt["eB"][:, n, col:col + 1], None, ALU.mult, None, [bkk[ci], "n_eB"], [oik[ci]])
                for ci, (h, d, n, ga_, gak, q_, qk_, ex) in enumerate(cur):
                    o_, ok_ = oo[ci].next()
                    cx.tt("dve", o_[:], bank[ci][:, 2, :], oi[ci][:], ALU.add, [bkk[ci], oik[ci]], [ok_])
                    if not second:
                        cx.dma("sp", f"n_oo_{s % 2}", sc["GO"][h, n * 128:(n + 1) * 128, :], o_[:], [ok_], [("GO", h, n)])
                    else:
                        p_, pk_, z_, zk_ = ex
                        cx.tt(GDN_AUX, o_[:], o_[:], p_[:], ALU.add, [ok_, pk_], [ok_])
                        cx.act(sj[ci][:], o_[:], AF.Square, [ok_], [ssk[ci]], accum_out=ssq[ci][:, 0:1])
                        cx.rstd(ssq[ci][:, 1:2], ssq[ci][:, 0:1], 1.0 / 128, ssk[ci])
                        cx.ts("dve", on[ci][:], o_[:], ssq[ci][:, 1:2], None, ALU.mult, None, [ok_, ssk[ci]], [onk[ci]])
                        cx.mm(bank[ci][:, 0, :], on[ci][:], g.identb[:], True, True, [onk[ci], "identb"], [bkk[ci]])
                        y_, yk_ = yy[ci].next()
                        cx.stt("dve", y_[:], bank[ci][:, 0, :], nwc[:, 0:1], z_[:], ALU.mult, ALU.mult, [bkk[ci], "n_nwc", zk_], [yk_])
                        cx.dma("sp", f"n_yy_{s % 2}", sc["YT"][2, h, :, n * 128:(n + 1) * 128], y_[:], [yk_], [])
            cx.phase_end()


def phase_post(cx, g, dr, layer, sc, x_in, x_out):
    nc = cx.nc
    with ExitStack() as st:
        wb = cx.sb(st, "o_wb", [128, 3, 4, D], BF16)
        wo = cx.sb(st, "o_wo", [128, KT, D], BF16)
        for n in range(3):
            cx.dma("pool", "o_wb", wb[:, n], dr["w_branch"][layer, n].rearrange("(k p) d -> p k d", p=128), [], ["o_wb"])
        cx.dma("pool", "o_wo", wo[:], dr["w_out"][layer].rearrange("(k p) d -> p k d", p=128), [], ["o_wo"])
        yT = Rot(cx, st, "o_yT", [128, 12, 512], BF16, 2)
        gT = Rot(cx, st, "o_gT", [128, 24, 512], BF16, 2)
        mT = Rot(cx, st, "o_mT", [128, KT, 512], BF16, 2)
        tmp = Rot(cx, st, "o_tmp", [128, 512], F32, 4)
        acc = Rot(cx, st, "o_acc", [128, 512], F32, 2)
        pb = Rot(cx, st, "o_pb", [128, 512], F32, 4, psum=True)
        po = Rot(cx, st, "o_po", [128, 512], F32, 2, psum=True)
        xr = Rot(cx, st, "o_xr", [128, D], F32, 2)
        yo = Rot(cx, st, "o_yo", [128, D], F32, 2)
        fs = Rot(cx, st, "o_fs", [128, 2], F32, 2)
        for ch in range(T // 512):
            t0 = ch * 512
            y_, yk = yT.next()
            cx.dma("sp", yk, y_[:].rearrange("p (n k) t -> p n k t", n=3),
                   sc["YT"][:, :, :, t0:t0 + 512].rearrange("n k p t -> p n k t"), [], [yk])
            g_, gk = gT.next()
            cx.dma("sp", gk, g_[:], sc["FM"][FM_MG:FM_MG + 24, :, OFF + t0:OFF + t0 + 512].rearrange("a p t -> p a t"), [], [gk])
            m_, mk_ = mT.next()
            for dt_ in range(KT):
                a_, ak = acc.next()
                for n in range(3):
                    p_, pk = pb.next()
                    for k in range(4):
                        cx.mm(p_[:], wb[:, n, k, dt_ * 128:(dt_ + 1) * 128], y_[:, n * 4 + k, :], k == 0, k == 3, ["o_wb", yk], [pk])
                    gsl = g_[:, n * 8 + dt_, :]
                    if n == 0:
                        cx.tt("dve", a_[:], p_[:], gsl, ALU.mult, [pk, gk], [ak])
                    else:
                        t_, tk = tmp.next()
                        cx.tt("dve", t_[:], p_[:], gsl, ALU.mult, [pk, gk], [tk])
                        if n == 1:
                            cx.tt("pool", a_[:], a_[:], t_[:], ALU.add, [ak, tk], [ak])
                        else:
                            cx.tt("pool", m_[:, dt_, :], a_[:], t_[:], ALU.add, [ak, tk], [mk_])
            for s in range(4):
                rows = slice(t0 + s * 128, t0 + (s + 1) * 128)
                x_, xk = xr.next()
                cx.dma("sp", xk, x_[:], x_in[rows, :], [], [xk])
                o_, ok = yo.next()
                f_, fk = fs.next()
                ps_ = []
                for half in range(2):
                    p_, pk = po.next()
                    for k in range(KT):
                        cx.mm(p_[:], m_[:, k, s * 128:(s + 1) * 128], wo[:, k, half * 512:(half + 1) * 512], k == 0, k == KT - 1,
                              [mk_, "o_wo"], [pk])
                    ps_.append((p_, pk))
                    cx.cp("act", o_[:, half * 512:(half + 1) * 512], p_[:], [pk], [ok])
                cx.act(xr_junk(cx, st)[:], o_[:], AF.Square, [ok], [fk], accum_out=f_[:, 0:1])
                cx.rstd(f_[:, 1:2], f_[:, 0:1], 1.0 / D, fk)
                cx.stt("dve", o_[:], o_[:], f_[:, 1:2], g.mod[:, 2, :], ALU.mult, ALU.mult, [ok, fk, ("mod", 2)], [ok])
                cx.tt("pool", o_[:], o_[:], x_[:], ALU.add, [ok, xk], [ok])
                cx.dma("sp", ok, x_out[rows, :], o_[:], [ok], [])
        cx.phase_end()


_JUNK = {}


def xr_junk(cx, st):
    k = id(st)
    if k not in _JUNK:
        _JUNK.clear()
        _JUNK[k] = cx.sb(st, "junk", [128, D], BF16)
    return _JUNK[k]


N_CORES = 8
DEPTH = 2
PHASE_LIMIT = 99
PHASE_SKIP = ()


def build_program():
    nc = bass.Bass("TRN2", target_bir_lowering=False)
    dr = {}

    def inp(name, shape, dt=F32):
        dr[name] = nc.dram_tensor(name, list(shape), dt, kind="ExternalInput").ap()

    inp("x", [T, D]); inp("cT", [128, KT]); inp("positions", [1, T], I32)
    inp("adaln_w", [DEPTH, D, 6 * D]); inp("adaln_b", [DEPTH, 6 * D]); inp("norm_w", [DEPTH, 4, D])
    inp("w_fm", [DEPTH, D, NFMG * 512]); inp("w_tm", [DEPTH, D, NTM])
    inp("gla_gate_up", [DEPTH, 2, 16, 256]); inp("gla_gate_bias", [DEPTH, 2, 256]); inp("gla_norm", [DEPTH, 128])
    inp("diff_lambda", [DEPTH, 4, 64]); inp("diff_norm", [DEPTH, 128])
    inp("gdn_conv", [DEPTH, 4, 1536]); inp("gdn_A_log", [DEPTH, 2, 4]); inp("gdn_dt_bias", [DEPTH, 2, 4]); inp("gdn_norm", [DEPTH, 128])
    inp("w_branch", [DEPTH, 3, 512, D]); inp("w_out", [DEPTH, D, D])
    inp("ffn_w1", [1, D, D_FF]); inp("ffn_w3", [1, D, D_FF]); inp("ffn_w2", [1, D_FF, D])
    inp("router_w", [1, D, N_EXP]); inp("moe_w1", [1, N_EXP, D, D_EXP]); inp("moe_w3", [1, N_EXP, D, D_EXP]); inp("moe_w2", [1, N_EXP, D_EXP, D])
    for nm in ("ident", "mask_f", "mask_b", "sm_f", "sm_b", "m_same", "m_c0", "m_c1"):
        inp(nm, [128, 128])
    inp("reset64", [128, 512]); inp("rot_inv", [128, 1]); inp("rot_sgn", [128, 1])
    out = nc.dram_tensor("out", [T, D], F32, kind="ExternalOutput").ap()

    sc = {}

    def scr(name, shape, dt):
        sc[name] = nc.dram_tensor(name, list(shape), dt, kind="Internal").ap()

    NT = T // 128
    PAGE_EL = 268435456 // 2
    arena = nc.dram_tensor("arenaA", [PAGE_EL], BF16, kind="Internal").ap()
    off = [0]

    def carve(name, shape, pattern, **kw):
        n = int(np.prod(shape))
        sc[name] = arena[off[0]:off[0] + n].rearrange(pattern, **kw)
        off[0] += n
        assert off[0] <= PAGE_EL

    carve("FM", [NFM, 128, TP], "(a p t) -> a p t", a=NFM, p=128)
    carve("TM", [T, NTM], "(t c) -> t c", c=NTM)
    carve("YT", [3, 4, 128, T], "(n k p t) -> n k p t", n=3, k=4, p=128)
    carve("GQT", [4, 128, T], "(h p t) -> h p t", h=4, p=128)
    carve("GKT", [4, 128, T], "(h p t) -> h p t", h=4, p=128)
    carve("GKM", [4, T, 128], "(h t c) -> h t c", h=4, c=128)
    carve("GVM", [4, T, 128], "(h t c) -> h t c", h=4, c=128)
    scr("GA", [4, 2, NT, 128, 4, 128], BF16)
    scr("GL", [T, 16], F32); scr("OFW", [4, 128, T], F32); scr("GO", [4, T, 128], F32)
    scr("XA", [T, D], F32); scr("XB", [T, D], F32)

    with ExitStack() as st:
        cx = Ctx(nc, st)
        g = setup_globals(cx, st, dr)
        nph = 0
        for layer in range(DEPTH):
            x_in = dr["x"] if layer == 0 else sc["XB"]
            x_out = sc["XB"] if layer == 0 else out
            if layer % 2 == 0:
                ex = [(dr["ffn_w1"][layer // 2], dr["ffn_w3"][layer // 2], dr["ffn_w2"][layer // 2])]
                rw = None
            else:
                ex = [(dr["moe_w1"][layer // 2, e], dr["moe_w3"][layer // 2, e], dr["moe_w2"][layer // 2, e]) for e in range(N_EXP)]
                rw = dr["router_w"][layer // 2]
            phases = [
                lambda: phase_mod(cx, g, dr, layer),
                lambda: phase_proj(cx, g, dr, x_in, layer, sc),
                lambda: phase_diff(cx, g, dr, layer, sc),
                lambda: phase_gla(cx, g, dr, layer, sc),
                lambda: phase_gdn(cx, g, dr, layer, sc),
                lambda: phase_post(cx, g, dr, layer, sc, x_in, sc["XA"]),
                lambda: phase_ffn(cx, g, dr, sc["XA"], x_out, ex, rw),
            ]
            for ph in phases:
                if nph < PHASE_LIMIT and nph not in PHASE_SKIP:
                    ph()
                nph += 1
    return nc


def kernel(x, c, positions, adaln_w, adaln_b, norm_w, w_in, gla_gate_up, gla_gate_bias, gla_norm,
           diff_lambda, diff_norm, gdn_conv, gdn_A_log, gdn_dt_bias, gdn_norm, w_branch, w_out,
           ffn_w1, ffn_w3, ffn_w2, router_w, moe_w1, moe_w3, moe_w2):
    f = lambda a: np.ascontiguousarray(np.asarray(a, dtype=np.float32))
    x = f(x); c = f(c)
    positions = np.ascontiguousarray(np.asarray(positions).astype(np.int32))
    w_in = f(w_in)
    packed = [pack_w_in(w_in[l]) for l in range(DEPTH)]
    w_fm = np.stack([p[0] for p in packed])
    w_tm = np.stack([p[1] for p in packed])
    mf, mb, rs64 = chunk_masks()
    sm_f, sm_b, m_same, m_c0, m_c1 = gdn_masks()
    inv, sgn = rot_consts()
    shared = {
        "adaln_w": f(adaln_w), "adaln_b": f(adaln_b), "norm_w": f(norm_w), "w_fm": w_fm, "w_tm": w_tm,
        "gla_gate_up": f(gla_gate_up), "gla_gate_bias": f(gla_gate_bias), "gla_norm": f(gla_norm),
        "diff_lambda": f(diff_lambda), "diff_norm": f(diff_norm),
        "gdn_conv": f(gdn_conv), "gdn_A_log": f(gdn_A_log), "gdn_dt_bias": f(gdn_dt_bias), "gdn_norm": f(gdn_norm),
        "w_branch": f(w_branch), "w_out": f(w_out), "ffn_w1": f(ffn_w1), "ffn_w3": f(ffn_w3), "ffn_w2": f(ffn_w2),
        "router_w": f(router_w), "moe_w1": f(moe_w1), "moe_w3": f(moe_w3), "moe_w2": f(moe_w2),
        "ident": np.eye(128, dtype=np.float32), "mask_f": mf, "mask_b": mb, "sm_f": sm_f, "sm_b": sm_b,
        "m_same": m_same, "m_c0": m_c0, "m_c1": m_c1, "reset64": rs64, "rot_inv": inv, "rot_sgn": sgn,
    }
    in_maps = []
    for core in range(N_CORES):
        b = core % 4
        m = dict(shared)
        m["x"] = x[b]
        m["cT"] = np.ascontiguousarray(c[b].reshape(KT, 128).T)
        m["positions"] = positions[b:b + 1]
        in_maps.append(m)
    nc = build_program()
    res = run_bass_kernel_spmd(nc, in_maps, core_ids=list(range(N_CORES)))
    return np.stack([np.asarray(res.results[b]["out"]) for b in range(4)]).astype(np.float32)
```

```python
from contextlib import ExitStack
import math
import numpy as np
import concourse.bass as bass
import concourse.mybir as mybir
from concourse.bass_utils import run_bass_kernel_spmd

F32 = mybir.dt.float32
BF16 = mybir.dt.bfloat16
I32 = mybir.dt.int32
AF = mybir.ActivationFunctionType
ALU = mybir.AluOpType
AX = mybir.AxisListType

D = 1024
T = 8192
KT = D // 128
EPS = 1e-6
D_FF = 2816
N_EXP = 8
D_EXP = 3584
ST = 2048
NSUB = ST // 128

SEM_CHUNK = 10 ** 9


class Sched:
    ENG = ("pe", "act", "dve", "pool", "sp")
    ENGMAP = {"pe": "tensor", "act": "scalar", "dve": "vector", "pool": "gpsimd", "sp": "sync"}
    DMA_RETIRE = 4000
    MAX_INFLIGHT = 8

    def __init__(self, nc, stack):
        self.nc = nc
        self.stack = stack
        self.q = {e: [] for e in self.ENG}
        self.cnt = {e: 0 for e in self.ENG}
        self.eng_sems = {e: [] for e in self.ENG}
        self.phys = []
        self.free = []
        self.key2phys = {}
        self.writers = {}
        self.readers = {}
        self.seen = {e: {} for e in self.ENG}
        self.nsem = 0
        self.ninstr = 0
        self.inflight = {}
        self.ninflight = {}

    def _new_sem(self, name):
        self.nsem += 1
        return self.stack.enter_context(self.nc.semaphore(name))

    def _eng_sem(self, e, idx):
        k = (idx - 1) // SEM_CHUNK
        while len(self.eng_sems[e]) <= k:
            self.eng_sems[e].append(self._new_sem(f"s_{e}_{len(self.eng_sems[e])}"))
        return self.eng_sems[e][k], (idx - 1) % SEM_CHUNK + 1

    def _phys_of(self, key, eng):
        p = self.key2phys.get(key)
        if p is None:
            fl = [i for i in self.free if self.phys[i][2] == eng]
            if fl:
                p = fl[-1]
                self.free.remove(p)
            else:
                p = len(self.phys)
                self.phys.append([self._new_sem(f"d_{p}"), 0, eng])
            self.key2phys[key] = p
        assert self.phys[p][2] == eng, (key, eng)
        return p

    def _unit_wait(self, unit, idx):
        if unit[0] == "e":
            return self._eng_sem(unit[1], idx)
        return self.phys[unit[1]][0], idx * 16

    def _collect(self, eng, reads, writes):
        need = {}

        def add(d):
            for u, i in d.items():
                if need.get(u, 0) < i:
                    need[u] = i
        for b in reads:
            add(self.writers.get(b, {}))
        for b in writes:
            add(self.writers.get(b, {}))
            add(self.readers.get(b, {}))
        return self._filter(eng, need)

    def _filter(self, eng, need):
        waits = []
        seen = self.seen[eng]
        for u, i in need.items():
            if u == ("e", "pe") and eng == "pe":
                continue
            if u[0] == "d":
                i = self.phys[u[1]][1]
            if seen.get(u, 0) >= i:
                continue
            seen[u] = i
            waits.append(self._unit_wait(u, i))
        return waits

    def op(self, eng, fn, reads=(), writes=()):
        waits = self._collect(eng, reads, writes)
        self.cnt[eng] += 1
        idx = self.cnt[eng]
        sem, _ = self._eng_sem(eng, idx)
        self.q[eng].append((waits, fn, sem, 1))
        u = ("e", eng)
        for b in reads:
            self.readers.setdefault(b, {})[u] = idx
        for b in writes:
            self.writers.setdefault(b, {})[u] = idx
        self.ninstr += 1

    def dma(self, eng, key, fn, reads=(), writes=()):
        waits = self._collect(eng, reads, writes)
        out = self.inflight.setdefault(eng, set())
        if self.ninflight.get(eng, 0) >= self.MAX_INFLIGHT:
            waits = waits + self._filter(eng, {("d", p_): self.phys[p_][1] for p_ in out})
            out.clear()
            self.ninflight[eng] = 0
        p = self._phys_of(key, eng)
        out.add(p)
        self.ninflight[eng] = self.ninflight.get(eng, 0) + 1
        self.phys[p][1] += 1
        idx = self.phys[p][1]
        self.q[eng].append((waits, fn, self.phys[p][0], 16))
        u = ("d", p)
        for b in reads:
            self.readers.setdefault(b, {})[u] = idx
        for b in writes:
            self.writers.setdefault(b, {})[u] = idx
        self.ninstr += 1

    def barrier(self):
        need = {("e", e): c for e, c in self.cnt.items() if c > 0}
        for p, (sem, c, _q) in enumerate(self.phys):
            if c > 0:
                need[("d", p)] = c
        for e in self.ENG:
            waits = self._filter(e, dict(need))
            self.q[e].append((waits, None, None, 0))
        self.writers = {}
        self.readers = {}
        self.key2phys = {}
        self.inflight = {}
        self.ninflight = {}
        self.free = [p for p, (sem, c, _q) in enumerate(self.phys) if c < self.DMA_RETIRE]

    def emit(self):
        nc = self.nc
        with nc.Block() as block:
            for e in self.ENG:
                items = self.q[e]
                if not items:
                    continue

                def body(engine, items=items):
                    for waits, fn, sem, inc in items:
                        for (ws, wv) in waits:
                            engine.wait_ge(ws, wv)
                        if fn is not None:
                            fn().then_inc(sem, inc)
                getattr(block, self.ENGMAP[e])(body)
        self.q = {e: [] for e in self.ENG}


class Ctx:
    def __init__(self, nc, stack):
        self.nc = nc
        self.S = Sched(nc, stack)
        self.stack = stack
        self.uid = 0

    def sb(self, st, name, shape, dt):
        self.uid += 1
        return st.enter_context(self.nc.sbuf_tensor(f"{name}_{self.uid}", shape, dt))

    def ps(self, st, name, shape, dt=F32):
        self.uid += 1
        return st.enter_context(self.nc.psum_tensor(f"{name}_{self.uid}", shape, dt))

    def mm(self, out, lhsT, rhs, start, stop, r, w):
        nc = self.nc
        self.S.op("pe", lambda: nc.tensor.matmul(out, lhsT, rhs, start=start, stop=stop), r, w)

    def tr(self, out, in_, ident, r, w):
        nc = self.nc
        self.S.op("pe", lambda: nc.tensor.transpose(out, in_, ident), r, w)

    def act(self, out, in_, func, r, w, bias=None, scale=1.0, accum_out=None):
        nc = self.nc
        kw = {}
        if bias is not None:
            kw["bias"] = bias
        if accum_out is not None:
            kw["accum_out"] = accum_out
        self.S.op("act", lambda: nc.scalar.activation(out=out, in_=in_, func=func, scale=scale, **kw), r, w)

    def _veng(self, eng):
        return self.nc.vector if eng == "dve" else self.nc.gpsimd

    def tt(self, eng, out, in0, in1, op, r, w):
        e = self._veng(eng)
        self.S.op(eng, lambda: e.tensor_tensor(out=out, in0=in0, in1=in1, op=op), r, w)

    def ts(self, eng, out, in0, s1, s2, op0, op1, r, w):
        e = self._veng(eng)
        if op1 is None:
            self.S.op(eng, lambda: e.tensor_scalar(out=out, in0=in0, scalar1=s1, scalar2=None, op0=op0), r, w)
        else:
            self.S.op(eng, lambda: e.tensor_scalar(out=out, in0=in0, scalar1=s1, scalar2=s2, op0=op0, op1=op1), r, w)

    def stt(self, eng, out, in0, scalar, in1, op0, op1, r, w):
        e = self._veng(eng)
        self.S.op(eng, lambda: e.scalar_tensor_tensor(out=out, in0=in0, scalar=scalar, in1=in1, op0=op0, op1=op1), r, w)

    def cp(self, eng, out, in_, r, w):
        nc = self.nc
        if eng == "act":
            self.S.op("act", lambda: nc.scalar.copy(out=out, in_=in_), r, w)
        else:
            e = self._veng(eng)
            self.S.op(eng, lambda: e.tensor_copy(out=out, in_=in_), r, w)

    def memset(self, eng, ap, val, w):
        e = self._veng(eng)
        self.S.op(eng, lambda: e.memset(ap, val), (), w)

    def reduce(self, eng, out, in_, op, r, w):
        e = self._veng(eng)
        self.S.op(eng, lambda: e.tensor_reduce(out=out, in_=in_, axis=AX.X, op=op), r, w)

    def dma(self, q, key, out, in_, r, w):
        nc = self.nc
        e = {"sp": nc.sync, "pool": nc.gpsimd, "act": nc.scalar}[q]
        self.S.dma(q, key, lambda: e.dma_start(out=out, in_=in_), r, w)

    def recip(self, out, in_, r, w):
        nc = self.nc
        self.S.op("dve", lambda: nc.vector.reciprocal(out=out, in_=in_), r, w)

    def rstd(self, out, in_, scale, key, eps=EPS):
        self.act(out, in_, AF.Sqrt, [key], [key], bias=eps, scale=scale)
        self.recip(out, out, [key], [key])

    def gather(self, key, out, in_full, idx_col, r, w):
        nc = self.nc
        self.S.dma("pool", key, lambda: nc.gpsimd.indirect_dma_start(
            out=out, out_offset=None, in_=in_full, in_offset=bass.IndirectOffsetOnAxis(ap=idx_col, axis=0)), r, w)

    def phase_end(self):
        self.S.barrier()
        self.S.emit()


def host_consts():
    c = {}
    c["ident"] = np.eye(128, dtype=np.float32)
    return c


class Glob:
    pass


def setup_globals(cx, st, dr):
    nc = cx.nc
    g = Glob()
    g.ident = cx.sb(st, "ident", [128, 128], F32)
    g.identb = cx.sb(st, "identb", [128, 128], BF16)
    g.ones32 = cx.sb(st, "ones32", [128, 128], F32)
    g.onesb = cx.sb(st, "onesb", [128, 128], BF16)
    g.zeros32 = cx.sb(st, "zeros32", [128, 128], F32)
    cx.dma("sp", "g_ident", g.ident[:], dr["ident"], [], ["ident"])
    cx.cp("dve", g.identb[:], g.ident[:], ["ident"], ["identb"])
    cx.memset("pool", g.ones32[:], 1.0, ["ones32"])
    cx.memset("pool", g.onesb[:], 1.0, ["onesb"])
    cx.memset("pool", g.zeros32[:], 0.0, ["zeros32"])
    g.mod = cx.sb(st, "mod", [128, 6, D], F32)
    return g


def phase_mod(cx, g, dr, layer):
    nc = cx.nc
    with ExitStack() as st:
        cT = cx.sb(st, "cT", [128, KT], F32)
        cs = cx.sb(st, "cs", [128, KT], F32)
        CB = cx.sb(st, "CB", [128, KT, 128], BF16)
        bias = cx.sb(st, "abias", [1, 6 * D], BF16)
        nwb = cx.sb(st, "nwb", [128, 4, D], F32)
        wbuf = [cx.sb(st, f"aw{i}", [128, KT, 512], BF16) for i in range(2)]
        pm = [cx.ps(st, f"pm{i}", [128, 512]) for i in range(2)]
        cx.dma("sp", "m_c", cT[:], dr["cT"], [], ["cT"])
        cx.act(cs[:], cT[:], AF.Silu, ["cT"], ["cs"])
        for kt in range(KT):
            cx.act(CB[:, kt, :], g.zeros32[:], AF.Identity, ["cs", "zeros32"], ["CB"], bias=cs[:, kt:kt + 1])
        cx.dma("pool", "m_b", bias[:].rearrange("o (c n) -> o c n", n=512),
               dr["adaln_b"][layer:layer + 1, :].rearrange("o (c n) -> o c n", n=512), [], ["abias"])
        cx.dma("sp", "m_nw", nwb[:].rearrange("p a d -> p (a d)"),
               dr["norm_w"][layer:layer + 1].rearrange("o a d -> o (a d)")[0].partition_broadcast(128), [], ["nwb"])
        wv = dr["adaln_w"][layer].rearrange("(kt p) n -> p kt n", p=128)
        for ch in range(12):
            wb = wbuf[ch % 2]
            wk = f"aw{ch % 2}"
            cx.dma("pool", wk, wb[:], wv[:, :, ch * 512:(ch + 1) * 512], [], [wk])
            p = pm[ch % 2]
            pk = f"pm{ch % 2}"
            for kt in range(KT):
                cx.mm(p[:], CB[:, kt, :], wb[:, kt, :], kt == 0, False, ["CB", wk], [pk])
            cx.mm(p[:], g.onesb[0:1, :], bias[0:1, ch * 512:(ch + 1) * 512], False, True, ["onesb", "abias"], [pk])
            j, half = ch // 2, ch % 2
            slot = {0: 1, 1: 0, 2: 2, 3: 4, 4: 3, 5: 5}[j]
            cx.cp("dve", g.mod[:, slot, half * 512:(half + 1) * 512], p[:], [pk], [("mod", slot)])
        for slot, nwi in ((0, 0), (3, 2)):
            cx.stt("dve", g.mod[:, slot, :], g.mod[:, slot, :], 1.0, nwb[:, nwi, :], ALU.add, ALU.mult,
                   [("mod", slot), "nwb"], [("mod", slot)])
        for slot, nwi in ((2, 1), (5, 3)):
            cx.tt("dve", g.mod[:, slot, :], g.mod[:, slot, :], nwb[:, nwi, :], ALU.mult,
                  [("mod", slot), "nwb"], [("mod", slot)])
        cx.phase_end()


class HTMaker:
    def __init__(self, cx, st, g, tag):
        self.cx, self.g, self.tag = cx, g, tag
        self.xt = [cx.sb(st, f"{tag}xt{i}", [128, D], F32) for i in range(2)]
        self.h = [cx.sb(st, f"{tag}h{i}", [128, D], F32) for i in range(2)]
        self.junk = cx.sb(st, f"{tag}junk", [128, D], BF16)
        self.ss = [cx.sb(st, f"{tag}ss{i}", [128, 2], F32) for i in range(2)]
        self.pT = [cx.ps(st, f"{tag}pT{i}", [128, 4, 128]) for i in range(2)]
        self.n = 0

    def run(self, x_rows, aslot, shslot, hT, hTkey, col0, gather=None):
        cx, g, tag = self.cx, self.g, self.tag
        i = self.n % 2
        self.n += 1
        xt, h, ss = self.xt[i], self.h[i], self.ss[i]
        kx, kh, ks = f"{tag}xt{i}", f"{tag}h{i}", f"{tag}ss{i}"
        if gather is None:
            cx.dma("sp", kx, xt[:], x_rows, [], [kx])
        else:
            cx.gather("g" + kx, xt[:, :], gather[0], gather[1], [gather[2]], [kx])
        cx.act(self.junk[:], xt[:], AF.Square, [kx], [ks], accum_out=ss[:, 0:1])
        cx.rstd(ss[:, 1:2], ss[:, 0:1], 1.0 / D, ks)
        cx.stt("dve", h[:], xt[:], ss[:, 1:2], g.mod[:, aslot, :], ALU.mult, ALU.mult, [kx, ks, ("mod", aslot)], [kh])
        cx.tt("pool", h[:], h[:], g.mod[:, shslot, :], ALU.add, [kh, ("mod", shslot)], [kh])
        for half in range(2):
            p = self.pT[half]
            pk = f"{tag}pT{half}"
            for q in range(4):
                kt = half * 4 + q
                cx.tr(p[:, q, :], h[:, kt * 128:(kt + 1) * 128], g.ident[:], [kh, "ident"], [pk])
            cx.cp("act", hT[:, half * 4:(half + 1) * 4, col0:col0 + 128], p[:], [pk], [hTkey])


def phase_ffn(cx, g, dr, x_in, x_out, experts, router=None, tok_range=None, tokidx=None):
    nc = cx.nc
    if tok_range is None:
        tok_range = (0, T)
    ST = min(globals()["ST"], tok_range[1] - tok_range[0])
    NSUB = ST // 128
    F = experts[0][0].shape[1]
    FG = 256
    NG = F // FG
    with ExitStack() as st:
        hm = HTMaker(cx, st, g, "f")
        hT = cx.sb(st, "f_hT", [128, KT, ST], BF16)
        acc = cx.sb(st, "f_acc", [128, NSUB, D], F32)
        actT = cx.sb(st, "f_actT", [128, 2, ST], BF16)
        w1b = [cx.sb(st, f"f_w1_{i}", [128, KT, FG], BF16) for i in range(2)]
        w3b = [cx.sb(st, f"f_w3_{i}", [128, KT, FG], BF16) for i in range(2)]
        w2b = [cx.sb(st, f"f_w2_{i}", [128, 2, D], BF16) for i in range(2)]
        sil = [cx.sb(st, f"f_sil{i}", [128, 512], BF16) for i in range(2)]
        pu1 = [cx.ps(st, f"f_pu1_{i}", [128, 512]) for i in range(2)]
        pu3 = [cx.ps(st, f"f_pu3_{i}", [128, 512]) for i in range(2)]
        py = [cx.ps(st, f"f_py{i}", [128, 512]) for i in range(2)]
        if router is not None:
            wr = cx.sb(st, "f_wr", [128, KT, N_EXP], BF16)
            comb = cx.sb(st, "f_comb", [128, NSUB, N_EXP], F32)
            rt = cx.sb(st, "f_rt", [128, 8, N_EXP], F32)
            rs = cx.sb(st, "f_rs", [128, 8], F32)
            cx.dma("pool", "f_wr", wr[:], router.rearrange("(kt p) n -> p kt n", p=128), [], ["f_wr"])
        yo = [cx.sb(st, f"f_yo{i}", [128, D], F32) for i in range(2)]
        xr = [cx.sb(st, f"f_xr{i}", [128, D], F32) for i in range(2)]
        fs = [cx.sb(st, f"f_fs{i}", [128, 2], F32) for i in range(2)]
        gcount = 0
        if tokidx is not None:
            idx = cx.sb(st, "f_idx", [128, (tok_range[1] - tok_range[0]) // 128], I32)
            cx.dma("sp", "f_idx", idx[:], tokidx, [], ["f_idx"])
        for t0 in range(tok_range[0], tok_range[1], ST):
            for s in range(NSUB):
                j = (t0 - tok_range[0]) // 128 + s
                gth = None if tokidx is None else (x_in[:, :], idx[:, j:j + 1], "f_idx")
                hm.run(x_in[t0 + s * 128:t0 + (s + 1) * 128, :], 3, 4, hT, "f_hT", s * 128, gather=gth)
            if router is not None:
                for s in range(NSUB):
                    pr = py[s % 2]
                    pk = f"f_py{s % 2}"
                    for kt in range(KT):
                        cx.mm(pr[:, 0:N_EXP], hT[:, kt, s * 128:(s + 1) * 128], wr[:, kt, :], kt == 0, kt == KT - 1,
                              ["f_hT", "f_wr"], [pk])
                    L, EQ, L2, SEL, EX = (rt[:, i, :] for i in range(5))
                    cx.cp("dve", L, pr[:, 0:N_EXP], [pk], ["f_rt"])
                    cx.reduce("dve", rs[:, 0:1], L, ALU.max, ["f_rt"], ["f_rs"])
                    cx.ts("dve", EQ, L, rs[:, 0:1], None, ALU.is_equal, None, ["f_rt", "f_rs"], ["f_rt"])
                    cx.stt("dve", L2, EQ, -1e30, L, ALU.mult, ALU.add, ["f_rt"], ["f_rt"])
                    cx.reduce("dve", rs[:, 1:2], L2, ALU.max, ["f_rt"], ["f_rs"])
                    cx.ts("dve", SEL, L, rs[:, 1:2], None, ALU.is_ge, None, ["f_rt", "f_rs"], ["f_rt"])
                    cx.ts("dve", rs[:, 2:3], rs[:, 0:1], -1.0, None, ALU.mult, None, ["f_rs"], ["f_rs"])
                    cx.act(EX, L, AF.Exp, ["f_rt", "f_rs"], ["f_rt"], bias=rs[:, 2:3])
                    cx.tt("dve", EX, EX, SEL, ALU.mult, ["f_rt"], ["f_rt"])
                    cx.reduce("dve", rs[:, 3:4], EX, ALU.add, ["f_rt"], ["f_rs"])
                    cx.S.op("dve", (lambda o=rs[:, 4:5], i_=rs[:, 3:4]: nc.vector.reciprocal(out=o, in_=i_)), ["f_rs"], ["f_rs"])
                    cx.ts("dve", comb[:, s, :], EX, rs[:, 4:5], None, ALU.mult, None, ["f_rt", "f_rs"], ["f_comb"])
            first = True
            for e, (w1, w3, w2) in enumerate(experts):
                w1v = w1.rearrange("(kt p) n -> p kt n", p=128)
                w3v = w3.rearrange("(kt p) n -> p kt n", p=128)
                w2v = w2.rearrange("(f p) n -> p f n", p=128)
                for gi in range(NG):
                    b = gcount % 2
                    gcount += 1
                    k1, k3, k2 = f"f_w1_{b}", f"f_w3_{b}", f"f_w2_{b}"
                    cx.dma("pool", k1, w1b[b][:], w1v[:, :, gi * FG:(gi + 1) * FG], [], [k1])
                    cx.dma("pool", k3, w3b[b][:], w3v[:, :, gi * FG:(gi + 1) * FG], [], [k3])
                    cx.dma("pool", k2, w2b[b][:], w2v[:, gi * 2:(gi + 1) * 2, :], [], [k2])
                    for ch in range(ST // 512):
                        for f in range(2):
                            pb = (ch * 2 + f) % 2
                            for kt in range(KT):
                                cx.mm(pu1[pb][:], w1b[b][:, kt, f * 128:(f + 1) * 128], hT[:, kt, ch * 512:(ch + 1) * 512],
                                      kt == 0, kt == KT - 1, [k1, "f_hT"], [f"f_pu1{pb}"])
                            for kt in range(KT):
                                cx.mm(pu3[pb][:], w3b[b][:, kt, f * 128:(f + 1) * 128], hT[:, kt, ch * 512:(ch + 1) * 512],
                                      kt == 0, kt == KT - 1, [k3, "f_hT"], [f"f_pu3{pb}"])
                            sb_ = sil[pb]
                            sk = f"f_sil{pb}"
                            cx.act(sb_[:], pu1[pb][:], AF.Silu, [f"f_pu1{pb}"], [sk])
                            cx.tt("dve", actT[:, f, ch * 512:(ch + 1) * 512], sb_[:], pu3[pb][:], ALU.mult,
                                  [sk, f"f_pu3{pb}"], [("f_actT", ch)])
                    for s in range(NSUB):
                        for half in range(2):
                            p = py[(s * 2 + half) % 2]
                            pk = f"f_py{(s * 2 + half) % 2}"
                            for f in range(2):
                                cx.mm(p[:], actT[:, f, s * 128:(s + 1) * 128], w2b[b][:, f, half * 512:(half + 1) * 512],
                                      f == 0, f == 1, [("f_actT", s // 4), k2], [pk])
                            a = acc[:, s, half * 512:(half + 1) * 512]
                            ak = ("f_acc", s)
                            if router is None:
                                if first:
                                    cx.cp("dve", a, p[:], [pk], [ak])
                                else:
                                    cx.tt("dve", a, a, p[:], ALU.add, [pk, ak], [ak])
                            else:
                                if first:
                                    cx.ts("dve", a, p[:], comb[:, s, e:e + 1], None, ALU.mult, None, [pk, "f_comb"], [ak])
                                else:
                                    cx.stt("dve", a, p[:], comb[:, s, e:e + 1], a, ALU.mult, ALU.add, [pk, "f_comb", ak], [ak])
                    first = False
            for s in range(NSUB):
                i = s % 2
                ky, kx, kf = f"f_yo{i}", f"f_xr{i}", f"f_fs{i}"
                rows = slice(t0 + s * 128, t0 + (s + 1) * 128)
                if tokidx is None:
                    cx.dma("sp", kx, xr[i][:], x_in[rows, :], [], [kx])
                else:
                    j = (t0 - tok_range[0]) // 128 + s
                    cx.gather("g" + kx, xr[i][:, :], x_in[:, :], idx[:, j:j + 1], ["f_idx"], [kx])
                cx.act(yo[i][:], acc[:, s, :], AF.Square, [("f_acc", s)], [ky, kf], accum_out=fs[i][:, 0:1])
                cx.rstd(fs[i][:, 1:2], fs[i][:, 0:1], 1.0 / D, kf)
                cx.stt("dve", yo[i][:], acc[:, s, :], fs[i][:, 1:2], g.mod[:, 5, :], ALU.mult, ALU.mult,
                       [("f_acc", s), kf, ("mod", 5)], [ky])
                cx.tt("pool", yo[i][:], yo[i][:], xr[i][:], ALU.add, [ky, kx], [ky])
                cx.dma("sp", ky, x_out[rows, :], yo[i][:], [ky], [("xout", s)])
        cx.phase_end()


OFF = 2
TP = T + 6
FM_GQ, FM_GK, FM_GR, FM_GW, FM_DQ, FM_DQS, FM_DK, FM_DKS, FM_NC, FM_NZ, FM_MG = 0, 2, 4, 8, 9, 13, 17, 21, 25, 37, 41
NFM = 65
NFMG = 17
TM_GV, TM_DV, TM_GL = 0, 512, 1024
NTM = 1040

IN_SIZES = (256, 256, 512, 512, 16, 16, 512, 512, 512, 512, 512, 512, 512, 4, 4, 4, 4, 3072)
IN_OFF = np.concatenate([[0], np.cumsum(IN_SIZES)]).astype(int)
(C_GQ, C_GK, C_GV, C_GR, C_GWF, C_GWB, C_DQ, C_DK, C_DV, C_NQ, C_NK, C_NV, C_NZ, C_NBF, C_NBB, C_NAF, C_NAB, C_MG) = IN_OFF[:-1]


def pack_w_in(w):
    z = lambda n: np.zeros((D, n), np.float32)
    rng = lambda a, n: w[:, a:a + n]
    swap = np.arange(512)
    for hd in range(8):
        for i in range(16):
            swap[hd * 64 + i] = hd * 64 + (i + 8 if i < 8 else i - 8)
    fm = [rng(C_GQ, 256), rng(C_GK, 256), rng(C_GR, 512),
          rng(C_GWF, 16), z(16), rng(C_GWB, 16), z(80),
          rng(C_DQ, 512), rng(C_DQ, 512)[:, swap], rng(C_DK, 512), rng(C_DK, 512)[:, swap],
          rng(C_NQ, 512), rng(C_NK, 512), rng(C_NV, 512), rng(C_NZ, 512), rng(C_MG, 3072), z(NFMG * 512 - NFM * 128)]
    fm = np.concatenate(fm, axis=1)
    assert fm.shape[1] == NFMG * 512
    tm = np.concatenate([rng(C_GV, 512), rng(C_DV, 512), rng(C_NAF, 4), rng(C_NAB, 4), rng(C_NBF, 4), rng(C_NBB, 4)], axis=1)
    assert tm.shape[1] == NTM
    return np.ascontiguousarray(fm), np.ascontiguousarray(tm)


def fm_evac_kind(tile):
    if tile < FM_GK:
        return ("scale", 0.125)
    if FM_GR <= tile < FM_GW:
        return ("act", AF.Silu)
    if FM_NZ <= tile < FM_MG:
        return ("act", AF.Silu)
    if tile >= FM_MG:
        return ("act", AF.Sigmoid)
    return ("copy", None)


def phase_proj(cx, g, dr, x_in, layer, sc):
    nc = cx.nc
    with ExitStack() as st:
        hm = HTMaker(cx, st, g, "p")
        hT = cx.sb(st, "p_hT", [128, KT, ST], BF16)
        wb = [cx.sb(st, f"p_w{i}", [128, KT, 512], BF16) for i in range(2)]
        stg = [cx.sb(st, f"p_stg{i}", [128, ST], BF16) for i in range(2)]
        tstg = [cx.sb(st, f"p_tstg{i}", [128, 512], BF16) for i in range(2)]
        gstg = [cx.sb(st, f"p_gstg{i}", [128, 16], F32) for i in range(2)]
        zpad = cx.sb(st, "p_zpad", [128, 8], BF16)
        pp = [cx.ps(st, f"p_pp{i}", [128, 512]) for i in range(4)]
        wfm = dr["w_fm"][layer].rearrange("(kt p) n -> p kt n", p=128)
        wtm = dr["w_tm"][layer].rearrange("(kt p) n -> p kt n", p=128)
        cx.memset("pool", zpad[:], 0.0, ["p_zpad"])
        for tile in range(FM_NC, FM_NC + 12):
            cx.dma("sp", "p_zp", sc["FM"][tile, :, 0:OFF], zpad[:, 0:OFF], ["p_zpad"], [])
            cx.dma("sp", "p_zp", sc["FM"][tile, :, OFF + T:TP], zpad[:, 0:TP - OFF - T], ["p_zpad"], [])
        wcount = 0
        pcount = 0
        scount = 0
        for si in range(T // ST):
            t0 = si * ST
            for s in range(NSUB):
                hm.run(x_in[t0 + s * 128:t0 + (s + 1) * 128, :], 0, 1, hT, "p_hT", s * 128)
            for gi in range(NFMG):
                b = wcount % 2
                wcount += 1
                wk = f"p_w{b}"
                cx.dma("pool", wk, wb[b][:], wfm[:, :, gi * 512:(gi + 1) * 512], [], [wk])
                for q in range(4):
                    tile = gi * 4 + q
                    if tile >= NFM:
                        break
                    sb_ = scount % 2
                    scount += 1
                    sk = f"p_stg{sb_}"
                    kind, arg = fm_evac_kind(tile)
                    for ch in range(ST // 512):
                        pb = pcount % 4
                        pcount += 1
                        pk = f"p_pp{pb}"
                        for kt in range(KT):
                            cx.mm(pp[pb][:], wb[b][:, kt, q * 128:(q + 1) * 128], hT[:, kt, ch * 512:(ch + 1) * 512],
                                  kt == 0, kt == KT - 1, [wk, "p_hT"], [pk])
                        o = stg[sb_][:, ch * 512:(ch + 1) * 512]
                        if kind == "act":
                            cx.act(o, pp[pb][:], arg, [pk], [sk])
                        elif kind == "scale":
                            cx.ts("dve", o, pp[pb][:], arg, None, ALU.mult, None, [pk], [sk])
                        else:
                            cx.cp("dve", o, pp[pb][:], [pk], [sk])
                    cx.dma("sp", sk, sc["FM"][tile, :, OFF + t0:OFF + t0 + ST], stg[sb_][:], [sk], [])
            for (c0, ncol) in ((TM_GV, 512), (TM_DV, 512), (TM_GL, 16)):
                b = wcount % 2
                wcount += 1
                wk = f"p_w{b}"
                cx.dma("pool", wk, wb[b][:, :, 0:ncol], wtm[:, :, c0:c0 + ncol], [], [wk])
                for s in range(NSUB):
                    pb = pcount % 4
                    pcount += 1
                    pk = f"p_pp{pb}"
                    for kt in range(KT):
                        cx.mm(pp[pb][:, 0:ncol], hT[:, kt, s * 128:(s + 1) * 128], wb[b][:, kt, 0:ncol],
                              kt == 0, kt == KT - 1, [wk, "p_hT"], [pk])
                    rows = slice(t0 + s * 128, t0 + (s + 1) * 128)
                    if ncol == 512:
                        tb = s % 2
                        tk = f"p_tstg{tb}"
                        cx.cp("dve" if s % 2 else "act", tstg[tb][:], pp[pb][:], [pk], [tk])
                        cx.dma("sp", tk, sc["TM"][rows, c0:c0 + 512], tstg[tb][:], [tk], [])
                    else:
                        tb = s % 2
                        tk = f"p_gstg{tb}"
                        cx.cp("dve", gstg[tb][:], pp[pb][:, 0:16], [pk], [tk])
                        cx.dma("sp", tk, sc["GL"][rows, :], gstg[tb][:], [tk], [])
        cx.phase_end()


def rot_consts():
    inv = np.zeros((128, 1), np.float32)
    sgn = np.zeros((128, 1), np.float32)
    for p in range(128):
        i = p % 64
        if i < 16:
            inv[p, 0] = 500000.0 ** (-(2 * (i % 8)) / 16.0)
            sgn[p, 0] = -1.0 if i < 8 else 1.0
    return inv, sgn


def phase_diff(cx, g, dr, layer, sc):
    nc = cx.nc
    lam_init = 0.8 - 0.6 * math.exp(-0.3 * layer)
    TWO_PI = 2.0 * math.pi
    CH = 2048
    with ExitStack() as st:
        Ct = cx.sb(st, "d_C", [128, T], BF16)
        St = cx.sb(st, "d_S", [128, T], BF16)
        inv = cx.sb(st, "d_inv", [128, 1], F32)
        sgn = cx.sb(st, "d_sgn", [128, 1], F32)
        pi_ = cx.sb(st, "d_pi", [128, CH], I32)
        ang = cx.sb(st, "d_ang", [128, CH], F32)
        tf = cx.sb(st, "d_tf", [128, CH], F32)
        ti = cx.sb(st, "d_ti", [128, CH], I32)
        fx = cx.sb(st, "d_fx", [128, CH], F32)
        cx.dma("sp", "d_c0", inv[:], dr["rot_inv"], [], ["d_inv"])
        cx.dma("sp", "d_c1", sgn[:], dr["rot_sgn"], [], ["d_sgn"])

        def reduced_sin(out_bf, shift, kout, scale_ap=None):
            cx.ts("dve", tf[:], ang[:], shift, 1.0 / TWO_PI, ALU.add, ALU.mult, ["d_ang"], ["d_tf"])
            cx.cp("dve", ti[:], tf[:], ["d_tf"], ["d_ti"])
            cx.cp("dve", tf[:], ti[:], ["d_ti"], ["d_tf"])
            cx.stt("dve", tf[:], tf[:], -TWO_PI, ang[:], ALU.mult, ALU.add, ["d_tf", "d_ang"], ["d_tf"])
            cx.ts("dve", tf[:], tf[:], shift, None, ALU.add, None, ["d_tf"], ["d_tf"])
            cx.ts("dve", fx[:], tf[:], math.pi, -TWO_PI, ALU.is_gt, ALU.mult, ["d_tf"], ["d_fx"])
            cx.tt("dve", tf[:], tf[:], fx[:], ALU.add, ["d_tf", "d_fx"], ["d_tf"])
            cx.ts("dve", fx[:], tf[:], -math.pi, TWO_PI, ALU.is_lt, ALU.mult, ["d_tf"], ["d_fx"])
            cx.tt("dve", tf[:], tf[:], fx[:], ALU.add, ["d_tf", "d_fx"], ["d_tf"])
            cx.ts("dve", tf[:], tf[:], -math.pi, math.pi, ALU.max, ALU.min, ["d_tf"], ["d_tf"])
            if scale_ap is None:
                cx.act(out_bf, tf[:], AF.Sin, ["d_tf"], [kout])
            else:
                cx.act(out_bf, tf[:], AF.Sin, ["d_tf", "d_sgn"], [kout], scale=scale_ap)

        for c in range(T // CH):
            cx.dma("sp", "d_pi", pi_[:], dr["positions"][0, c * CH:(c + 1) * CH].partition_broadcast(128), [], ["d_pi"])
            cx.cp("dve", ang[:], pi_[:], ["d_pi"], ["d_ang"])
            cx.ts("dve", ang[:], ang[:], inv[:, 0:1], None, ALU.mult, None, ["d_ang", "d_inv"], ["d_ang"])
            reduced_sin(Ct[:, c * CH:(c + 1) * CH], math.pi / 2.0, "d_C")
            reduced_sin(St[:, c * CH:(c + 1) * CH], 0.0, "d_S", scale_ap=sgn[:, 0:1])

        lp = cx.sb(st, "d_lp", [128, 4, 64], F32)
        pr = cx.sb(st, "d_pr", [128, 2, 64], F32)
        lc = cx.sb(st, "d_lc", [128, 8], F32)
        nwc = cx.sb(st, "d_nwc", [128, 1], F32)
        cx.dma("sp", "d_c2", lp[:].rearrange("p a b -> p (a b)"),
               dr["diff_lambda"][layer:layer + 1].rearrange("o a b -> o (a b)")[0].partition_broadcast(128), [], ["d_lp"])
        cx.dma("sp", "d_c3", nwc[:], dr["diff_norm"][layer].rearrange("(p o) -> p o", o=1), [], ["d_nwc"])
        cx.tt("dve", pr[:, 0, :], lp[:, 0, :], lp[:, 1, :], ALU.mult, ["d_lp"], ["d_pr"])
        cx.tt("dve", pr[:, 1, :], lp[:, 2, :], lp[:, 3, :], ALU.mult, ["d_lp"], ["d_pr"])
        cx.reduce("dve", lc[:, 0:1], pr[:, 0, :], ALU.add, ["d_pr"], ["d_lc"])
        cx.reduce("dve", lc[:, 1:2], pr[:, 1, :], ALU.add, ["d_pr"], ["d_lc"])
        cx.act(lc[:, 2:4], lc[:, 0:2], AF.Exp, ["d_lc"], ["d_lc"])
        cx.tt("dve", lc[:, 4:5], lc[:, 3:4], lc[:, 2:3], ALU.subtract, ["d_lc"], ["d_lc"])
        cx.ts("dve", lc[:, 4:5], lc[:, 4:5], -lam_init, None, ALU.add, None, ["d_lc"], ["d_lc"])
        cx.ts("dve", nwc[:], nwc[:], 1.0 - lam_init, None, ALU.mult, None, ["d_nwc"], ["d_nwc"])

        qb = cx.sb(st, "d_q", [128, T], BF16)
        kb = cx.sb(st, "d_k", [128, T], BF16)
        xb = cx.sb(st, "d_x", [128, T], BF16)
        vt = cx.sb(st, "d_v", [128, T // 128, 128], BF16)
        pbuf = [cx.sb(st, f"d_p{i}", [128, 512], BF16) for i in range(4)]
        accL = [cx.sb(st, f"d_acc{i}", [128, 512], F32) for i in range(2)]
        accS = cx.sb(st, "d_accS", [128, 512], BF16)
        osb = [cx.sb(st, f"d_os{i}", [128, 512], F32) for i in range(2)]
        rl = cx.sb(st, "d_rl", [128, 512], F32)
        od = cx.sb(st, "d_od", [128, 512], F32)
        sq = cx.sb(st, "d_sq", [128, 512], BF16)
        yst = [cx.sb(st, f"d_y{i}", [128, 512], BF16) for i in range(2)]
        pS = [cx.ps(st, f"d_pS{i}", [128, 512]) for i in range(3)]
        pO = cx.ps(st, "d_pO", [128, 512])
        pL = cx.ps(st, "d_pL", [128, 512])
        pN = cx.ps(st, "d_pN", [128, 512])
        NK = T // 128
        ycount = 0
        for ht in range(4):
            for (dst, dk_, t_main, t_sw) in ((qb, "d_q", FM_DQ + ht, FM_DQS + ht), (kb, "d_k", FM_DK + ht, FM_DKS + ht)):
                cx.dma("sp", dk_, dst[:], sc["FM"][t_main, :, OFF:OFF + T], [], [dk_])
                cx.dma("sp", "d_x", xb[:], sc["FM"][t_sw, :, OFF:OFF + T], [], ["d_x"])
                cx.tt("dve", dst[:], dst[:], Ct[:], ALU.mult, [dk_, "d_C"], [dk_])
                cx.tt("pool", xb[:], xb[:], St[:], ALU.mult, ["d_x", "d_S"], ["d_x"])
                cx.tt("dve", dst[:], dst[:], xb[:], ALU.add, [dk_, "d_x"], [dk_])
            cx.dma("sp", "d_v", vt[:], sc["TM"][:, TM_DV + ht * 128:TM_DV + (ht + 1) * 128].rearrange("(n p) c -> p n c", p=128),
                   [], ["d_v"])
            for qc in range(T // 512):
                qs_ = slice(qc * 512, (qc + 1) * 512)
                for s in range(2):
                    rs_ = slice(s * 64, (s + 1) * 64)

                    def smm(kt, i):
                        cx.mm(pS[i % 3][:], kb[rs_, kt * 128:(kt + 1) * 128], qb[rs_, qs_], True, True,
                              ["d_k", "d_q"], [f"d_pS{i % 3}"])
                    base = (qc * 2 + s) * NK
                    smm(0, base)
                    for kt in range(NK):
                        i = base + kt
                        if kt + 1 < NK:
                            smm(kt + 1, i + 1)
                        pb_ = pbuf[i % 4]
                        pk = f"d_p{i % 4}"
                        cx.act(pb_[:], pS[i % 3][:], AF.Exp, [f"d_pS{i % 3}"], [pk], scale=0.125)
                        cx.mm(pO[:], vt[:, kt, :], pb_[:], kt == 0, kt == NK - 1, ["d_v", pk], ["d_pO"])
                        ae = "dve" if kt % 2 == 0 else "pool"
                        a_, ak_ = accL[kt % 2], f"d_acc{kt % 2}"
                        if kt < 2:
                            cx.cp(ae, a_[:], pb_[:], [pk], [ak_])
                        else:
                            cx.tt(ae, a_[:], a_[:], pb_[:], ALU.add, [ak_, pk], [ak_])
                    cx.tt("dve", accS[:], accL[0][:], accL[1][:], ALU.add, ["d_acc0", "d_acc1"], ["d_accS"])
                    cx.mm(pL[:], g.onesb[:], accS[:], True, True, ["onesb", "d_accS"], ["d_pL"])
                    cx.recip(rl[:], pL[:], ["d_pL"], ["d_rl"])
                    cx.tt("dve", osb[s][:], pO[:], rl[:], ALU.mult, ["d_pO", "d_rl"], [f"d_os{s}"])
                cx.stt("dve", od[:], osb[1][:], lc[:, 4:5], osb[0][:], ALU.mult, ALU.add, ["d_os0", "d_os1", "d_lc"], ["d_od"])
                cx.act(sq[:], od[:], AF.Square, ["d_od"], ["d_sq"])
                cx.mm(pN[:], g.onesb[:], sq[:], True, True, ["onesb", "d_sq"], ["d_pN"])
                cx.act(rl[:], pN[:], AF.Sqrt, ["d_pN"], ["d_rl"], bias=EPS, scale=1.0 / 128)
                cx.recip(rl[:], rl[:], ["d_rl"], ["d_rl"])
                yb = yst[ycount % 2]
                yk = f"d_y{ycount % 2}"
                ycount += 1
                cx.stt("dve", yb[:], od[:], nwc[:, 0:1], rl[:], ALU.mult, ALU.mult, ["d_od", "d_nwc", "d_rl"], [yk])
                cx.dma("sp", yk, sc["YT"][1, ht, :, qs_], yb[:], [yk], [])
        cx.phase_end()


class Rot:
    def __init__(self, cx, st, name, shape, dt, n, psum=False):
        mk = cx.ps if psum else cx.sb
        self.t = [mk(st, f"{name}{i}", shape, dt) for i in range(n)]
        self.k = [f"{name}#{i}" for i in range(n)]
        self.i = -1

    def next(self):
        self.i += 1
        j = self.i % len(self.t)
        return self.t[j], self.k[j]

    def cur(self):
        j = self.i % len(self.t)
        return self.t[j], self.k[j]


def chunk_masks():
    j = np.arange(128)[:, None]
    i = np.arange(128)[None, :]
    same = (j // 64) == (i // 64)
    mf = (same & (j <= i)).astype(np.float32)
    mb = (same & (j >= i)).astype(np.float32)
    reset = np.ones((128, 512), np.float32)
    reset[:, ::64] = 0.0
    return mf, mb, reset


def phase_gla(cx, g, dr, layer, sc):
    nc = cx.nc
    NB = T // 512
    with ExitStack() as st:
        gu = cx.sb(st, "a_gu", [64, 256], BF16)
        nb = cx.sb(st, "a_nb", [128, 4], F32)
        nwc = cx.sb(st, "a_nwc", [128, 1], F32)
        mk = [cx.sb(st, f"a_mk{i}", [128, 128], F32) for i in range(2)]
        reset = cx.sb(st, "a_reset", [128, 512], F32)
        S = cx.sb(st, "a_S", [128, 128], BF16)
        cx.dma("pool", "a_c0", gu[0:16, :], dr["gla_gate_up"][layer, 0], [], ["a_gu"])
        cx.dma("pool", "a_c0", gu[32:48, :], dr["gla_gate_up"][layer, 1], [], ["a_gu"])
        for d in range(2):
            for rt in range(2):
                cx.dma("sp", "a_c1", nb[:, d * 2 + rt:d * 2 + rt + 1],
                       dr["gla_gate_bias"][layer, d, rt * 128:(rt + 1) * 128].rearrange("(p o) -> p o", o=1), [], ["a_nb"])
        cx.ts("dve", nb[:], nb[:], -1.0, None, ALU.mult, None, ["a_nb"], ["a_nb"])
        cx.dma("sp", "a_c2", nwc[:], dr["gla_norm"][layer].rearrange("(p o) -> p o", o=1), [], ["a_nwc"])
        cx.dma("sp", "a_c3", mk[0][:], dr["mask_f"], [], ["a_mk0"])
        cx.dma("sp", "a_c3", mk[1][:], dr["mask_b"], [], ["a_mk1"])
        cx.dma("sp", "a_c4", reset[:], dr["reset64"], [], ["a_reset"])

        gw = Rot(cx, st, "a_gw", [64, 512], BF16, 2)
        qk = Rot(cx, st, "a_qk", [128, 2, 512], BF16, 2)
        vtm = Rot(cx, st, "a_v", [128, 4, 256], BF16, 2)
        f32r = {n: Rot(cx, st, f"a_{n}", [128, 512], F32, 2) for n in ("e", "sp", "Bp", "Dm", "E3")}
        E12 = Rot(cx, st, "a_E12", [128, 512], F32, 2)
        tot = Rot(cx, st, "a_tot", [128, 8, 1], F32, 2)
        bfr = {n: Rot(cx, st, f"a_{n}", [128, 512], BF16, 2) for n in ("qt", "kt", "qd", "ke")}
        ketm = Rot(cx, st, "a_ketm", [128, 128], BF16, 2)
        Am = Rot(cx, st, "a_Am", [128, 128], BF16, 3)
        ofw = Rot(cx, st, "a_ofw", [128, 512], F32, 4)
        grb = Rot(cx, st, "a_gr", [128, 512], BF16, 4)
        osb = Rot(cx, st, "a_o", [128, 512], F32, 2)
        sq = Rot(cx, st, "a_sq", [128, 512], BF16, 2)
        rl = Rot(cx, st, "a_rl", [128, 512], F32, 2)
        yb = Rot(cx, st, "a_y", [128, 512], BF16, 2)
        pZ = cx.ps(st, "a_pZ", [128, 512])
        pN = pZ
        pO = Rot(cx, st, "a_pO", [128, 512], F32, 3, psum=True)
        pA = Rot(cx, st, "a_pA", [128, 128], F32, 2, psum=True)
        pKV = cx.ps(st, "a_pKV", [128, 128])
        pT = cx.ps(st, "a_pT", [128, 2, 128], BF16)

        for d in range(2):
            mid, last = (31, 63) if d == 0 else (32, 0)
            blocks = list(range(NB)) if d == 0 else list(range(NB - 1, -1, -1))
            for rt in range(2):
                cx.memset("dve", S[:], 0.0, ["a_S"])
                prep = {}

                def gates(b):
                    c0 = b * 512
                    cols = slice(OFF + c0, OFF + c0 + 512)
                    gwt, gwk = gw.next()
                    cx.dma("sp", gwk, gwt[:], sc["FM"][FM_GW, 0:64, cols], [], [gwk])
                    qkt, qkk = qk.next()
                    cx.dma("sp", qkk, qkt[:, 0, :], sc["FM"][FM_GQ + rt, :, cols], [], [qkk])
                    cx.dma("sp", qkk, qkt[:, 1, :], sc["FM"][FM_GK + rt, :, cols], [], [qkk])
                    vt, vk = vtm.next()
                    cx.dma("sp", vk, vt[:], sc["TM"][c0:c0 + 512, TM_GV + rt * 256:TM_GV + (rt + 1) * 256]
                           .rearrange("(n p) c -> p n c", p=128), [], [vk])
                    r0 = d * 32
                    cx.mm(pZ[:], gu[r0:r0 + 16, rt * 128:(rt + 1) * 128], gwt[r0:r0 + 16, :], True, True, ["a_gu", gwk], ["a_pZ"])
                    e, ek = f32r["e"].next()
                    cx.act(e[:], pZ[:], AF.Exp, ["a_pZ", "a_nb"], [ek], bias=nb[:, d * 2 + rt:d * 2 + rt + 1], scale=-1.0)
                    sp, spk = f32r["sp"].next()
                    cx.act(sp[:], e[:], AF.Ln, [ek], [spk], bias=1.0)
                    Bp, Bk = f32r["Bp"].next()
                    nc_ = nc
                    cx.S.op("dve", (lambda o=Bp[:], r_=reset[:], s_=sp[:]: nc_.vector.tensor_tensor_scan(o, r_, s_, 0.0, ALU.mult, ALU.add)),
                            ["a_reset", spk], [Bk])
                    B3 = Bp[:].rearrange("p (c j) -> p c j", j=64)
                    if d == 1:
                        Dm_, Dk_ = f32r["Dm"].next()
                        cx.tt("pool", Dm_[:], sp[:], Bp[:], ALU.subtract, [spk, Bk], [Dk_])
                        tot_, totk_ = tot.next()
                        cx.cp("dve", tot_[:], B3[:, :, 63:64], [Bk], [totk_])
                        cx.tt("dve", B3, Dm_[:].rearrange("p (c j) -> p c j", j=64), tot_[:].to_broadcast([128, 8, 64]),
                              ALU.add, [Dk_, totk_], [Bk])
                    Dm, Dk = f32r["Dm"].next()
                    cx.tt("dve", Dm[:].rearrange("p (c j) -> p c j", j=64), B3, B3[:, :, mid:mid + 1].to_broadcast([128, 8, 64]),
                          ALU.subtract, [Bk], [Dk])
                    E3, E3k = f32r["E3"].next()
                    cx.act(E3[:], Bp[:], AF.Exp, [Bk], [E3k], scale=-1.0 / 16)
                    qt, qtk = bfr["qt"].next()
                    kt_, ktk = bfr["kt"].next()
                    qd, qdk = bfr["qd"].next()
                    ke, kek = bfr["ke"].next()
                    Ea, Eak = E12.next()
                    cx.act(Ea[:], Dm[:], AF.Exp, [Dk], [Eak], scale=-1.0 / 16)
                    cx.tt("pool", qt[:], qkt[:, 0, :], Ea[:], ALU.mult, [qkk, Eak], [qtk])
                    Eb, Ebk = E12.next()
                    cx.act(Eb[:], Dm[:], AF.Exp, [Dk], [Ebk], scale=1.0 / 16)
                    cx.tt("pool", kt_[:], qkt[:, 1, :], Eb[:], ALU.mult, [qkk, Ebk], [ktk])
                    cx.tt("dve", qd[:], qkt[:, 0, :], E3[:], ALU.mult, [qkk, E3k], [qdk])
                    Dl, Dlk = f32r["Dm"].next()
                    cx.tt("dve", Dl[:].rearrange("p (c j) -> p c j", j=64), B3, B3[:, :, last:last + 1].to_broadcast([128, 8, 64]),
                          ALU.subtract, [Bk], [Dlk])
                    Ec, Eck = E12.next()
                    cx.act(Ec[:], Dl[:], AF.Exp, [Dlk], [Eck], scale=1.0 / 16)
                    cx.tt("pool", ke[:], qkt[:, 1, :], Ec[:], ALU.mult, [qkk, Eck], [kek])
                    ex = {}
                    if d == 1:
                        ex["ofw"] = []
                        ex["gr"] = []
                        for hh in range(2):
                            h = rt * 2 + hh
                            o_, ok_ = ofw.next()
                            cx.dma("sp", ok_, o_[:], sc["OFW"][h, :, c0:c0 + 512], [], [ok_])
                            ex["ofw"].append((o_, ok_))
                            g_, gk_ = grb.next()
                            cx.dma("sp", gk_, g_[:], sc["FM"][FM_GR + h, :, cols], [], [gk_])
                            ex["gr"].append((g_, gk_))
                    prep[b] = dict(vt=vt, vk=vk, E3=E3, E3k=E3k, qt=qt, qtk=qtk, kt=kt_, ktk=ktk, qd=qd, qdk=qdk, ke=ke, kek=kek, **ex)

                def tiles(b):
                    P = prep.pop(b)
                    c0 = b * 512
                    po = []
                    for hh in range(2):
                        po.append(pO.next())
                    torder = range(4) if d == 0 else range(3, -1, -1)
                    for tt_ in torder:
                        ts_ = slice(tt_ * 128, (tt_ + 1) * 128)
                        kb_, kbk = ketm.next()
                        tb = ketm.i % 2
                        cx.tr(pT[:, tb, :], P["ke"][:, ts_], g.identb[:], [P["kek"], "identb"], ["a_pT"])
                        cx.cp("act", kb_[:], pT[:, tb, :], ["a_pT"], [kbk])
                        for hh in range(2):
                            rows = slice(hh * 64, (hh + 1) * 64)
                            am, amk = Am.next()
                            pa_, pak_ = pA.next()
                            cx.mm(pa_[:], P["kt"][rows, ts_], P["qt"][rows, ts_], True, True, [P["ktk"], P["qtk"]], [pak_])
                            cx.tt("dve", am[:], pa_[:], mk[d][:], ALU.mult, [pak_, f"a_mk{d}"], [amk])
                            pot, pok = po[hh]
                            cx.mm(pot[:, ts_], P["vt"][:, tt_, hh * 128:(hh + 1) * 128], am[:], True, False, [P["vk"], amk], [pok])
                            corder = (0, 1) if d == 0 else (1, 0)
                            for ci, c in enumerate(corder):
                                cs = slice(tt_ * 128 + c * 64, tt_ * 128 + (c + 1) * 64)
                                cx.mm(pot[:, cs], S[rows, :], P["qd"][rows, cs], False, ci == 1, ["a_S", P["qdk"]], [pok])
                                crow = slice(c * 64, (c + 1) * 64)
                                cx.mm(pKV[rows, :], kb_[crow, hh * 64:(hh + 1) * 64], P["vt"][crow, tt_, hh * 128:(hh + 1) * 128],
                                      True, True, [kbk, P["vk"]], ["a_pKV"])
                                dcol = tt_ * 128 + c * 64 + last
                                cx.stt("dve", S[rows, :], S[rows, :], P["E3"][rows, dcol:dcol + 1], pKV[rows, :], ALU.mult, ALU.add,
                                       ["a_S", P["E3k"], "a_pKV"], ["a_S"])
                    for hh in range(2):
                        h = rt * 2 + hh
                        pot, pok = po[hh]
                        o_, ok_ = osb.next()
                        if d == 0:
                            cx.cp("act", o_[:], pot[:], [pok], [ok_])
                            cx.dma("sp", ok_, sc["OFW"][h, :, c0:c0 + 512], o_[:], [ok_], [])
                        else:
                            f_, fk_ = P["ofw"][hh]
                            cx.tt("dve", o_[:], pot[:], f_[:], ALU.add, [pok, fk_], [ok_])
                            s_, sk_ = sq.next()
                            cx.act(s_[:], o_[:], AF.Square, [ok_], [sk_])
                            cx.mm(pN[:], g.onesb[:], s_[:], True, True, ["onesb", sk_], ["a_pZ"])
                            r_, rk_ = rl.next()
                            cx.act(r_[:], pN[:], AF.Sqrt, ["a_pZ"], [rk_], bias=EPS, scale=1.0 / 128)
                            cx.recip(r_[:], r_[:], [rk_], [rk_])
                            cx.stt("dve", o_[:], o_[:], nwc[:, 0:1], r_[:], ALU.mult, ALU.mult, [ok_, "a_nwc", rk_], [ok_])
                            y_, yk_ = yb.next()
                            g_, gk_ = P["gr"][hh]
                            cx.tt("pool", y_[:], o_[:], g_[:], ALU.mult, [ok_, gk_], [yk_])
                            cx.dma("sp", yk_, sc["YT"][0, h, :, c0:c0 + 512], y_[:], [yk_], [])

                gates(blocks[0])
                for bi, b in enumerate(blocks):
                    if bi + 1 < len(blocks):
                        gates(blocks[bi + 1])
                    tiles(b)
            if d == 0:
                cx.S.barrier()
        cx.phase_end()


def gdn_masks():
    j = np.arange(128)[:, None]
    i = np.arange(128)[None, :]
    same = (j // 64) == (i // 64)
    sm_f = (same & (j < i)).astype(np.float32)
    sm_b = (same & (j > i)).astype(np.float32)
    m_same = same.astype(np.float32)
    m_c0 = np.broadcast_to((j < 64), (128, 128)).astype(np.float32)
    m_c1 = np.broadcast_to((j >= 64), (128, 128)).astype(np.float32)
    return sm_f, sm_b, m_same, np.ascontiguousarray(m_c0), np.ascontiguousarray(m_c1)


GDN_STAGES = 4
GDN_SB_STEPS = 10 ** 6
GDN_AUX = "dve"


def phase_gdn(cx, g, dr, layer, sc):
    nc = cx.nc
    NT = T // 128
    HALF = NT // 2
    with ExitStack() as st0:
        msk = {}
        for nm in ("mask_f", "mask_b", "sm_f", "sm_b", "m_same", "m_c0", "m_c1"):
            msk[nm] = cx.sb(st0, f"n_{nm}", [128, 128], F32)
            cx.dma("sp", f"n_{nm}", msk[nm][:], dr[nm], [], [f"n_{nm}"])
        GLs = cx.sb(st0, "n_GL", [128, NT, 16], F32)
        gt = {nm: cx.sb(st0, f"n_{nm}", [128, NT, 8], F32) for nm in
              ("g", "beta", "Bc", "eB", "eEnd", "dec0", "dec1", "nbeta", "bEB", "tmp")}
        dtb = cx.sb(st0, "n_dtb", [128, 8], F32)
        nA = cx.sb(st0, "n_nA", [128, 8], F32)
        nwc = cx.sb(st0, "n_nwc", [128, 1], F32)
        with ExitStack() as st:
            pg = cx.ps(st, "n_pg", [128, NT, 4])
            cx.dma("sp", "n_GL", GLs[:], sc["GL"].rearrange("(n p) c -> p n c", p=128), [], ["n_GL"])
            cx.dma("sp", "n_c0", dtb[:], dr["gdn_dt_bias"][layer:layer + 1].rearrange("o a b -> o (a b)")[0].partition_broadcast(128), [], ["n_dtb"])
            cx.dma("sp", "n_c1", nA[:], dr["gdn_A_log"][layer:layer + 1].rearrange("o a b -> o (a b)")[0].partition_broadcast(128), [], ["n_nA"])
            cx.dma("sp", "n_c2", nwc[:], dr["gdn_norm"][layer].rearrange("(p o) -> p o", o=1), [], ["n_nwc"])
            cx.act(nA[:], nA[:], AF.Exp, ["n_nA"], ["n_nA"])
            cx.ts("dve", nA[:], nA[:], -1.0, None, ALU.mult, None, ["n_nA"], ["n_nA"])
            bc = lambda t: t[:].unsqueeze(1).to_broadcast([128, NT, 8])
            cx.tt("dve", gt["tmp"][:], GLs[:, :, 0:8], bc(dtb), ALU.add, ["n_GL", "n_dtb"], ["n_tmp"])
            cx.act(gt["tmp"][:], gt["tmp"][:], AF.Exp, ["n_tmp"], ["n_tmp"])
            cx.act(gt["tmp"][:], gt["tmp"][:], AF.Ln, ["n_tmp"], ["n_tmp"], bias=1.0)
            cx.tt("dve", gt["g"][:], gt["tmp"][:], bc(nA), ALU.mult, ["n_tmp", "n_nA"], ["n_g"])
            cx.act(gt["beta"][:], GLs[:, :, 8:16], AF.Sigmoid, ["n_GL"], ["n_beta"])
            cx.ts("dve", gt["nbeta"][:], gt["beta"][:], -1.0, None, ALU.mult, None, ["n_beta"], ["n_nbeta"])
            for d in range(2):
                cs = slice(d * 4, (d + 1) * 4)
                um = msk["mask_f"] if d == 0 else msk["mask_b"]
                for (mm_, dst) in ((um, "Bc"), (msk["m_same"], "eEnd"), (msk["m_c0"], "dec0"), (msk["m_c1"], "dec1")):
                    cx.mm(pg[:], mm_[:], gt["g"][:, :, cs], True, True, [f"n_{'mask_f' if d == 0 else 'mask_b'}", "n_m_same", "n_m_c0", "n_m_c1", "n_g"], ["n_pg"])
                    cx.cp("dve", gt[dst][:, :, cs], pg[:], ["n_pg"], [f"n_{dst}"])
            cx.tt("dve", gt["eEnd"][:], gt["eEnd"][:], gt["Bc"][:], ALU.subtract, ["n_eEnd", "n_Bc"], ["n_eEnd"])
            cx.act(gt["eEnd"][:], gt["eEnd"][:], AF.Exp, ["n_eEnd"], ["n_eEnd"])
            cx.act(gt["eB"][:], gt["Bc"][:], AF.Exp, ["n_Bc"], ["n_eB"])
            cx.act(gt["dec0"][:], gt["dec0"][:], AF.Exp, ["n_dec0"], ["n_dec0"])
            cx.act(gt["dec1"][:], gt["dec1"][:], AF.Exp, ["n_dec1"], ["n_dec1"])
            cx.tt("dve", gt["bEB"][:], gt["beta"][:], gt["eB"][:], ALU.mult, ["n_beta", "n_eB"], ["n_bEB"])
            cx.S.barrier()
            cx.S.emit()
        if GDN_STAGES < 2:
            return

        with ExitStack() as st:
            cw = cx.sb(st, "n_cw", [4, 1536], F32)
            cwT = cx.sb(st, "n_cwT", [128, 12, 4], F32)
            dw = cx.sb(st, "n_dw", [128, 12, 4, 128], BF16)
            xin = Rot(cx, st, "n_xin", [128, TP], BF16, 2)
            sS = Rot(cx, st, "n_s", [128, 512], F32, 2)
            sQ = Rot(cx, st, "n_sq", [128, 512], BF16, 2)
            rR = Rot(cx, st, "n_r", [128, 512], F32, 2)
            stg = Rot(cx, st, "n_stg", [128, 512], BF16, 3)
            tms = Rot(cx, st, "n_tms", [128, 4, 128], BF16, 3)
            pc = Rot(cx, st, "n_pc", [128, 512], F32, 2, psum=True)
            pn = Rot(cx, st, "n_pn", [128, 512], F32, 2, psum=True)
            pt = Rot(cx, st, "n_pt", [128, 4, 128], F32, 2, psum=True)
            pw_ = cx.ps(st, "n_pw", [128, 12, 4])
            cx.dma("sp", "n_cw", cw[:], dr["gdn_conv"][layer], [], ["n_cw"])
            for ct in range(12):
                cx.tr(pw_[:, ct, :], cw[0:4, ct * 128:(ct + 1) * 128], g.ident[0:4, 0:4], ["n_cw", "ident"], ["n_pw"])
            cx.cp("dve", cwT[:], pw_[:], ["n_pw"], ["n_cwT"])
            for ct in range(12):
                for w in range(4):
                    cx.ts(GDN_AUX if (ct + w) % 2 else "dve", dw[:, ct, w, :], g.identb[:], cwT[:, ct, w:w + 1], None, ALU.mult, None,
                          ["identb", "n_cwT"], ["n_dw"])
            for h in range(4):
                for kind in range(3):
                    ct = kind * 4 + h
                    xt, xk = xin.next()
                    cx.dma("sp", xk, xt[:], sc["FM"][FM_NC + ct], [], [xk])
                    if kind < 2:
                        dst = sc["GQT"] if kind == 0 else sc["GKT"]
                        scale = (128 ** -0.5) if kind == 0 else 1.0
                        for ch in range(T // 512):
                            t0 = ch * 512
                            p_, pk_ = pc.next()
                            for w in range(4):
                                cx.mm(p_[:], dw[:, ct, w, :], xt[:, OFF - 1 + w + t0:OFF - 1 + w + t0 + 512], w == 0, w == 3, ["n_dw", xk], [pk_])
                            s_, sk_ = sS.next()
                            cx.act(s_[:], p_[:], AF.Silu, [pk_], [sk_])
                            q_, qk_ = sQ.next()
                            cx.tt(GDN_AUX, q_[:], s_[:], s_[:], ALU.mult, [sk_], [qk_])
                            n_, nk_ = pn.next()
                            cx.mm(n_[:], g.onesb[:], q_[:], True, True, ["onesb", qk_], [nk_])
                            r_, rk_ = rR.next()
                            cx.act(r_[:], n_[:], AF.Sqrt, [nk_], [rk_], bias=EPS, scale=1.0)
                            cx.recip(r_[:], r_[:], [rk_], [rk_])
                            o_, ok_ = stg.next()
                            cx.stt("dve", o_[:], s_[:], scale, r_[:], ALU.mult, ALU.mult, [sk_, rk_], [ok_])
                            cx.dma("sp", ok_, dst[h, :, t0:t0 + 512], o_[:], [ok_], [])
                            if kind == 1:
                                t_, tk_ = pt.next()
                                for q in range(4):
                                    cx.mm(t_[:, q, :], o_[:, q * 128:(q + 1) * 128], g.identb[:], True, True, [ok_, "identb"], [tk_])
                                m_, mk_ = tms.next()
                                cx.cp("act", m_[:], t_[:], [tk_], [mk_])
                                cx.dma("sp", mk_, sc["GKM"][h, t0:t0 + 512, :].rearrange("(n p) c -> p n c", p=128), m_[:], [mk_], [])
                    else:
                        for ch in range(T // 512):
                            t0 = ch * 512
                            t_, tk_ = pt.next()
                            for q in range(4):
                                for w in range(4):
                                    c0 = OFF - 1 + w + t0 + q * 128
                                    cx.mm(t_[:, q, :], xt[:, c0:c0 + 128], dw[:, ct, w, :], w == 0, w == 3, [xk, "n_dw"], [tk_])
                            m_, mk_ = tms.next()
                            cx.act(m_[:], t_[:], AF.Silu, [tk_], [mk_])
                            cx.dma("sp", mk_, sc["GVM"][h, t0:t0 + 512, :].rearrange("(n p) c -> p n c", p=128), m_[:], [mk_], [])
            cx.S.barrier()
            cx.S.emit()
        if GDN_STAGES < 3:
            return

        NI = min(8, NT)
        with ExitStack() as st:
            bank = [cx.ps(st, f"n_bk{i}", [128, 128]) for i in range(NI)]
            bkk = [f"n_bk{i}" for i in range(NI)]
            qT8 = Rot(cx, st, "n_q8", [128, NI * 128], BF16, 2)
            kT8 = Rot(cx, st, "n_k8", [128, NI * 128], BF16, 2)
            km8 = Rot(cx, st, "n_km8", [128, NI, 128], BF16, 2)
            vm8 = Rot(cx, st, "n_vm8", [128, NI, 128], BF16, 2)
            outp = Rot(cx, st, "n_out", [128, NI, 4, 128], BF16, 2)

            def ibuf(nm, dt):
                ts_ = [cx.sb(st, f"n_{nm}{i}", [128, 128], dt) for i in range(NI)]
                return ts_, [f"n_{nm}#{i}" for i in range(NI)]
            Lg, Lgk = ibuf("Lg", F32)
            Ge, Gek = ibuf("Ge", F32)
            GM, GMk = ibuf("GM", F32)
            GI, GIk = ibuf("GI", F32)
            Tm = [ibuf(f"Tm{j}", BF16) for j in range(2)]
            Ym = [ibuf(f"Ym{j}", BF16) for j in range(2)]
            Xm, Xk = ibuf("X", BF16)
            at, atk = ibuf("at", BF16)
            bv, bvk = ibuf("bv", BF16)
            rw, rwk = ibuf("rw", BF16)
            for h in range(4):
                for d in range(2):
                    col = d * 4 + h
                    LM, LMk = (msk["mask_f"], "n_mask_f") if d == 0 else (msk["mask_b"], "n_mask_b")
                    SM, SMk = (msk["sm_b"], "n_sm_b") if d == 0 else (msk["sm_f"], "n_sm_f")
                    for n0 in range(0, NT, NI):
                        q8, q8k = qT8.next()
                        k8, k8k = kT8.next()
                        km, kmk = km8.next()
                        vm, vmk = vm8.next()
                        op_, opk = outp.next()
                        tsl = slice(n0 * 128, (n0 + NI) * 128)
                        cx.dma("sp", q8k, q8[:], sc["GQT"][h, :, tsl], [], [q8k])
                        cx.dma("sp", k8k, k8[:], sc["GKT"][h, :, tsl], [], [k8k])
                        cx.dma("sp", kmk, km[:], sc["GKM"][h, tsl, :].rearrange("(n p) c -> p n c", p=128), [], [kmk])
                        cx.dma("sp", vmk, vm[:], sc["GVM"][h, tsl, :].rearrange("(n p) c -> p n c", p=128), [], [vmk])
                        R_ = range(NI)
                        tl = lambda i: slice(i * 128, (i + 1) * 128)
                        for i in R_:
                            cx.ts("dve", Lg[i][:], LM[:], gt["g"][:, n0 + i, col:col + 1], None, ALU.mult, None, [LMk, "n_g"], [Lgk[i]])
                        for i in R_:
                            cx.mm(bank[i][:], Lg[i][:], SM[:], True, True, [Lgk[i], SMk], [bkk[i]])
                        for i in R_:
                            cx.act(Ge[i][:], bank[i][:], AF.Exp, [bkk[i]], [Gek[i]])
                        for i in R_:
                            cx.tt(GDN_AUX, GM[i][:], Ge[i][:], SM[:], ALU.mult, [Gek[i], SMk], [GMk[i]])
                        for i in R_:
                            cx.tt(GDN_AUX, GI[i][:], GM[i][:], g.ident[:], ALU.add, [GMk[i], "ident"], [GIk[i]])
                        for i in R_:
                            cx.mm(bank[i][:], k8[:, tl(i)], k8[:, tl(i)], True, True, [k8k], [bkk[i]])
                        T0, T0k = Tm[0]
                        for i in R_:
                            cx.stt("dve", T0[i][:], bank[i][:], gt["nbeta"][:, n0 + i, col:col + 1], GM[i][:], ALU.mult, ALU.mult,
                                   [bkk[i], "n_nbeta", GMk[i]], [T0k[i]])
                        for i in R_:
                            cx.mm(bank[i][:], q8[:, tl(i)], k8[:, tl(i)], True, True, [q8k, k8k], [bkk[i]])
                        for i in R_:
                            cx.tt("dve", at[i][:], bank[i][:], GI[i][:], ALU.mult, [bkk[i], GIk[i]], [atk[i]])
                        for i in R_:
                            cx.mm(bank[i][:], at[i][:], g.identb[:], True, True, [atk[i], "identb"], [bkk[i]])
                        for i in R_:
                            cx.cp("act", op_[:, i, 2, :], bank[i][:], [bkk[i]], [opk])
                        Y0, Y0k = Ym[0]
                        for i in R_:
                            cx.mm(bank[i][:], T0[i][:], g.identb[:], True, True, [T0k[i], "identb"], [bkk[i]])
                        for i in R_:
                            cx.cp("act", Y0[i][:], bank[i][:], [bkk[i]], [Y0k[i]])
                        for i in R_:
                            cx.tt(GDN_AUX, Xm[i][:], Y0[i][:], g.identb[:], ALU.add, [Y0k[i], "identb"], [Xk[i]])
                        cur = 0
                        for it in range(5):
                            Tc, Tck = Tm[cur]
                            Yc, Yck = Ym[cur]
                            Tn, Tnk = Tm[1 - cur]
                            Yn, Ynk = Ym[1 - cur]
                            for i in R_:
                                cx.mm(bank[i][:], Yc[i][:], Tc[i][:], True, True, [Yck[i], Tck[i]], [bkk[i]])
                            for i in R_:
                                cx.cp("act", Tn[i][:], bank[i][:], [bkk[i]], [Tnk[i]])
                            if it < 4:
                                for i in R_:
                                    cx.mm(bank[i][:], Tc[i][:], Yc[i][:], True, True, [Yck[i], Tck[i]], [bkk[i]])
                                for i in R_:
                                    cx.cp("dve", Yn[i][:], bank[i][:], [bkk[i]], [Ynk[i]])
                            for i in R_:
                                cx.mm(bank[i][:], Tn[i][:], Xm[i][:], True, True, [Tnk[i], Xk[i]], [bkk[i]])
                            for i in R_:
                                cx.tt("dve", Xm[i][:], Xm[i][:], bank[i][:], ALU.add, [Xk[i], bkk[i]], [Xk[i]])
                            cur = 1 - cur
                        for i in R_:
                            cx.ts(GDN_AUX, bv[i][:], vm[:, i, :], gt["beta"][:, n0 + i, col:col + 1], None, ALU.mult, None, [vmk, "n_beta"], [bvk[i]])
                        for i in R_:
                            cx.ts(GDN_AUX, rw[i][:], km[:, i, :], gt["bEB"][:, n0 + i, col:col + 1], None, ALU.mult, None, [kmk, "n_bEB"], [rwk[i]])
                        for i in R_:
                            cx.ts(GDN_AUX, op_[:, i, 3, :], km[:, i, :], gt["eEnd"][:, n0 + i, col:col + 1], None, ALU.mult, None, [kmk, "n_eEnd"], [opk])
                        for i in R_:
                            cx.mm(bank[i][:], Xm[i][:], bv[i][:], True, True, [Xk[i], bvk[i]], [bkk[i]])
                        for i in R_:
                            cx.cp("act", op_[:, i, 0, :], bank[i][:], [bkk[i]], [opk])
                        for i in R_:
                            cx.mm(bank[i][:], rw[i][:], Xm[i][:], True, True, [rwk[i], Xk[i]], [bkk[i]])
                        for i in R_:
                            cx.cp("dve", op_[:, i, 1, :], bank[i][:], [bkk[i]], [opk])
                        cx.dma("sp", opk, sc["GA"][h, d, n0:n0 + NI].rearrange("n p m c -> p n m c"), op_[:], [opk], [])
            cx.S.barrier()
            cx.S.emit()
        if GDN_STAGES < 4:
            return

        with ExitStack() as st:
            chains = [(h, d) for h in range(4) for d in range(2)]
            bank = [cx.ps(st, f"n_sbk{i}", [128, 4, 128]) for i in range(8)]
            bkk = [f"n_sbk{i}" for i in range(8)]
            Sst = [cx.sb(st, f"n_S{i}", [128, 128], BF16) for i in range(8)]
            Sk = [f"n_S{i}" for i in range(8)]
            ga = [Rot(cx, st, f"n_ga{i}_", [128, 4, 128], BF16, 2) for i in range(8)]
            qt = [Rot(cx, st, f"n_qt{i}_", [128, 128], BF16, 2) for i in range(8)]
            vn = [cx.sb(st, f"n_vn{i}", [128, 128], BF16) for i in range(8)]
            vnk = [f"n_vn{i}" for i in range(8)]
            oi = [cx.sb(st, f"n_oi{i}", [128, 128], F32) for i in range(8)]
            oik = [f"n_oi{i}" for i in range(8)]
            oo = [Rot(cx, st, f"n_oo{i}_", [128, 128], F32, 2) for i in range(8)]
            par = [Rot(cx, st, f"n_par{i}_", [128, 128], F32, 2) for i in range(8)]
            nz = [Rot(cx, st, f"n_nz{i}_", [128, 128], BF16, 2) for i in range(8)]
            on = [cx.sb(st, f"n_on{i}", [128, 128], BF16) for i in range(8)]
            onk = [f"n_on{i}" for i in range(8)]
            sj = [cx.sb(st, f"n_sj{i}", [128, 128], BF16) for i in range(8)]
            ssq = [cx.sb(st, f"n_ss{i}", [128, 2], F32) for i in range(8)]
            ssk = [f"n_ss{i}" for i in range(8)]
            yy = [Rot(cx, st, f"n_yy{i}_", [128, 128], BF16, 2) for i in range(8)]
            for i in range(8):
                cx.memset("pool", Sst[i][:], 0.0, [Sk[i]])
            for s in range(min(NT, GDN_SB_STEPS)):
                second = s >= HALF
                cur = []
                for ci, (h, d) in enumerate(chains):
                    n = s if d == 0 else NT - 1 - s
                    par2 = s % 2
                    ga_, gak = ga[ci].next()
                    cx.dma("sp", f"n_ga_{par2}", ga_[:], sc["GA"][h, d, n], [], [gak])
                    q_, qk_ = qt[ci].next()
                    cx.dma("sp", f"n_qt_{par2}", q_[:], sc["GQT"][h, :, n * 128:(n + 1) * 128], [], [qk_])
                    ex = None
                    if second:
                        p_, pk_ = par[ci].next()
                        cx.dma("sp", f"n_par_{par2}", p_[:], sc["GO"][h, n * 128:(n + 1) * 128, :], [("GO", h, n)], [pk_])
                        z_, zk_ = nz[ci].next()
                        cx.dma("sp", f"n_nz_{par2}", z_[:], sc["FM"][FM_NZ + h, :, OFF + n * 128:OFF + (n + 1) * 128], [], [zk_])
                        ex = (p_, pk_, z_, zk_)
                    cur.append((h, d, n, ga_, gak, q_, qk_, ex))
                for step in range(2):
                    for ci, (h, d, n, ga_, gak, q_, qk_, ex) in enumerate(cur):
                        c = step if d == 0 else 1 - step
                        cr = slice(c * 64, (c + 1) * 64)
                        col = d * 4 + h
                        B = bank[ci]
                        cx.mm(B[cr, 0, :], ga_[:, 1, cr], Sst[ci][:], True, True, [gak, Sk[ci]], [bkk[ci]])
                        cx.mm(B[cr, 1, :], q_[:, cr], Sst[ci][:], True, True, [qk_, Sk[ci]], [bkk[ci]])
                    for ci, (h, d, n, ga_, gak, q_, qk_, ex) in enumerate(cur):
                        c = step if d == 0 else 1 - step
                        cr = slice(c * 64, (c + 1) * 64)
                        cx.tt("dve", vn[ci][cr, :], ga_[cr, 0, :], bank[ci][cr, 0, :], ALU.subtract, [gak, bkk[ci]], [vnk[ci]])
                    for ci, (h, d, n, ga_, gak, q_, qk_, ex) in enumerate(cur):
                        c = step if d == 0 else 1 - step
                        cr = slice(c * 64, (c + 1) * 64)
                        B = bank[ci]
                        cx.mm(B[cr, 2, :], ga_[cr, 2, cr], vn[ci][cr, :], True, True, [gak, vnk[ci]], [bkk[ci]])
                        cx.mm(B[:, 3, :], ga_[cr, 3, :], vn[ci][cr, :], True, True, [gak, vnk[ci]], [bkk[ci]])
                    for ci, (h, d, n, ga_, gak, q_, qk_, ex) in enumerate(cur):
                        c = step if d == 0 else 1 - step
                        col = d * 4 + h
                        dec = gt["dec0"] if c == 0 else gt["dec1"]
                        cx.stt("dve", Sst[ci][:], Sst[ci][:], dec[:, n, col:col + 1], bank[ci][:, 3, :], ALU.mult, ALU.add,
                               [Sk[ci], f"n_dec{c}", bkk[ci]], [Sk[ci]])
                for ci, (h, d, n, ga_, gak, q_, qk_, ex) in enumerate(cur):
                    col = d * 4 + h
                    cx.ts("dve", oi[ci][:], bank[ci][:, 1, :], gt["eB"][:, n, col:col + 1], None, ALU.mult, None, [bkk[ci], "n_eB"], [oik[ci]])
                for ci, (h, d, n, ga_, gak, q_, qk_, ex) in enumerate(cur):
                    o_, ok_ = oo[ci].next()
                    cx.tt("dve", o_[:], bank[ci][:, 2, :], oi[ci][:], ALU.add, [bkk[ci], oik[ci]], [ok_])
                    if not second:
                        cx.dma("sp", f"n_oo_{s % 2}", sc["GO"][h, n * 128:(n + 1) * 128, :], o_[:], [ok_], [("GO", h, n)])
                    else:
                        p_, pk_, z_, zk_ = ex
                        cx.tt(GDN_AUX, o_[:], o_[:], p_[:], ALU.add, [ok_, pk_], [ok_])
                        cx.act(sj[ci][:], o_[:], AF.Square, [ok_], [ssk[ci]], accum_out=ssq[ci][:, 0:1])
                        cx.rstd(ssq[ci][:, 1:2], ssq[ci][:, 0:1], 1.0 / 128, ssk[ci])
                        cx.ts("dve", on[ci][:], o_[:], ssq[ci][:, 1:2], None, ALU.mult, None, [ok_, ssk[ci]], [onk[ci]])
                        cx.mm(bank[ci][:, 0, :], on[ci][:], g.identb[:], True, True, [onk[ci], "identb"], [bkk[ci]])
                        y_, yk_ = yy[ci].next()
                        cx.stt("dve", y_[:], bank[ci][:, 0, :], nwc[:, 0:1], z_[:], ALU.mult, ALU.mult, [bkk[ci], "n_nwc", zk_], [yk_])
                        cx.dma("sp", f"n_yy_{s % 2}", sc["YT"][2, h, :, n * 128:(n + 1) * 128], y_[:], [yk_], [])
            cx.phase_end()


def phase_post(cx, g, dr, layer, sc, x_in, x_out):
    nc = cx.nc
    with ExitStack() as st:
        wb = cx.sb(st, "o_wb", [128, 3, 4, D], BF16)
        wo = cx.sb(st, "o_wo", [128, KT, D], BF16)
        for n in range(3):
            cx.dma("pool", "o_wb", wb[:, n], dr["w_branch"][layer, n].rearrange("(k p) d -> p k d", p=128), [], ["o_wb"])
        cx.dma("pool", "o_wo", wo[:], dr["w_out"][layer].rearrange("(k p) d -> p k d", p=128), [], ["o_wo"])
        yT = Rot(cx, st, "o_yT", [128, 12, 512], BF16, 2)
        gT = Rot(cx, st, "o_gT", [128, 24, 512], BF16, 2)
        mT = Rot(cx, st, "o_mT", [128, KT, 512], BF16, 2)
        tmp = Rot(cx, st, "o_tmp", [128, 512], F32, 4)
        acc = Rot(cx, st, "o_acc", [128, 512], F32, 2)
        pb = Rot(cx, st, "o_pb", [128, 512], F32, 4, psum=True)
        po = Rot(cx, st, "o_po", [128, 512], F32, 2, psum=True)
        xr = Rot(cx, st, "o_xr", [128, D], F32, 2)
        yo = Rot(cx, st, "o_yo", [128, D], F32, 2)
        fs = Rot(cx, st, "o_fs", [128, 2], F32, 2)
        for ch in range(T // 512):
            t0 = ch * 512
            y_, yk = yT.next()
            cx.dma("sp", yk, y_[:].rearrange("p (n k) t -> p n k t", n=3),
                   sc["YT"][:, :, :, t0:t0 + 512].rearrange("n k p t -> p n k t"), [], [yk])
            g_, gk = gT.next()
            cx.dma("sp", gk, g_[:], sc["FM"][FM_MG:FM_MG + 24, :, OFF + t0:OFF + t0 + 512].rearrange("a p t -> p a t"), [], [gk])
            m_, mk_ = mT.next()
            for dt_ in range(KT):
                a_, ak = acc.next()
                for n in range(3):
                    p_, pk = pb.next()
                    for k in range(4):
                        cx.mm(p_[:], wb[:, n, k, dt_ * 128:(dt_ + 1) * 128], y_[:, n * 4 + k, :], k == 0, k == 3, ["o_wb", yk], [pk])
                    gsl = g_[:, n * 8 + dt_, :]
                    if n == 0:
                        cx.tt("dve", a_[:], p_[:], gsl, ALU.mult, [pk, gk], [ak])
                    else:
                        t_, tk = tmp.next()
                        cx.tt("dve", t_[:], p_[:], gsl, ALU.mult, [pk, gk], [tk])
                        if n == 1:
                            cx.tt("pool", a_[:], a_[:], t_[:], ALU.add, [ak, tk], [ak])
                        else:
                            cx.tt("pool", m_[:, dt_, :], a_[:], t_[:], ALU.add, [ak, tk], [mk_])
            for s in range(4):
                rows = slice(t0 + s * 128, t0 + (s + 1) * 128)
                x_, xk = xr.next()
                cx.dma("sp", xk, x_[:], x_in[rows, :], [], [xk])
                o_, ok = yo.next()
                f_, fk = fs.next()
                ps_ = []
                for half in range(2):
                    p_, pk = po.next()
                    for k in range(KT):
                        cx.mm(p_[:], m_[:, k, s * 128:(s + 1) * 128], wo[:, k, half * 512:(half + 1) * 512], k == 0, k == KT - 1,
                              [mk_, "o_wo"], [pk])
                    ps_.append((p_, pk))
                    cx.cp("act", o_[:, half * 512:(half + 1) * 512], p_[:], [pk], [ok])
                cx.act(xr_junk(cx, st)[:], o_[:], AF.Square, [ok], [fk], accum_out=f_[:, 0:1])
                cx.rstd(f_[:, 1:2], f_[:, 0:1], 1.0 / D, fk)
                cx.stt("dve", o_[:], o_[:], f_[:, 1:2], g.mod[:, 2, :], ALU.mult, ALU.mult, [ok, fk, ("mod", 2)], [ok])
                cx.tt("pool", o_[:], o_[:], x_[:], ALU.add, [ok, xk], [ok])
                cx.dma("sp", ok, x_out[rows, :], o_[:], [ok], [])
        cx.phase_end()


_JUNK = {}


def xr_junk(cx, st):
    k = id(st)
    if k not in _JUNK:
        _JUNK.clear()
        _JUNK[k] = cx.sb(st, "junk", [128, D], BF16)
    return _JUNK[k]


N_CORES = 8
DEPTH = 2
PHASE_LIMIT = 99
PHASE_SKIP = ()


def build_program():
    nc = bass.Bass("TRN2", target_bir_lowering=False)
    dr = {}

    def inp(name, shape, dt=F32):
        dr[name] = nc.dram_tensor(name, list(shape), dt, kind="ExternalInput").ap()

    inp("x", [T, D]); inp("cT", [128, KT]); inp("positions", [1, T], I32)
    inp("adaln_w", [DEPTH, D, 6 * D]); inp("adaln_b", [DEPTH, 6 * D]); inp("norm_w", [DEPTH, 4, D])
    inp("w_fm", [DEPTH, D, NFMG * 512]); inp("w_tm", [DEPTH, D, NTM])
    inp("gla_gate_up", [DEPTH, 2, 16, 256]); inp("gla_gate_bias", [DEPTH, 2, 256]); inp("gla_norm", [DEPTH, 128])
    inp("diff_lambda", [DEPTH, 4, 64]); inp("diff_norm", [DEPTH, 128])
    inp("gdn_conv", [DEPTH, 4, 1536]); inp("gdn_A_log", [DEPTH, 2, 4]); inp("gdn_dt_bias", [DEPTH, 2, 4]); inp("gdn_norm", [DEPTH, 128])
    inp("w_branch", [DEPTH, 3, 512, D]); inp("w_out", [DEPTH, D, D])
    inp("ffn_w1", [1, D, D_FF]); inp("ffn_w3", [1, D, D_FF]); inp("ffn_w2", [1, D_FF, D])
    inp("router_w", [1, D, N_EXP]); inp("moe_w1", [1, N_EXP, D, D_EXP]); inp("moe_w3", [1, N_EXP, D, D_EXP]); inp("moe_w2", [1, N_EXP, D_EXP, D])
    for nm in ("ident", "mask_f", "mask_b", "sm_f", "sm_b", "m_same", "m_c0", "m_c1"):
        inp(nm, [128, 128])
    inp("reset64", [128, 512]); inp("rot_inv", [128, 1]); inp("rot_sgn", [128, 1])
    inp("tokidx", [128, (T // 2) // 128], I32)
    out = nc.dram_tensor("out", [T // 2, D], F32, kind="ExternalOutput").ap()

    sc = {}

    def scr(name, shape, dt):
        sc[name] = nc.dram_tensor(name, list(shape), dt, kind="Internal").ap()

    NT = T // 128
    PAGE_EL = 268435456 // 2
    arena = nc.dram_tensor("arenaA", [PAGE_EL], BF16, kind="Internal").ap()
    off = [0]

    def carve(name, shape, pattern, **kw):
        n = int(np.prod(shape))
        sc[name] = arena[off[0]:off[0] + n].rearrange(pattern, **kw)
        off[0] += n
        assert off[0] <= PAGE_EL

    carve("FM", [NFM, 128, TP], "(a p t) -> a p t", a=NFM, p=128)
    carve("TM", [T, NTM], "(t c) -> t c", c=NTM)
    carve("YT", [3, 4, 128, T], "(n k p t) -> n k p t", n=3, k=4, p=128)
    carve("GQT", [4, 128, T], "(h p t) -> h p t", h=4, p=128)
    carve("GKT", [4, 128, T], "(h p t) -> h p t", h=4, p=128)
    carve("GKM", [4, T, 128], "(h t c) -> h t c", h=4, c=128)
    carve("GVM", [4, T, 128], "(h t c) -> h t c", h=4, c=128)
    scr("GA", [4, 2, NT, 128, 4, 128], BF16)
    scr("GL", [T, 16], F32); scr("OFW", [4, 128, T], F32); scr("GO", [4, T, 128], F32)
    scr("XA", [T, D], F32); scr("XB", [T, D], F32)

    with ExitStack() as st:
        cx = Ctx(nc, st)
        g = setup_globals(cx, st, dr)
        nph = 0
        for layer in range(DEPTH):
            x_in = dr["x"] if layer == 0 else sc["XB"]
            x_out = sc["XB"] if layer == 0 else out
            if layer % 2 == 0:
                ex = [(dr["ffn_w1"][layer // 2], dr["ffn_w3"][layer // 2], dr["ffn_w2"][layer // 2])]
                rw = None
            else:
                ex = [(dr["moe_w1"][layer // 2, e], dr["moe_w3"][layer // 2, e], dr["moe_w2"][layer // 2, e]) for e in range(N_EXP)]
                rw = dr["router_w"][layer // 2]
            phases = [
                lambda: phase_mod(cx, g, dr, layer),
                lambda: phase_proj(cx, g, dr, x_in, layer, sc),
                lambda: phase_diff(cx, g, dr, layer, sc),
                lambda: phase_gla(cx, g, dr, layer, sc),
                lambda: phase_gdn(cx, g, dr, layer, sc),
                lambda: phase_post(cx, g, dr, layer, sc, x_in, sc["XA"]),
                (lambda: phase_ffn(cx, g, dr, sc["XA"], x_out, ex, rw)) if layer < DEPTH - 1 else
                (lambda: phase_ffn(cx, g, dr, sc["XA"], x_out, ex, rw, tok_range=(0, T // 2), tokidx=dr["tokidx"])),
            ]
            for ph in phases:
                if nph < PHASE_LIMIT and nph not in PHASE_SKIP:
                    ph()
                nph += 1
    return nc


def kernel(x, c, positions, adaln_w, adaln_b, norm_w, w_in, gla_gate_up, gla_gate_bias, gla_norm,
           diff_lambda, diff_norm, gdn_conv, gdn_A_log, gdn_dt_bias, gdn_norm, w_branch, w_out,
           ffn_w1, ffn_w3, ffn_w2, router_w, moe_w1, moe_w3, moe_w2):
    f = lambda a: np.ascontiguousarray(np.asarray(a, dtype=np.float32))
    x = f(x); c = f(c)
    positions = np.ascontiguousarray(np.asarray(positions).astype(np.int32))
    w_in = f(w_in)
    packed = [pack_w_in(w_in[l]) for l in range(DEPTH)]
    w_fm = np.stack([p[0] for p in packed])
    w_tm = np.stack([p[1] for p in packed])
    mf, mb, rs64 = chunk_masks()
    sm_f, sm_b, m_same, m_c0, m_c1 = gdn_masks()
    inv, sgn = rot_consts()
    shared = {
        "adaln_w": f(adaln_w), "adaln_b": f(adaln_b), "norm_w": f(norm_w), "w_fm": w_fm, "w_tm": w_tm,
        "gla_gate_up": f(gla_gate_up), "gla_gate_bias": f(gla_gate_bias), "gla_norm": f(gla_norm),
        "diff_lambda": f(diff_lambda), "diff_norm": f(diff_norm),
        "gdn_conv": f(gdn_conv), "gdn_A_log": f(gdn_A_log), "gdn_dt_bias": f(gdn_dt_bias), "gdn_norm": f(gdn_norm),
        "w_branch": f(w_branch), "w_out": f(w_out), "ffn_w1": f(ffn_w1), "ffn_w3": f(ffn_w3), "ffn_w2": f(ffn_w2),
        "router_w": f(router_w), "moe_w1": f(moe_w1), "moe_w3": f(moe_w3), "moe_w2": f(moe_w2),
        "ident": np.eye(128, dtype=np.float32), "mask_f": mf, "mask_b": mb, "sm_f": sm_f, "sm_b": sm_b,
        "m_same": m_same, "m_c0": m_c0, "m_c1": m_c1, "reset64": rs64, "rot_inv": inv, "rot_sgn": sgn,
    }
    in_maps = []
    for core in range(N_CORES):
        b = core % 4
        m = dict(shared)
        m["x"] = x[b]
        m["cT"] = np.ascontiguousarray(c[b].reshape(KT, 128).T)
        m["positions"] = positions[b:b + 1]
        half = core // 4
        m["tokidx"] = (half * (T // 2) + np.arange((T // 2) // 128, dtype=np.int32)[None, :] * 128
                       + np.arange(128, dtype=np.int32)[:, None]).astype(np.int32)
        in_maps.append(m)
    nc = build_program()
    res = run_bass_kernel_spmd(nc, in_maps, core_ids=list(range(N_CORES)))
    full = np.empty((4, T, D), np.float32)
    for core in range(N_CORES):
        b, half = core % 4, core // 4
        full[b, half * (T // 2):(half + 1) * (T // 2)] = np.asarray(res.results[core]["out"])
    return full
```

```python
from contextlib import ExitStack
import math
import numpy as np
import concourse.bass as bass
import concourse.mybir as mybir
from concourse.bass_utils import run_bass_kernel_spmd

F32 = mybir.dt.float32
BF16 = mybir.dt.bfloat16
I32 = mybir.dt.int32
AF = mybir.ActivationFunctionType
ALU = mybir.AluOpType
AX = mybir.AxisListType

D = 1024
T = 8192
KT = D // 128
EPS = 1e-6
D_FF = 2816
N_EXP = 8
D_EXP = 3584
ST = 2048
NSUB = ST // 128

SEM_CHUNK = 10 ** 9


class Sched:
    ENG = ("pe", "act", "dve", "pool", "sp")
    ENGMAP = {"pe": "tensor", "act": "scalar", "dve": "vector", "pool": "gpsimd", "sp": "sync"}
    DMA_RETIRE = 4000
    MAX_INFLIGHT = 8

    def __init__(self, nc, stack):
        self.nc = nc
        self.stack = stack
        self.q = {e: [] for e in self.ENG}
        self.cnt = {e: 0 for e in self.ENG}
        self.eng_sems = {e: [] for e in self.ENG}
        self.phys = []
        self.free = []
        self.key2phys = {}
        self.writers = {}
        self.readers = {}
        self.seen = {e: {} for e in self.ENG}
        self.nsem = 0
        self.ninstr = 0
        self.inflight = {}
        self.ninflight = {}

    def _new_sem(self, name):
        self.nsem += 1
        return self.stack.enter_context(self.nc.semaphore(name))

    def _eng_sem(self, e, idx):
        k = (idx - 1) // SEM_CHUNK
        while len(self.eng_sems[e]) <= k:
            self.eng_sems[e].append(self._new_sem(f"s_{e}_{len(self.eng_sems[e])}"))
        return self.eng_sems[e][k], (idx - 1) % SEM_CHUNK + 1

    def _phys_of(self, key, eng):
        p = self.key2phys.get(key)
        if p is None:
            fl = [i for i in self.free if self.phys[i][2] == eng]
            if fl:
                p = fl[-1]
                self.free.remove(p)
            else:
                p = len(self.phys)
                self.phys.append([self._new_sem(f"d_{p}"), 0, eng])
            self.key2phys[key] = p
        assert self.phys[p][2] == eng, (key, eng)
        return p

    def _unit_wait(self, unit, idx):
        if unit[0] == "e":
            return self._eng_sem(unit[1], idx)
        return self.phys[unit[1]][0], idx * 16

    def _collect(self, eng, reads, writes):
        need = {}

        def add(d):
            for u, i in d.items():
                if need.get(u, 0) < i:
                    need[u] = i
        for b in reads:
            add(self.writers.get(b, {}))
        for b in writes:
            add(self.writers.get(b, {}))
            add(self.readers.get(b, {}))
        return self._filter(eng, need)

    def _filter(self, eng, need):
        waits = []
        seen = self.seen[eng]
        for u, i in need.items():
            if u == ("e", "pe") and eng == "pe":
                continue
            if u[0] == "d":
                i = self.phys[u[1]][1]
            if seen.get(u, 0) >= i:
                continue
            seen[u] = i
            waits.append(self._unit_wait(u, i))
        return waits

    def op(self, eng, fn, reads=(), writes=()):
        waits = self._collect(eng, reads, writes)
        self.cnt[eng] += 1
        idx = self.cnt[eng]
        sem, _ = self._eng_sem(eng, idx)
        self.q[eng].append((waits, fn, sem, 1))
        u = ("e", eng)
        for b in reads:
            self.readers.setdefault(b, {})[u] = idx
        for b in writes:
            self.writers.setdefault(b, {})[u] = idx
        self.ninstr += 1

    def dma(self, eng, key, fn, reads=(), writes=()):
        waits = self._collect(eng, reads, writes)
        out = self.inflight.setdefault(eng, set())
        if self.ninflight.get(eng, 0) >= self.MAX_INFLIGHT:
            waits = waits + self._filter(eng, {("d", p_): self.phys[p_][1] for p_ in out})
            out.clear()
            self.ninflight[eng] = 0
        p = self._phys_of(key, eng)
        out.add(p)
        self.ninflight[eng] = self.ninflight.get(eng, 0) + 1
        self.phys[p][1] += 1
        idx = self.phys[p][1]
        self.q[eng].append((waits, fn, self.phys[p][0], 16))
        u = ("d", p)
        for b in reads:
            self.readers.setdefault(b, {})[u] = idx
        for b in writes:
            self.writers.setdefault(b, {})[u] = idx
        self.ninstr += 1

    def barrier(self):
        need = {("e", e): c for e, c in self.cnt.items() if c > 0}
        for p, (sem, c, _q) in enumerate(self.phys):
            if c > 0:
                need[("d", p)] = c
        for e in self.ENG:
            waits = self._filter(e, dict(need))
            self.q[e].append((waits, None, None, 0))
        self.writers = {}
        self.readers = {}
        self.key2phys = {}
        self.inflight = {}
        self.ninflight = {}
        self.free = [p for p, (sem, c, _q) in enumerate(self.phys) if c < self.DMA_RETIRE]

    def emit(self):
        nc = self.nc
        with nc.Block() as block:
            for e in self.ENG:
                items = self.q[e]
                if not items:
                    continue

                def body(engine, items=items):
                    for waits, fn, sem, inc in items:
                        for (ws, wv) in waits:
                            engine.wait_ge(ws, wv)
                        if fn is not None:
                            fn().then_inc(sem, inc)
                getattr(block, self.ENGMAP[e])(body)
        self.q = {e: [] for e in self.ENG}


class Ctx:
    def __init__(self, nc, stack):
        self.nc = nc
        self.S = Sched(nc, stack)
        self.stack = stack
        self.uid = 0

    def sb(self, st, name, shape, dt):
        self.uid += 1
        return st.enter_context(self.nc.sbuf_tensor(f"{name}_{self.uid}", shape, dt))

    def ps(self, st, name, shape, dt=F32):
        self.uid += 1
        return st.enter_context(self.nc.psum_tensor(f"{name}_{self.uid}", shape, dt))

    def mm(self, out, lhsT, rhs, start, stop, r, w):
        nc = self.nc
        self.S.op("pe", lambda: nc.tensor.matmul(out, lhsT, rhs, start=start, stop=stop), r, w)

    def tr(self, out, in_, ident, r, w):
        nc = self.nc
        self.S.op("pe", lambda: nc.tensor.transpose(out, in_, ident), r, w)

    def act(self, out, in_, func, r, w, bias=None, scale=1.0, accum_out=None):
        nc = self.nc
        kw = {}
        if bias is not None:
            kw["bias"] = bias
        if accum_out is not None:
            kw["accum_out"] = accum_out
        self.S.op("act", lambda: nc.scalar.activation(out=out, in_=in_, func=func, scale=scale, **kw), r, w)

    def _veng(self, eng):
        return self.nc.vector if eng == "dve" else self.nc.gpsimd

    def tt(self, eng, out, in0, in1, op, r, w):
        e = self._veng(eng)
        self.S.op(eng, lambda: e.tensor_tensor(out=out, in0=in0, in1=in1, op=op), r, w)

    def ts(self, eng, out, in0, s1, s2, op0, op1, r, w):
        e = self._veng(eng)
        if op1 is None:
            self.S.op(eng, lambda: e.tensor_scalar(out=out, in0=in0, scalar1=s1, scalar2=None, op0=op0), r, w)
        else:
            self.S.op(eng, lambda: e.tensor_scalar(out=out, in0=in0, scalar1=s1, scalar2=s2, op0=op0, op1=op1), r, w)

    def stt(self, eng, out, in0, scalar, in1, op0, op1, r, w):
        e = self._veng(eng)
        self.S.op(eng, lambda: e.scalar_tensor_tensor(out=out, in0=in0, scalar=scalar, in1=in1, op0=op0, op1=op1), r, w)

    def cp(self, eng, out, in_, r, w):
        nc = self.nc
        if eng == "act":
            self.S.op("act", lambda: nc.scalar.copy(out=out, in_=in_), r, w)
        else:
            e = self._veng(eng)
            self.S.op(eng, lambda: e.tensor_copy(out=out, in_=in_), r, w)

    def memset(self, eng, ap, val, w):
        e = self._veng(eng)
        self.S.op(eng, lambda: e.memset(ap, val), (), w)

    def reduce(self, eng, out, in_, op, r, w):
        e = self._veng(eng)
        self.S.op(eng, lambda: e.tensor_reduce(out=out, in_=in_, axis=AX.X, op=op), r, w)

    def dma(self, q, key, out, in_, r, w):
        nc = self.nc
        e = {"sp": nc.sync, "pool": nc.gpsimd, "act": nc.scalar}[q]
        self.S.dma(q, key, lambda: e.dma_start(out=out, in_=in_), r, w)

    def recip(self, out, in_, r, w):
        nc = self.nc
        self.S.op("dve", lambda: nc.vector.reciprocal(out=out, in_=in_), r, w)

    def rstd(self, out, in_, scale, key, eps=EPS):
        self.act(out, in_, AF.Sqrt, [key], [key], bias=eps, scale=scale)
        self.recip(out, out, [key], [key])

    def gather(self, key, out, in_full, idx_col, r, w):
        nc = self.nc
        self.S.dma("pool", key, lambda: nc.gpsimd.indirect_dma_start(
            out=out, out_offset=None, in_=in_full, in_offset=bass.IndirectOffsetOnAxis(ap=idx_col, axis=0)), r, w)

    def phase_end(self):
        self.S.barrier()
        self.S.emit()


def host_consts():
    c = {}
    c["ident"] = np.eye(128, dtype=np.float32)
    return c


class Glob:
    pass


def setup_globals(cx, st, dr):
    nc = cx.nc
    g = Glob()
    g.ident = cx.sb(st, "ident", [128, 128], F32)
    g.identb = cx.sb(st, "identb", [128, 128], BF16)
    g.ones32 = cx.sb(st, "ones32", [128, 128], F32)
    g.onesb = cx.sb(st, "onesb", [128, 128], BF16)
    g.zeros32 = cx.sb(st, "zeros32", [128, 128], F32)
    cx.dma("sp", "g_ident", g.ident[:], dr["ident"], [], ["ident"])
    cx.cp("dve", g.identb[:], g.ident[:], ["ident"], ["identb"])
    cx.memset("pool", g.ones32[:], 1.0, ["ones32"])
    cx.memset("pool", g.onesb[:], 1.0, ["onesb"])
    cx.memset("pool", g.zeros32[:], 0.0, ["zeros32"])
    g.mod = cx.sb(st, "mod", [128, 6, D], F32)
    return g


def phase_mod(cx, g, dr, layer):
    nc = cx.nc
    with ExitStack() as st:
        cT = cx.sb(st, "cT", [128, KT], F32)
        cs = cx.sb(st, "cs", [128, KT], F32)
        CB = cx.sb(st, "CB", [128, KT, 128], BF16)
        bias = cx.sb(st, "abias", [1, 6 * D], BF16)
        nwb = cx.sb(st, "nwb", [128, 4, D], F32)
        wbuf = [cx.sb(st, f"aw{i}", [128, KT, 512], BF16) for i in range(2)]
        pm = [cx.ps(st, f"pm{i}", [128, 512]) for i in range(2)]
        cx.dma("sp", "m_c", cT[:], dr["cT"], [], ["cT"])
        cx.act(cs[:], cT[:], AF.Silu, ["cT"], ["cs"])
        for kt in range(KT):
            cx.act(CB[:, kt, :], g.zeros32[:], AF.Identity, ["cs", "zeros32"], ["CB"], bias=cs[:, kt:kt + 1])
        cx.dma("pool", "m_b", bias[:].rearrange("o (c n) -> o c n", n=512),
               dr["adaln_b"][layer:layer + 1, :].rearrange("o (c n) -> o c n", n=512), [], ["abias"])
        cx.dma("sp", "m_nw", nwb[:].rearrange("p a d -> p (a d)"),
               dr["norm_w"][layer:layer + 1].rearrange("o a d -> o (a d)")[0].partition_broadcast(128), [], ["nwb"])
        wv = dr["adaln_w"][layer].rearrange("(kt p) n -> p kt n", p=128)
        for ch in range(12):
            wb = wbuf[ch % 2]
            wk = f"aw{ch % 2}"
            cx.dma("pool", wk, wb[:], wv[:, :, ch * 512:(ch + 1) * 512], [], [wk])
            p = pm[ch % 2]
            pk = f"pm{ch % 2}"
            for kt in range(KT):
                cx.mm(p[:], CB[:, kt, :], wb[:, kt, :], kt == 0, False, ["CB", wk], [pk])
            cx.mm(p[:], g.onesb[0:1, :], bias[0:1, ch * 512:(ch + 1) * 512], False, True, ["onesb", "abias"], [pk])
            j, half = ch // 2, ch % 2
            slot = {0: 1, 1: 0, 2: 2, 3: 4, 4: 3, 5: 5}[j]
            cx.cp("dve", g.mod[:, slot, half * 512:(half + 1) * 512], p[:], [pk], [("mod", slot)])
        for slot, nwi in ((0, 0), (3, 2)):
            cx.stt("dve", g.mod[:, slot, :], g.mod[:, slot, :], 1.0, nwb[:, nwi, :], ALU.add, ALU.mult,
                   [("mod", slot), "nwb"], [("mod", slot)])
        for slot, nwi in ((2, 1), (5, 3)):
            cx.tt("dve", g.mod[:, slot, :], g.mod[:, slot, :], nwb[:, nwi, :], ALU.mult,
                  [("mod", slot), "nwb"], [("mod", slot)])
        cx.phase_end()


class HTMaker:
    def __init__(self, cx, st, g, tag):
        self.cx, self.g, self.tag = cx, g, tag
        self.xt = [cx.sb(st, f"{tag}xt{i}", [128, D], F32) for i in range(2)]
        self.h = [cx.sb(st, f"{tag}h{i}", [128, D], F32) for i in range(2)]
        self.junk = cx.sb(st, f"{tag}junk", [128, D], BF16)
        self.ss = [cx.sb(st, f"{tag}ss{i}", [128, 2], F32) for i in range(2)]
        self.pT = [cx.ps(st, f"{tag}pT{i}", [128, 4, 128]) for i in range(2)]
        self.n = 0

    def run(self, x_rows, aslot, shslot, hT, hTkey, col0, gather=None):
        cx, g, tag = self.cx, self.g, self.tag
        i = self.n % 2
        self.n += 1
        xt, h, ss = self.xt[i], self.h[i], self.ss[i]
        kx, kh, ks = f"{tag}xt{i}", f"{tag}h{i}", f"{tag}ss{i}"
        if gather is None:
            cx.dma("sp", kx, xt[:], x_rows, [], [kx])
        else:
            cx.gather("g" + kx, xt[:, :], gather[0], gather[1], [gather[2]], [kx])
        cx.act(self.junk[:], xt[:], AF.Square, [kx], [ks], accum_out=ss[:, 0:1])
        cx.rstd(ss[:, 1:2], ss[:, 0:1], 1.0 / D, ks)
        cx.stt("dve", h[:], xt[:], ss[:, 1:2], g.mod[:, aslot, :], ALU.mult, ALU.mult, [kx, ks, ("mod", aslot)], [kh])
        cx.tt("pool", h[:], h[:], g.mod[:, shslot, :], ALU.add, [kh, ("mod", shslot)], [kh])
        for half in range(2):
            p = self.pT[half]
            pk = f"{tag}pT{half}"
            for q in range(4):
                kt = half * 4 + q
                cx.tr(p[:, q, :], h[:, kt * 128:(kt + 1) * 128], g.ident[:], [kh, "ident"], [pk])
            cx.cp("act", hT[:, half * 4:(half + 1) * 4, col0:col0 + 128], p[:], [pk], [hTkey])


def phase_ffn(cx, g, dr, x_in, x_out, experts, router=None, tok_range=None, tokidx=None):
    nc = cx.nc
    if tok_range is None:
        tok_range = (0, T)
    ST = min(globals()["ST"], tok_range[1] - tok_range[0])
    NSUB = ST // 128
    F = experts[0][0].shape[1]
    FG = 256
    NG = F // FG
    with ExitStack() as st:
        hm = HTMaker(cx, st, g, "f")
        hT = cx.sb(st, "f_hT", [128, KT, ST], BF16)
        acc = cx.sb(st, "f_acc", [128, NSUB, D], F32)
        actT = cx.sb(st, "f_actT", [128, 2, ST], BF16)
        w1b = [cx.sb(st, f"f_w1_{i}", [128, KT, FG], BF16) for i in range(2)]
        w3b = [cx.sb(st, f"f_w3_{i}", [128, KT, FG], BF16) for i in range(2)]
        w2b = [cx.sb(st, f"f_w2_{i}", [128, 2, D], BF16) for i in range(2)]
        sil = [cx.sb(st, f"f_sil{i}", [128, 512], BF16) for i in range(2)]
        pu1 = [cx.ps(st, f"f_pu1_{i}", [128, 512]) for i in range(2)]
        pu3 = [cx.ps(st, f"f_pu3_{i}", [128, 512]) for i in range(2)]
        py = [cx.ps(st, f"f_py{i}", [128, 512]) for i in range(2)]
        if router is not None:
            wr = cx.sb(st, "f_wr", [128, KT, N_EXP], BF16)
            comb = cx.sb(st, "f_comb", [128, NSUB, N_EXP], F32)
            rt = cx.sb(st, "f_rt", [128, 8, N_EXP], F32)
            rs = cx.sb(st, "f_rs", [128, 8], F32)
            cx.dma("pool", "f_wr", wr[:], router.rearrange("(kt p) n -> p kt n", p=128), [], ["f_wr"])
        yo = [cx.sb(st, f"f_yo{i}", [128, D], F32) for i in range(2)]
        xr = [cx.sb(st, f"f_xr{i}", [128, D], F32) for i in range(2)]
        fs = [cx.sb(st, f"f_fs{i}", [128, 2], F32) for i in range(2)]
        gcount = 0
        if tokidx is not None:
            idx = cx.sb(st, "f_idx", [128, (tok_range[1] - tok_range[0]) // 128], I32)
            cx.dma("sp", "f_idx", idx[:], tokidx, [], ["f_idx"])
        for t0 in range(tok_range[0], tok_range[1], ST):
            for s in range(NSUB):
                j = (t0 - tok_range[0]) // 128 + s
                gth = None if tokidx is None else (x_in[:, :], idx[:, j:j + 1], "f_idx")
                hm.run(x_in[t0 + s * 128:t0 + (s + 1) * 128, :], 3, 4, hT, "f_hT", s * 128, gather=gth)
            if router is not None:
                for s in range(NSUB):
                    pr = py[s % 2]
                    pk = f"f_py{s % 2}"
                    for kt in range(KT):
                        cx.mm(pr[:, 0:N_EXP], hT[:, kt, s * 128:(s + 1) * 128], wr[:, kt, :], kt == 0, kt == KT - 1,
                              ["f_hT", "f_wr"], [pk])
                    L, EQ, L2, SEL, EX = (rt[:, i, :] for i in range(5))
                    cx.cp("dve", L, pr[:, 0:N_EXP], [pk], ["f_rt"])
                    cx.reduce("dve", rs[:, 0:1], L, ALU.max, ["f_rt"], ["f_rs"])
                    cx.ts("dve", EQ, L, rs[:, 0:1], None, ALU.is_equal, None, ["f_rt", "f_rs"], ["f_rt"])
                    cx.stt("dve", L2, EQ, -1e30, L, ALU.mult, ALU.add, ["f_rt"], ["f_rt"])
                    cx.reduce("dve", rs[:, 1:2], L2, ALU.max, ["f_rt"], ["f_rs"])
                    cx.ts("dve", SEL, L, rs[:, 1:2], None, ALU.is_ge, None, ["f_rt", "f_rs"], ["f_rt"])
                    cx.ts("dve", rs[:, 2:3], rs[:, 0:1], -1.0, None, ALU.mult, None, ["f_rs"], ["f_rs"])
                    cx.act(EX, L, AF.Exp, ["f_rt", "f_rs"], ["f_rt"], bias=rs[:, 2:3])
                    cx.tt("dve", EX, EX, SEL, ALU.mult, ["f_rt"], ["f_rt"])
                    cx.reduce("dve", rs[:, 3:4], EX, ALU.add, ["f_rt"], ["f_rs"])
                    cx.S.op("dve", (lambda o=rs[:, 4:5], i_=rs[:, 3:4]: nc.vector.reciprocal(out=o, in_=i_)), ["f_rs"], ["f_rs"])
                    cx.ts("dve", comb[:, s, :], EX, rs[:, 4:5], None, ALU.mult, None, ["f_rt", "f_rs"], ["f_comb"])
            first = True
            for e, (w1, w3, w2) in enumerate(experts):
                w1v = w1.rearrange("(kt p) n -> p kt n", p=128)
                w3v = w3.rearrange("(kt p) n -> p kt n", p=128)
                w2v = w2.rearrange("(f p) n -> p f n", p=128)
                for gi in range(NG):
                    b = gcount % 2
                    gcount += 1
                    k1, k3, k2 = f"f_w1_{b}", f"f_w3_{b}", f"f_w2_{b}"
                    cx.dma("pool", k1, w1b[b][:], w1v[:, :, gi * FG:(gi + 1) * FG], [], [k1])
                    cx.dma("pool", k3, w3b[b][:], w3v[:, :, gi * FG:(gi + 1) * FG], [], [k3])
                    cx.dma("pool", k2, w2b[b][:], w2v[:, gi * 2:(gi + 1) * 2, :], [], [k2])
                    for ch in range(ST // 512):
                        for f in range(2):
                            pb = (ch * 2 + f) % 2
                            for kt in range(KT):
                                cx.mm(pu1[pb][:], w1b[b][:, kt, f * 128:(f + 1) * 128], hT[:, kt, ch * 512:(ch + 1) * 512],
                                      kt == 0, kt == KT - 1, [k1, "f_hT"], [f"f_pu1{pb}"])
                            for kt in range(KT):
                                cx.mm(pu3[pb][:], w3b[b][:, kt, f * 128:(f + 1) * 128], hT[:, kt, ch * 512:(ch + 1) * 512],
                                      kt == 0, kt == KT - 1, [k3, "f_hT"], [f"f_pu3{pb}"])
                            sb_ = sil[pb]
                            sk = f"f_sil{pb}"
                            cx.act(sb_[:], pu1[pb][:], AF.Silu, [f"f_pu1{pb}"], [sk])
                            cx.tt("dve", actT[:, f, ch * 512:(ch + 1) * 512], sb_[:], pu3[pb][:], ALU.mult,
                                  [sk, f"f_pu3{pb}"], [("f_actT", ch)])
                    for s in range(NSUB):
                        for half in range(2):
                            p = py[(s * 2 + half) % 2]
                            pk = f"f_py{(s * 2 + half) % 2}"
                            for f in range(2):
                                cx.mm(p[:], actT[:, f, s * 128:(s + 1) * 128], w2b[b][:, f, half * 512:(half + 1) * 512],
                                      f == 0, f == 1, [("f_actT", s // 4), k2], [pk])
                            a = acc[:, s, half * 512:(half + 1) * 512]
                            ak = ("f_acc", s)
                            if router is None:
                                if first:
                                    cx.cp("dve", a, p[:], [pk], [ak])
                                else:
                                    cx.tt("dve", a, a, p[:], ALU.add, [pk, ak], [ak])
                            else:
                                if first:
                                    cx.ts("dve", a, p[:], comb[:, s, e:e + 1], None, ALU.mult, None, [pk, "f_comb"], [ak])
                                else:
                                    cx.stt("dve", a, p[:], comb[:, s, e:e + 1], a, ALU.mult, ALU.add, [pk, "f_comb", ak], [ak])
                    first = False
            for s in range(NSUB):
                i = s % 2
                ky, kx, kf = f"f_yo{i}", f"f_xr{i}", f"f_fs{i}"
                rows = slice(t0 + s * 128, t0 + (s + 1) * 128)
                if tokidx is None:
                    cx.dma("sp", kx, xr[i][:], x_in[rows, :], [], [kx])
                else:
                    j = (t0 - tok_range[0]) // 128 + s
                    cx.gather("g" + kx, xr[i][:, :], x_in[:, :], idx[:, j:j + 1], ["f_idx"], [kx])
                cx.act(yo[i][:], acc[:, s, :], AF.Square, [("f_acc", s)], [ky, kf], accum_out=fs[i][:, 0:1])
                cx.rstd(fs[i][:, 1:2], fs[i][:, 0:1], 1.0 / D, kf)
                cx.stt("dve", yo[i][:], acc[:, s, :], fs[i][:, 1:2], g.mod[:, 5, :], ALU.mult, ALU.mult,
                       [("f_acc", s), kf, ("mod", 5)], [ky])
                cx.tt("pool", yo[i][:], yo[i][:], xr[i][:], ALU.add, [ky, kx], [ky])
                cx.dma("sp", ky, x_out[rows, :], yo[i][:], [ky], [("xout", s)])
        cx.phase_end()


OFF = 2
TP = T + 6
FM_GQ, FM_GK, FM_GR, FM_GW, FM_DQ, FM_DQS, FM_DK, FM_DKS, FM_NC, FM_NZ, FM_MG = 0, 2, 4, 8, 9, 13, 17, 21, 25, 37, 41
NFM = 65
NFMG = 17
TM_GV, TM_DV, TM_GL = 0, 512, 1024
NTM = 1040

IN_SIZES = (256, 256, 512, 512, 16, 16, 512, 512, 512, 512, 512, 512, 512, 4, 4, 4, 4, 3072)
IN_OFF = np.concatenate([[0], np.cumsum(IN_SIZES)]).astype(int)
(C_GQ, C_GK, C_GV, C_GR, C_GWF, C_GWB, C_DQ, C_DK, C_DV, C_NQ, C_NK, C_NV, C_NZ, C_NBF, C_NBB, C_NAF, C_NAB, C_MG) = IN_OFF[:-1]


def pack_w_in(w):
    z = lambda n: np.zeros((D, n), np.float32)
    rng = lambda a, n: w[:, a:a + n]
    swap = np.arange(512)
    for hd in range(8):
        for i in range(16):
            swap[hd * 64 + i] = hd * 64 + (i + 8 if i < 8 else i - 8)
    fm = [rng(C_GQ, 256), rng(C_GK, 256), rng(C_GR, 512),
          rng(C_GWF, 16), z(16), rng(C_GWB, 16), z(80),
          rng(C_DQ, 512), rng(C_DQ, 512)[:, swap], rng(C_DK, 512), rng(C_DK, 512)[:, swap],
          rng(C_NQ, 512), rng(C_NK, 512), rng(C_NV, 512), rng(C_NZ, 512), rng(C_MG, 3072), z(NFMG * 512 - NFM * 128)]
    fm = np.concatenate(fm, axis=1)
    assert fm.shape[1] == NFMG * 512
    tm = np.concatenate([rng(C_GV, 512), rng(C_DV, 512), rng(C_NAF, 4), rng(C_NAB, 4), rng(C_NBF, 4), rng(C_NBB, 4)], axis=1)
    assert tm.shape[1] == NTM
    return np.ascontiguousarray(fm), np.ascontiguousarray(tm)


def fm_evac_kind(tile):
    if tile < FM_GK:
        return ("scale", 0.125)
    if FM_GR <= tile < FM_GW:
        return ("act", AF.Silu)
    if FM_NZ <= tile < FM_MG:
        return ("act", AF.Silu)
    if tile >= FM_MG:
        return ("act", AF.Sigmoid)
    return ("copy", None)


def phase_proj(cx, g, dr, x_in, layer, sc):
    nc = cx.nc
    with ExitStack() as st:
        hm = HTMaker(cx, st, g, "p")
        hT = cx.sb(st, "p_hT", [128, KT, ST], BF16)
        wb = [cx.sb(st, f"p_w{i}", [128, KT, 512], BF16) for i in range(2)]
        stg = [cx.sb(st, f"p_stg{i}", [128, ST], BF16) for i in range(2)]
        tstg = [cx.sb(st, f"p_tstg{i}", [128, 512], BF16) for i in range(2)]
        gstg = [cx.sb(st, f"p_gstg{i}", [128, 16], F32) for i in range(2)]
        zpad = cx.sb(st, "p_zpad", [128, 8], BF16)
        pp = [cx.ps(st, f"p_pp{i}", [128, 512]) for i in range(4)]
        wfm = dr["w_fm"][layer].rearrange("(kt p) n -> p kt n", p=128)
        wtm = dr["w_tm"][layer].rearrange("(kt p) n -> p kt n", p=128)
        cx.memset("pool", zpad[:], 0.0, ["p_zpad"])
        for tile in range(FM_NC, FM_NC + 12):
            cx.dma("sp", "p_zp", sc["FM"][tile, :, 0:OFF], zpad[:, 0:OFF], ["p_zpad"], [])
            cx.dma("sp", "p_zp", sc["FM"][tile, :, OFF + T:TP], zpad[:, 0:TP - OFF - T], ["p_zpad"], [])
        wcount = 0
        pcount = 0
        scount = 0
        for si in range(T // ST):
            t0 = si * ST
            for s in range(NSUB):
                hm.run(x_in[t0 + s * 128:t0 + (s + 1) * 128, :], 0, 1, hT, "p_hT", s * 128)
            for gi in range(NFMG):
                b = wcount % 2
                wcount += 1
                wk = f"p_w{b}"
                cx.dma("pool", wk, wb[b][:], wfm[:, :, gi * 512:(gi + 1) * 512], [], [wk])
                for q in range(4):
                    tile = gi * 4 + q
                    if tile >= NFM:
                        break
                    sb_ = scount % 2
                    scount += 1
                    sk = f"p_stg{sb_}"
                    kind, arg = fm_evac_kind(tile)
                    for ch in range(ST // 512):
                        pb = pcount % 4
                        pcount += 1
                        pk = f"p_pp{pb}"
                        for kt in range(KT):
                            cx.mm(pp[pb][:], wb[b][:, kt, q * 128:(q + 1) * 128], hT[:, kt, ch * 512:(ch + 1) * 512],
                                  kt == 0, kt == KT - 1, [wk, "p_hT"], [pk])
                        o = stg[sb_][:, ch * 512:(ch + 1) * 512]
                        if kind == "act":
                            cx.act(o, pp[pb][:], arg, [pk], [sk])
                        elif kind == "scale":
                            cx.ts("dve", o, pp[pb][:], arg, None, ALU.mult, None, [pk], [sk])
                        else:
                            cx.cp("dve", o, pp[pb][:], [pk], [sk])
                    cx.dma("sp", sk, sc["FM"][tile, :, OFF + t0:OFF + t0 + ST], stg[sb_][:], [sk], [])
            for (c0, ncol) in ((TM_GV, 512), (TM_DV, 512), (TM_GL, 16)):
                b = wcount % 2
                wcount += 1
                wk = f"p_w{b}"
                cx.dma("pool", wk, wb[b][:, :, 0:ncol], wtm[:, :, c0:c0 + ncol], [], [wk])
                for s in range(NSUB):
                    pb = pcount % 4
                    pcount += 1
                    pk = f"p_pp{pb}"
                    for kt in range(KT):
                        cx.mm(pp[pb][:, 0:ncol], hT[:, kt, s * 128:(s + 1) * 128], wb[b][:, kt, 0:ncol],
                              kt == 0, kt == KT - 1, [wk, "p_hT"], [pk])
                    rows = slice(t0 + s * 128, t0 + (s + 1) * 128)
                    if ncol == 512:
                        tb = s % 2
                        tk = f"p_tstg{tb}"
                        cx.cp("dve" if s % 2 else "act", tstg[tb][:], pp[pb][:], [pk], [tk])
                        cx.dma("sp", tk, sc["TM"][rows, c0:c0 + 512], tstg[tb][:], [tk], [])
                    else:
                        tb = s % 2
                        tk = f"p_gstg{tb}"
                        cx.cp("dve", gstg[tb][:], pp[pb][:, 0:16], [pk], [tk])
                        cx.dma("sp", tk, sc["GL"][rows, :], gstg[tb][:], [tk], [])
        cx.phase_end()


def rot_consts():
    inv = np.zeros((128, 1), np.float32)
    sgn = np.zeros((128, 1), np.float32)
    for p in range(128):
        i = p % 64
        if i < 16:
            inv[p, 0] = 500000.0 ** (-(2 * (i % 8)) / 16.0)
            sgn[p, 0] = -1.0 if i < 8 else 1.0
    return inv, sgn


def phase_diff(cx, g, dr, layer, sc):
    nc = cx.nc
    lam_init = 0.8 - 0.6 * math.exp(-0.3 * layer)
    TWO_PI = 2.0 * math.pi
    CH = 2048
    with ExitStack() as st:
        Ct = cx.sb(st, "d_C", [128, T], BF16)
        St = cx.sb(st, "d_S", [128, T], BF16)
        inv = cx.sb(st, "d_inv", [128, 1], F32)
        sgn = cx.sb(st, "d_sgn", [128, 1], F32)
        pi_ = cx.sb(st, "d_pi", [128, CH], I32)
        ang = cx.sb(st, "d_ang", [128, CH], F32)
        tf = cx.sb(st, "d_tf", [128, CH], F32)
        ti = cx.sb(st, "d_ti", [128, CH], I32)
        fx = cx.sb(st, "d_fx", [128, CH], F32)
        cx.dma("sp", "d_c0", inv[:], dr["rot_inv"], [], ["d_inv"])
        cx.dma("sp", "d_c1", sgn[:], dr["rot_sgn"], [], ["d_sgn"])

        def reduced_sin(out_bf, shift, kout, scale_ap=None):
            cx.ts("dve", tf[:], ang[:], shift, 1.0 / TWO_PI, ALU.add, ALU.mult, ["d_ang"], ["d_tf"])
            cx.cp("dve", ti[:], tf[:], ["d_tf"], ["d_ti"])
            cx.cp("dve", tf[:], ti[:], ["d_ti"], ["d_tf"])
            cx.stt("dve", tf[:], tf[:], -TWO_PI, ang[:], ALU.mult, ALU.add, ["d_tf", "d_ang"], ["d_tf"])
            cx.ts("dve", tf[:], tf[:], shift, None, ALU.add, None, ["d_tf"], ["d_tf"])
            cx.ts("dve", fx[:], tf[:], math.pi, -TWO_PI, ALU.is_gt, ALU.mult, ["d_tf"], ["d_fx"])
            cx.tt("dve", tf[:], tf[:], fx[:], ALU.add, ["d_tf", "d_fx"], ["d_tf"])
            cx.ts("dve", fx[:], tf[:], -math.pi, TWO_PI, ALU.is_lt, ALU.mult, ["d_tf"], ["d_fx"])
            cx.tt("dve", tf[:], tf[:], fx[:], ALU.add, ["d_tf", "d_fx"], ["d_tf"])
            cx.ts("dve", tf[:], tf[:], -math.pi, math.pi, ALU.max, ALU.min, ["d_tf"], ["d_tf"])
            if scale_ap is None:
                cx.act(out_bf, tf[:], AF.Sin, ["d_tf"], [kout])
            else:
                cx.act(out_bf, tf[:], AF.Sin, ["d_tf", "d_sgn"], [kout], scale=scale_ap)

        for c in range(T // CH):
            cx.dma("sp", "d_pi", pi_[:], dr["positions"][0, c * CH:(c + 1) * CH].partition_broadcast(128), [], ["d_pi"])
            cx.cp("dve", ang[:], pi_[:], ["d_pi"], ["d_ang"])
            cx.ts("dve", ang[:], ang[:], inv[:, 0:1], None, ALU.mult, None, ["d_ang", "d_inv"], ["d_ang"])
            reduced_sin(Ct[:, c * CH:(c + 1) * CH], math.pi / 2.0, "d_C")
            reduced_sin(St[:, c * CH:(c + 1) * CH], 0.0, "d_S", scale_ap=sgn[:, 0:1])

        lp = cx.sb(st, "d_lp", [128, 4, 64], F32)
        pr = cx.sb(st, "d_pr", [128, 2, 64], F32)
        lc = cx.sb(st, "d_lc", [128, 8], F32)
        nwc = cx.sb(st, "d_nwc", [128, 1], F32)
        cx.dma("sp", "d_c2", lp[:].rearrange("p a b -> p (a b)"),
               dr["diff_lambda"][layer:layer + 1].rearrange("o a b -> o (a b)")[0].partition_broadcast(128), [], ["d_lp"])
        cx.dma("sp", "d_c3", nwc[:], dr["diff_norm"][layer].rearrange("(p o) -> p o", o=1), [], ["d_nwc"])
        cx.tt("dve", pr[:, 0, :], lp[:, 0, :], lp[:, 1, :], ALU.mult, ["d_lp"], ["d_pr"])
        cx.tt("dve", pr[:, 1, :], lp[:, 2, :], lp[:, 3, :], ALU.mult, ["d_lp"], ["d_pr"])
        cx.reduce("dve", lc[:, 0:1], pr[:, 0, :], ALU.add, ["d_pr"], ["d_lc"])
        cx.reduce("dve", lc[:, 1:2], pr[:, 1, :], ALU.add, ["d_pr"], ["d_lc"])
        cx.act(lc[:, 2:4], lc[:, 0:2], AF.Exp, ["d_lc"], ["d_lc"])
        cx.tt("dve", lc[:, 4:5], lc[:, 3:4], lc[:, 2:3], ALU.subtract, ["d_lc"], ["d_lc"])
        cx.ts("dve", lc[:, 4:5], lc[:, 4:5], -lam_init, None, ALU.add, None, ["d_lc"], ["d_lc"])
        cx.ts("dve", nwc[:], nwc[:], 1.0 - lam_init, None, ALU.mult, None, ["d_nwc"], ["d_nwc"])

        qb = cx.sb(st, "d_q", [128, T], BF16)
        kb = cx.sb(st, "d_k", [128, T], BF16)
        xb = cx.sb(st, "d_x", [128, T], BF16)
        vt = cx.sb(st, "d_v", [128, T // 128, 128], BF16)
        pbuf = [cx.sb(st, f"d_p{i}", [128, 512], BF16) for i in range(4)]
        accL = [cx.sb(st, f"d_acc{i}", [128, 512], F32) for i in range(4)]
        accS = cx.sb(st, "d_accS", [128, 512], BF16)
        osb = [cx.sb(st, f"d_os{i}", [128, 512], F32) for i in range(2)]
        rl = cx.sb(st, "d_rl", [128, 512], F32)
        od = cx.sb(st, "d_od", [128, 512], F32)
        sq = cx.sb(st, "d_sq", [128, 512], BF16)
        yst = [cx.sb(st, f"d_y{i}", [128, 512], BF16) for i in range(2)]
        pS = [cx.ps(st, f"d_pS{i}", [128, 512]) for i in range(4)]
        pO = [cx.ps(st, f"d_pO{i}", [128, 512]) for i in range(2)]
        pL = cx.ps(st, "d_pL", [128, 512])
        pN = cx.ps(st, "d_pN", [128, 512])
        NK = T // 128
        ycount = 0
        for ht in range(4):
            for (dst, dk_, t_main, t_sw) in ((qb, "d_q", FM_DQ + ht, FM_DQS + ht), (kb, "d_k", FM_DK + ht, FM_DKS + ht)):
                cx.dma("sp", dk_, dst[:], sc["FM"][t_main, :, OFF:OFF + T], [], [dk_])
                cx.dma("sp", "d_x", xb[:], sc["FM"][t_sw, :, OFF:OFF + T], [], ["d_x"])
                cx.tt("dve", dst[:], dst[:], Ct[:], ALU.mult, [dk_, "d_C"], [dk_])
                cx.tt("pool", xb[:], xb[:], St[:], ALU.mult, ["d_x", "d_S"], ["d_x"])
                cx.tt("dve", dst[:], dst[:], xb[:], ALU.add, [dk_, "d_x"], [dk_])
            cx.dma("sp", "d_v", vt[:], sc["TM"][:, TM_DV + ht * 128:TM_DV + (ht + 1) * 128].rearrange("(n p) c -> p n c", p=128),
                   [], ["d_v"])
            for qc in range(T // 512):
                qs_ = slice(qc * 512, (qc + 1) * 512)

                def smm(kt, s, it):
                    rs_ = slice(s * 64, (s + 1) * 64)
                    cx.mm(pS[it % 4][:], kb[rs_, kt * 128:(kt + 1) * 128], qb[rs_, qs_], True, True,
                          ["d_k", "d_q"], [f"d_pS{it % 4}"])
                base = qc * 2 * NK
                smm(0, 0, base)
                smm(0, 1, base + 1)
                for kt in range(NK):
                    for s in range(2):
                        it = base + kt * 2 + s
                        if kt + 1 < NK:
                            smm(kt + 1, s, it + 2)
                        pb_ = pbuf[it % 4]
                        pk = f"d_p{it % 4}"
                        cx.act(pb_[:], pS[it % 4][:], AF.Exp, [f"d_pS{it % 4}"], [pk], scale=0.125)
                        cx.mm(pO[s][:], vt[:, kt, :], pb_[:], kt == 0, kt == NK - 1, ["d_v", pk], [f"d_pO{s}"])
                        j = kt % 2
                        ae = "dve" if (s + j) % 2 == 0 else "pool"
                        a_, ak_ = accL[s * 2 + j], f"d_acc{s * 2 + j}"
                        if kt < 2:
                            cx.cp(ae, a_[:], pb_[:], [pk], [ak_])
                        else:
                            cx.tt(ae, a_[:], a_[:], pb_[:], ALU.add, [ak_, pk], [ak_])
                for s in range(2):
                    cx.tt("dve", accS[:], accL[s * 2][:], accL[s * 2 + 1][:], ALU.add, [f"d_acc{s * 2}", f"d_acc{s * 2 + 1}"], ["d_accS"])
                    cx.mm(pL[:], g.onesb[:], accS[:], True, True, ["onesb", "d_accS"], ["d_pL"])
                    cx.recip(rl[:], pL[:], ["d_pL"], ["d_rl"])
                    cx.tt("dve", osb[s][:], pO[s][:], rl[:], ALU.mult, [f"d_pO{s}", "d_rl"], [f"d_os{s}"])
                cx.stt("dve", od[:], osb[1][:], lc[:, 4:5], osb[0][:], ALU.mult, ALU.add, ["d_os0", "d_os1", "d_lc"], ["d_od"])
                cx.act(sq[:], od[:], AF.Square, ["d_od"], ["d_sq"])
                cx.mm(pN[:], g.onesb[:], sq[:], True, True, ["onesb", "d_sq"], ["d_pN"])
                cx.act(rl[:], pN[:], AF.Sqrt, ["d_pN"], ["d_rl"], bias=EPS, scale=1.0 / 128)
                cx.recip(rl[:], rl[:], ["d_rl"], ["d_rl"])
                yb = yst[ycount % 2]
                yk = f"d_y{ycount % 2}"
                ycount += 1
                cx.stt("dve", yb[:], od[:], nwc[:, 0:1], rl[:], ALU.mult, ALU.mult, ["d_od", "d_nwc", "d_rl"], [yk])
                cx.dma("sp", yk, sc["YT"][1, ht, :, qs_], yb[:], [yk], [])
        cx.phase_end()


class Rot:
    def __init__(self, cx, st, name, shape, dt, n, psum=False):
        mk = cx.ps if psum else cx.sb
        self.t = [mk(st, f"{name}{i}", shape, dt) for i in range(n)]
        self.k = [f"{name}#{i}" for i in range(n)]
        self.i = -1

    def next(self):
        self.i += 1
        j = self.i % len(self.t)
        return self.t[j], self.k[j]

    def cur(self):
        j = self.i % len(self.t)
        return self.t[j], self.k[j]


def chunk_masks():
    j = np.arange(128)[:, None]
    i = np.arange(128)[None, :]
    same = (j // 64) == (i // 64)
    mf = (same & (j <= i)).astype(np.float32)
    mb = (same & (j >= i)).astype(np.float32)
    reset = np.ones((128, 512), np.float32)
    reset[:, ::64] = 0.0
    return mf, mb, reset


def phase_gla(cx, g, dr, layer, sc):
    nc = cx.nc
    NB = T // 512
    with ExitStack() as st:
        gu = cx.sb(st, "a_gu", [64, 256], BF16)
        nb = cx.sb(st, "a_nb", [128, 4], F32)
        nwc = cx.sb(st, "a_nwc", [128, 1], F32)
        mk = [cx.sb(st, f"a_mk{i}", [128, 128], F32) for i in range(2)]
        reset = cx.sb(st, "a_reset", [128, 512], F32)
        S = cx.sb(st, "a_S", [128, 128], BF16)
        cx.dma("pool", "a_c0", gu[0:16, :], dr["gla_gate_up"][layer, 0], [], ["a_gu"])
        cx.dma("pool", "a_c0", gu[32:48, :], dr["gla_gate_up"][layer, 1], [], ["a_gu"])
        for d in range(2):
            for rt in range(2):
                cx.dma("sp", "a_c1", nb[:, d * 2 + rt:d * 2 + rt + 1],
                       dr["gla_gate_bias"][layer, d, rt * 128:(rt + 1) * 128].rearrange("(p o) -> p o", o=1), [], ["a_nb"])
        cx.ts("dve", nb[:], nb[:], -1.0, None, ALU.mult, None, ["a_nb"], ["a_nb"])
        cx.dma("sp", "a_c2", nwc[:], dr["gla_norm"][layer].rearrange("(p o) -> p o", o=1), [], ["a_nwc"])
        cx.dma("sp", "a_c3", mk[0][:], dr["mask_f"], [], ["a_mk0"])
        cx.dma("sp", "a_c3", mk[1][:], dr["mask_b"], [], ["a_mk1"])
        cx.dma("sp", "a_c4", reset[:], dr["reset64"], [], ["a_reset"])

        gw = Rot(cx, st, "a_gw", [64, 512], BF16, 2)
        qk = Rot(cx, st, "a_qk", [128, 2, 512], BF16, 2)
        vtm = Rot(cx, st, "a_v", [128, 4, 256], BF16, 2)
        f32r = {n: Rot(cx, st, f"a_{n}", [128, 512], F32, 2) for n in ("e", "sp", "Bp", "Dm", "E3")}
        E12 = Rot(cx, st, "a_E12", [128, 512], F32, 2)
        tot = Rot(cx, st, "a_tot", [128, 8, 1], F32, 2)
        bfr = {n: Rot(cx, st, f"a_{n}", [128, 512], BF16, 2) for n in ("qt", "kt", "qd", "ke")}
        ketm = Rot(cx, st, "a_ketm", [128, 128], BF16, 2)
        Am = Rot(cx, st, "a_Am", [128, 128], BF16, 3)
        ofw = Rot(cx, st, "a_ofw", [128, 512], F32, 4)
        grb = Rot(cx, st, "a_gr", [128, 512], BF16, 4)
        osb = Rot(cx, st, "a_o", [128, 512], F32, 2)
        sq = Rot(cx, st, "a_sq", [128, 512], BF16, 2)
        rl = Rot(cx, st, "a_rl", [128, 512], F32, 2)
        yb = Rot(cx, st, "a_y", [128, 512], BF16, 2)
        pZ = cx.ps(st, "a_pZ", [128, 512])
        pN = pZ
        pO = Rot(cx, st, "a_pO", [128, 512], F32, 3, psum=True)
        pA = Rot(cx, st, "a_pA", [128, 128], F32, 2, psum=True)
        pKV = cx.ps(st, "a_pKV", [128, 128])
        pT = cx.ps(st, "a_pT", [128, 2, 128], BF16)

        for d in range(2):
            mid, last = (31, 63) if d == 0 else (32, 0)
            blocks = list(range(NB)) if d == 0 else list(range(NB - 1, -1, -1))
            for rt in range(2):
                cx.memset("dve", S[:], 0.0, ["a_S"])
                prep = {}

                def gates(b):
                    c0 = b * 512
                    cols = slice(OFF + c0, OFF + c0 + 512)
                    gwt, gwk = gw.next()
                    cx.dma("sp", gwk, gwt[:], sc["FM"][FM_GW, 0:64, cols], [], [gwk])
                    qkt, qkk = qk.next()
                    cx.dma("sp", qkk, qkt[:, 0, :], sc["FM"][FM_GQ + rt, :, cols], [], [qkk])
                    cx.dma("sp", qkk, qkt[:, 1, :], sc["FM"][FM_GK + rt, :, cols], [], [qkk])
                    vt, vk = vtm.next()
                    cx.dma("sp", vk, vt[:], sc["TM"][c0:c0 + 512, TM_GV + rt * 256:TM_GV + (rt + 1) * 256]
                           .rearrange("(n p) c -> p n c", p=128), [], [vk])
                    r0 = d * 32
                    cx.mm(pZ[:], gu[r0:r0 + 16, rt * 128:(rt + 1) * 128], gwt[r0:r0 + 16, :], True, True, ["a_gu", gwk], ["a_pZ"])
                    e, ek = f32r["e"].next()
                    cx.act(e[:], pZ[:], AF.Exp, ["a_pZ", "a_nb"], [ek], bias=nb[:, d * 2 + rt:d * 2 + rt + 1], scale=-1.0)
                    sp, spk = f32r["sp"].next()
                    cx.act(sp[:], e[:], AF.Ln, [ek], [spk], bias=1.0)
                    Bp, Bk = f32r["Bp"].next()
                    nc_ = nc
                    cx.S.op("dve", (lambda o=Bp[:], r_=reset[:], s_=sp[:]: nc_.vector.tensor_tensor_scan(o, r_, s_, 0.0, ALU.mult, ALU.add)),
                            ["a_reset", spk], [Bk])
                    B3 = Bp[:].rearrange("p (c j) -> p c j", j=64)
                    if d == 1:
                        Dm_, Dk_ = f32r["Dm"].next()
                        cx.tt("pool", Dm_[:], sp[:], Bp[:], ALU.subtract, [spk, Bk], [Dk_])
                        tot_, totk_ = tot.next()
                        cx.cp("dve", tot_[:], B3[:, :, 63:64], [Bk], [totk_])
                        cx.tt("dve", B3, Dm_[:].rearrange("p (c j) -> p c j", j=64), tot_[:].to_broadcast([128, 8, 64]),
                              ALU.add, [Dk_, totk_], [Bk])
                    Dm, Dk = f32r["Dm"].next()
                    cx.tt("dve", Dm[:].rearrange("p (c j) -> p c j", j=64), B3, B3[:, :, mid:mid + 1].to_broadcast([128, 8, 64]),
                          ALU.subtract, [Bk], [Dk])
                    E3, E3k = f32r["E3"].next()
                    cx.act(E3[:], Bp[:], AF.Exp, [Bk], [E3k], scale=-1.0 / 16)
                    qt, qtk = bfr["qt"].next()
                    kt_, ktk = bfr["kt"].next()
                    qd, qdk = bfr["qd"].next()
                    ke, kek = bfr["ke"].next()
                    Ea, Eak = E12.next()
                    cx.act(Ea[:], Dm[:], AF.Exp, [Dk], [Eak], scale=-1.0 / 16)
                    cx.tt("pool", qt[:], qkt[:, 0, :], Ea[:], ALU.mult, [qkk, Eak], [qtk])
                    Eb, Ebk = E12.next()
                    cx.act(Eb[:], Dm[:], AF.Exp, [Dk], [Ebk], scale=1.0 / 16)
                    cx.tt("pool", kt_[:], qkt[:, 1, :], Eb[:], ALU.mult, [qkk, Ebk], [ktk])
                    cx.tt("dve", qd[:], qkt[:, 0, :], E3[:], ALU.mult, [qkk, E3k], [qdk])
                    Dl, Dlk = f32r["Dm"].next()
                    cx.tt("dve", Dl[:].rearrange("p (c j) -> p c j", j=64), B3, B3[:, :, last:last + 1].to_broadcast([128, 8, 64]),
                          ALU.subtract, [Bk], [Dlk])
                    Ec, Eck = E12.next()
                    cx.act(Ec[:], Dl[:], AF.Exp, [Dlk], [Eck], scale=1.0 / 16)
                    cx.tt("pool", ke[:], qkt[:, 1, :], Ec[:], ALU.mult, [qkk, Eck], [kek])
                    ex = {}
                    if d == 1:
                        ex["ofw"] = []
                        ex["gr"] = []
                        for hh in range(2):
                            h = rt * 2 + hh
                            o_, ok_ = ofw.next()
                            cx.dma("sp", ok_, o_[:], sc["OFW"][h, :, c0:c0 + 512], [], [ok_])
                            ex["ofw"].append((o_, ok_))
                            g_, gk_ = grb.next()
                            cx.dma("sp", gk_, g_[:], sc["FM"][FM_GR + h, :, cols], [], [gk_])
                            ex["gr"].append((g_, gk_))
                    prep[b] = dict(vt=vt, vk=vk, E3=E3, E3k=E3k, qt=qt, qtk=qtk, kt=kt_, ktk=ktk, qd=qd, qdk=qdk, ke=ke, kek=kek, **ex)

                def tiles(b):
                    P = prep.pop(b)
                    c0 = b * 512
                    po = []
                    for hh in range(2):
                        po.append(pO.next())
                    torder = range(4) if d == 0 else range(3, -1, -1)
                    for tt_ in torder:
                        ts_ = slice(tt_ * 128, (tt_ + 1) * 128)
                        kb_, kbk = ketm.next()
                        tb = ketm.i % 2
                        cx.tr(pT[:, tb, :], P["ke"][:, ts_], g.identb[:], [P["kek"], "identb"], ["a_pT"])
                        cx.cp("act", kb_[:], pT[:, tb, :], ["a_pT"], [kbk])
                        for hh in range(2):
                            rows = slice(hh * 64, (hh + 1) * 64)
                            am, amk = Am.next()
                            pa_, pak_ = pA.next()
                            cx.mm(pa_[:], P["kt"][rows, ts_], P["qt"][rows, ts_], True, True, [P["ktk"], P["qtk"]], [pak_])
                            cx.tt("dve", am[:], pa_[:], mk[d][:], ALU.mult, [pak_, f"a_mk{d}"], [amk])
                            pot, pok = po[hh]
                            cx.mm(pot[:, ts_], P["vt"][:, tt_, hh * 128:(hh + 1) * 128], am[:], True, False, [P["vk"], amk], [pok])
                            corder = (0, 1) if d == 0 else (1, 0)
                            for ci, c in enumerate(corder):
                                cs = slice(tt_ * 128 + c * 64, tt_ * 128 + (c + 1) * 64)
                                cx.mm(pot[:, cs], S[rows, :], P["qd"][rows, cs], False, ci == 1, ["a_S", P["qdk"]], [pok])
                                crow = slice(c * 64, (c + 1) * 64)
                                cx.mm(pKV[rows, :], kb_[crow, hh * 64:(hh + 1) * 64], P["vt"][crow, tt_, hh * 128:(hh + 1) * 128],
                                      True, True, [kbk, P["vk"]], ["a_pKV"])
                                dcol = tt_ * 128 + c * 64 + last
                                cx.stt("dve", S[rows, :], S[rows, :], P["E3"][rows, dcol:dcol + 1], pKV[rows, :], ALU.mult, ALU.add,
                                       ["a_S", P["E3k"], "a_pKV"], ["a_S"])
                    for hh in range(2):
                        h = rt * 2 + hh
                        pot, pok = po[hh]
                        o_, ok_ = osb.next()
                        if d == 0:
                            cx.cp("act", o_[:], pot[:], [pok], [ok_])
                            cx.dma("sp", ok_, sc["OFW"][h, :, c0:c0 + 512], o_[:], [ok_], [])
                        else:
                            f_, fk_ = P["ofw"][hh]
                            cx.tt("dve", o_[:], pot[:], f_[:], ALU.add, [pok, fk_], [ok_])
                            s_, sk_ = sq.next()
                            cx.act(s_[:], o_[:], AF.Square, [ok_], [sk_])
                            cx.mm(pN[:], g.onesb[:], s_[:], True, True, ["onesb", sk_], ["a_pZ"])
                            r_, rk_ = rl.next()
                            cx.act(r_[:], pN[:], AF.Sqrt, ["a_pZ"], [rk_], bias=EPS, scale=1.0 / 128)
                            cx.recip(r_[:], r_[:], [rk_], [rk_])
                            cx.stt("dve", o_[:], o_[:], nwc[:, 0:1], r_[:], ALU.mult, ALU.mult, [ok_, "a_nwc", rk_], [ok_])
                            y_, yk_ = yb.next()
                            g_, gk_ = P["gr"][hh]
                            cx.tt("pool", y_[:], o_[:], g_[:], ALU.mult, [ok_, gk_], [yk_])
                            cx.dma("sp", yk_, sc["YT"][0, h, :, c0:c0 + 512], y_[:], [yk_], [])

                gates(blocks[0])
                for bi, b in enumerate(blocks):
                    if bi + 1 < len(blocks):
                        gates(blocks[bi + 1])
                    tiles(b)
            if d == 0:
                cx.S.barrier()
        cx.phase_end()


def gdn_masks():
    j = np.arange(128)[:, None]
    i = np.arange(128)[None, :]
    same = (j // 64) == (i // 64)
    sm_f = (same & (j < i)).astype(np.float32)
    sm_b = (same & (j > i)).astype(np.float32)
    m_same = same.astype(np.float32)
    m_c0 = np.broadcast_to((j < 64), (128, 128)).astype(np.float32)
    m_c1 = np.broadcast_to((j >= 64), (128, 128)).astype(np.float32)
    return sm_f, sm_b, m_same, np.ascontiguousarray(m_c0), np.ascontiguousarray(m_c1)


GDN_STAGES = 4
GDN_SB_STEPS = 10 ** 6
GDN_AUX = "dve"


def phase_gdn(cx, g, dr, layer, sc):
    nc = cx.nc
    NT = T // 128
    HALF = NT // 2
    with ExitStack() as st0:
        msk = {}
        for nm in ("mask_f", "mask_b", "sm_f", "sm_b", "m_same", "m_c0", "m_c1"):
            msk[nm] = cx.sb(st0, f"n_{nm}", [128, 128], F32)
            cx.dma("sp", f"n_{nm}", msk[nm][:], dr[nm], [], [f"n_{nm}"])
        GLs = cx.sb(st0, "n_GL", [128, NT, 16], F32)
        gt = {nm: cx.sb(st0, f"n_{nm}", [128, NT, 8], F32) for nm in
              ("g", "beta", "Bc", "eB", "eEnd", "dec0", "dec1", "nbeta", "bEB", "tmp")}
        dtb = cx.sb(st0, "n_dtb", [128, 8], F32)
        nA = cx.sb(st0, "n_nA", [128, 8], F32)
        nwc = cx.sb(st0, "n_nwc", [128, 1], F32)
        with ExitStack() as st:
            pg = cx.ps(st, "n_pg", [128, NT, 4])
            cx.dma("sp", "n_GL", GLs[:], sc["GL"].rearrange("(n p) c -> p n c", p=128), [], ["n_GL"])
            cx.dma("sp", "n_c0", dtb[:], dr["gdn_dt_bias"][layer:layer + 1].rearrange("o a b -> o (a b)")[0].partition_broadcast(128), [], ["n_dtb"])
            cx.dma("sp", "n_c1", nA[:], dr["gdn_A_log"][layer:layer + 1].rearrange("o a b -> o (a b)")[0].partition_broadcast(128), [], ["n_nA"])
            cx.dma("sp", "n_c2", nwc[:], dr["gdn_norm"][layer].rearrange("(p o) -> p o", o=1), [], ["n_nwc"])
            cx.act(nA[:], nA[:], AF.Exp, ["n_nA"], ["n_nA"])
            cx.ts("dve", nA[:], nA[:], -1.0, None, ALU.mult, None, ["n_nA"], ["n_nA"])
            bc = lambda t: t[:].unsqueeze(1).to_broadcast([128, NT, 8])
            cx.tt("dve", gt["tmp"][:], GLs[:, :, 0:8], bc(dtb), ALU.add, ["n_GL", "n_dtb"], ["n_tmp"])
            cx.act(gt["tmp"][:], gt["tmp"][:], AF.Exp, ["n_tmp"], ["n_tmp"])
            cx.act(gt["tmp"][:], gt["tmp"][:], AF.Ln, ["n_tmp"], ["n_tmp"], bias=1.0)
            cx.tt("dve", gt["g"][:], gt["tmp"][:], bc(nA), ALU.mult, ["n_tmp", "n_nA"], ["n_g"])
            cx.act(gt["beta"][:], GLs[:, :, 8:16], AF.Sigmoid, ["n_GL"], ["n_beta"])
            cx.ts("dve", gt["nbeta"][:], gt["beta"][:], -1.0, None, ALU.mult, None, ["n_beta"], ["n_nbeta"])
            for d in range(2):
                cs = slice(d * 4, (d + 1) * 4)
                um = msk["mask_f"] if d == 0 else msk["mask_b"]
                for (mm_, dst) in ((um, "Bc"), (msk["m_same"], "eEnd"), (msk["m_c0"], "dec0"), (msk["m_c1"], "dec1")):
                    cx.mm(pg[:], mm_[:], gt["g"][:, :, cs], True, True, [f"n_{'mask_f' if d == 0 else 'mask_b'}", "n_m_same", "n_m_c0", "n_m_c1", "n_g"], ["n_pg"])
                    cx.cp("dve", gt[dst][:, :, cs], pg[:], ["n_pg"], [f"n_{dst}"])
            cx.tt("dve", gt["eEnd"][:], gt["eEnd"][:], gt["Bc"][:], ALU.subtract, ["n_eEnd", "n_Bc"], ["n_eEnd"])
            cx.act(gt["eEnd"][:], gt["eEnd"][:], AF.Exp, ["n_eEnd"], ["n_eEnd"])
            cx.act(gt["eB"][:], gt["Bc"][:], AF.Exp, ["n_Bc"], ["n_eB"])
            cx.act(gt["dec0"][:], gt["dec0"][:], AF.Exp, ["n_dec0"], ["n_dec0"])
            cx.act(gt["dec1"][:], gt["dec1"][:], AF.Exp, ["n_dec1"], ["n_dec1"])
            cx.tt("dve", gt["bEB"][:], gt["beta"][:], gt["eB"][:], ALU.mult, ["n_beta", "n_eB"], ["n_bEB"])
            cx.S.barrier()
            cx.S.emit()
        if GDN_STAGES < 2:
            return

        with ExitStack() as st:
            cw = cx.sb(st, "n_cw", [4, 1536], F32)
            cwT = cx.sb(st, "n_cwT", [128, 12, 4], F32)
            dw = cx.sb(st, "n_dw", [128, 12, 4, 128], BF16)
            xin = Rot(cx, st, "n_xin", [128, TP], BF16, 2)
            sS = Rot(cx, st, "n_s", [128, 512], F32, 2)
            sQ = Rot(cx, st, "n_sq", [128, 512], BF16, 2)
            rR = Rot(cx, st, "n_r", [128, 512], F32, 2)
            stg = Rot(cx, st, "n_stg", [128, 512], BF16, 3)
            tms = Rot(cx, st, "n_tms", [128, 4, 128], BF16, 3)
            pc = Rot(cx, st, "n_pc", [128, 512], F32, 2, psum=True)
            pn = Rot(cx, st, "n_pn", [128, 512], F32, 2, psum=True)
            pt = Rot(cx, st, "n_pt", [128, 4, 128], F32, 2, psum=True)
            pw_ = cx.ps(st, "n_pw", [128, 12, 4])
            cx.dma("sp", "n_cw", cw[:], dr["gdn_conv"][layer], [], ["n_cw"])
            for ct in range(12):
                cx.tr(pw_[:, ct, :], cw[0:4, ct * 128:(ct + 1) * 128], g.ident[0:4, 0:4], ["n_cw", "ident"], ["n_pw"])
            cx.cp("dve", cwT[:], pw_[:], ["n_pw"], ["n_cwT"])
            for ct in range(12):
                for w in range(4):
                    cx.ts(GDN_AUX if (ct + w) % 2 else "dve", dw[:, ct, w, :], g.identb[:], cwT[:, ct, w:w + 1], None, ALU.mult, None,
                          ["identb", "n_cwT"], ["n_dw"])
            for h in range(4):
                for kind in range(3):
                    ct = kind * 4 + h
                    xt, xk = xin.next()
                    cx.dma("sp", xk, xt[:], sc["FM"][FM_NC + ct], [], [xk])
                    if kind < 2:
                        dst = sc["GQT"] if kind == 0 else sc["GKT"]
                        scale = (128 ** -0.5) if kind == 0 else 1.0
                        for ch in range(T // 512):
                            t0 = ch * 512
                            p_, pk_ = pc.next()
                            for w in range(4):
                                cx.mm(p_[:], dw[:, ct, w, :], xt[:, OFF - 1 + w + t0:OFF - 1 + w + t0 + 512], w == 0, w == 3, ["n_dw", xk], [pk_])
                            s_, sk_ = sS.next()
                            cx.act(s_[:], p_[:], AF.Silu, [pk_], [sk_])
                            q_, qk_ = sQ.next()
                            cx.tt(GDN_AUX, q_[:], s_[:], s_[:], ALU.mult, [sk_], [qk_])
                            n_, nk_ = pn.next()
                            cx.mm(n_[:], g.onesb[:], q_[:], True, True, ["onesb", qk_], [nk_])
                            r_, rk_ = rR.next()
                            cx.act(r_[:], n_[:], AF.Sqrt, [nk_], [rk_], bias=EPS, scale=1.0)
                            cx.recip(r_[:], r_[:], [rk_], [rk_])
                            o_, ok_ = stg.next()
                            cx.stt("dve", o_[:], s_[:], scale, r_[:], ALU.mult, ALU.mult, [sk_, rk_], [ok_])
                            cx.dma("sp", ok_, dst[h, :, t0:t0 + 512], o_[:], [ok_], [])
                            if kind == 1:
                                t_, tk_ = pt.next()
                                for q in range(4):
                                    cx.mm(t_[:, q, :], o_[:, q * 128:(q + 1) * 128], g.identb[:], True, True, [ok_, "identb"], [tk_])
                                m_, mk_ = tms.next()
                                cx.cp("act", m_[:], t_[:], [tk_], [mk_])
                                cx.dma("sp", mk_, sc["GKM"][h, t0:t0 + 512, :].rearrange("(n p) c -> p n c", p=128), m_[:], [mk_], [])
                    else:
                        for ch in range(T // 512):
                            t0 = ch * 512
                            t_, tk_ = pt.next()
                            for q in range(4):
                                for w in range(4):
                                    c0 = OFF - 1 + w + t0 + q * 128
                                    cx.mm(t_[:, q, :], xt[:, c0:c0 + 128], dw[:, ct, w, :], w == 0, w == 3, [xk, "n_dw"], [tk_])
                            m_, mk_ = tms.next()
                            cx.act(m_[:], t_[:], AF.Silu, [tk_], [mk_])
                            cx.dma("sp", mk_, sc["GVM"][h, t0:t0 + 512, :].rearrange("(n p) c -> p n c", p=128), m_[:], [mk_], [])
            cx.S.barrier()
            cx.S.emit()
        if GDN_STAGES < 3:
            return

        NI = min(8, NT)
        with ExitStack() as st:
            bank = [cx.ps(st, f"n_bk{i}", [128, 128]) for i in range(NI)]
            bkk = [f"n_bk{i}" for i in range(NI)]
            qT8 = Rot(cx, st, "n_q8", [128, NI * 128], BF16, 2)
            kT8 = Rot(cx, st, "n_k8", [128, NI * 128], BF16, 2)
            km8 = Rot(cx, st, "n_km8", [128, NI, 128], BF16, 2)
            vm8 = Rot(cx, st, "n_vm8", [128, NI, 128], BF16, 2)
            outp = Rot(cx, st, "n_out", [128, NI, 4, 128], BF16, 2)

            def ibuf(nm, dt):
                ts_ = [cx.sb(st, f"n_{nm}{i}", [128, 128], dt) for i in range(NI)]
                return ts_, [f"n_{nm}#{i}" for i in range(NI)]
            Lg, Lgk = ibuf("Lg", F32)
            Ge, Gek = ibuf("Ge", F32)
            GM, GMk = ibuf("GM", F32)
            GI, GIk = ibuf("GI", F32)
            Tm = [ibuf(f"Tm{j}", BF16) for j in range(2)]
            Ym = [ibuf(f"Ym{j}", BF16) for j in range(2)]
            Xm, Xk = ibuf("X", BF16)
            at, atk = ibuf("at", BF16)
            bv, bvk = ibuf("bv", BF16)
            rw, rwk = ibuf("rw", BF16)
            for h in range(4):
                for d in range(2):
                    col = d * 4 + h
                    LM, LMk = (msk["mask_f"], "n_mask_f") if d == 0 else (msk["mask_b"], "n_mask_b")
                    SM, SMk = (msk["sm_b"], "n_sm_b") if d == 0 else (msk["sm_f"], "n_sm_f")
                    for n0 in range(0, NT, NI):
                        q8, q8k = qT8.next()
                        k8, k8k = kT8.next()
                        km, kmk = km8.next()
                        vm, vmk = vm8.next()
                        op_, opk = outp.next()
                        tsl = slice(n0 * 128, (n0 + NI) * 128)
                        cx.dma("sp", q8k, q8[:], sc["GQT"][h, :, tsl], [], [q8k])
                        cx.dma("sp", k8k, k8[:], sc["GKT"][h, :, tsl], [], [k8k])
                        cx.dma("sp", kmk, km[:], sc["GKM"][h, tsl, :].rearrange("(n p) c -> p n c", p=128), [], [kmk])
                        cx.dma("sp", vmk, vm[:], sc["GVM"][h, tsl, :].rearrange("(n p) c -> p n c", p=128), [], [vmk])
                        R_ = range(NI)
                        tl = lambda i: slice(i * 128, (i + 1) * 128)
                        for i in R_:
                            cx.ts("dve", Lg[i][:], LM[:], gt["g"][:, n0 + i, col:col + 1], None, ALU.mult, None, [LMk, "n_g"], [Lgk[i]])
                        for i in R_:
                            cx.mm(bank[i][:], Lg[i][:], SM[:], True, True, [Lgk[i], SMk], [bkk[i]])
                        for i in R_:
                            cx.act(Ge[i][:], bank[i][:], AF.Exp, [bkk[i]], [Gek[i]])
                        for i in R_:
                            cx.tt(GDN_AUX, GM[i][:], Ge[i][:], SM[:], ALU.mult, [Gek[i], SMk], [GMk[i]])
                        for i in R_:
                            cx.tt(GDN_AUX, GI[i][:], GM[i][:], g.ident[:], ALU.add, [GMk[i], "ident"], [GIk[i]])
                        for i in R_:
                            cx.mm(bank[i][:], k8[:, tl(i)], k8[:, tl(i)], True, True, [k8k], [bkk[i]])
                        T0, T0k = Tm[0]
                        for i in R_:
                            cx.stt("dve", T0[i][:], bank[i][:], gt["nbeta"][:, n0 + i, col:col + 1], GM[i][:], ALU.mult, ALU.mult,
                                   [bkk[i], "n_nbeta", GMk[i]], [T0k[i]])
                        for i in R_:
                            cx.mm(bank[i][:], q8[:, tl(i)], k8[:, tl(i)], True, True, [q8k, k8k], [bkk[i]])
                        for i in R_:
                            cx.tt("dve", at[i][:], bank[i][:], GI[i][:], ALU.mult, [bkk[i], GIk[i]], [atk[i]])
                        for i in R_:
                            cx.mm(bank[i][:], at[i][:], g.identb[:], True, True, [atk[i], "identb"], [bkk[i]])
                        for i in R_:
                            cx.cp("act", op_[:, i, 2, :], bank[i][:], [bkk[i]], [opk])
                        Y0, Y0k = Ym[0]
                        for i in R_:
                            cx.mm(bank[i][:], T0[i][:], g.identb[:], True, True, [T0k[i], "identb"], [bkk[i]])
                        for i in R_:
                            cx.cp("act", Y0[i][:], bank[i][:], [bkk[i]], [Y0k[i]])
                        for i in R_:
                            cx.tt(GDN_AUX, Xm[i][:], Y0[i][:], g.identb[:], ALU.add, [Y0k[i], "identb"], [Xk[i]])
                        cur = 0
                        for it in range(5):
                            Tc, Tck = Tm[cur]
                            Yc, Yck = Ym[cur]
                            Tn, Tnk = Tm[1 - cur]
                            Yn, Ynk = Ym[1 - cur]
                            for i in R_:
                                cx.mm(bank[i][:], Yc[i][:], Tc[i][:], True, True, [Yck[i], Tck[i]], [bkk[i]])
                            for i in R_:
                                cx.cp("act", Tn[i][:], bank[i][:], [bkk[i]], [Tnk[i]])
                            if it < 4:
                                for i in R_:
                                    cx.mm(bank[i][:], Tc[i][:], Yc[i][:], True, True, [Yck[i], Tck[i]], [bkk[i]])
                                for i in R_:
                                    cx.cp("dve", Yn[i][:], bank[i][:], [bkk[i]], [Ynk[i]])
                            for i in R_:
                                cx.mm(bank[i][:], Tn[i][:], Xm[i][:], True, True, [Tnk[i], Xk[i]], [bkk[i]])
                            for i in R_:
                                cx.tt("dve", Xm[i][:], Xm[i][:], bank[i][:], ALU.add, [Xk[i], bkk[i]], [Xk[i]])
                            cur = 1 - cur
                        for i in R_:
                            cx.ts(GDN_AUX, bv[i][:], vm[:, i, :], gt["beta"][:, n0 + i, col:col + 1], None, ALU.mult, None, [vmk, "n_beta"], [bvk[i]])
                        for i in R_:
                            cx.ts(GDN_AUX, rw[i][:], km[:, i, :], gt["bEB"][:, n0 + i, col:col + 1], None, ALU.mult, None, [kmk, "n_bEB"], [rwk[i]])
                        for i in R_:
                            cx.ts(GDN_AUX, op_[:, i, 3, :], km[:, i, :], gt["eEnd"][:, n0 + i, col:col + 1], None, ALU.mult, None, [kmk, "n_eEnd"], [opk])
                        for i in R_:
                            cx.mm(bank[i][:], Xm[i][:], bv[i][:], True, True, [Xk[i], bvk[i]], [bkk[i]])
                        for i in R_:
                            cx.cp("act", op_[:, i, 0, :], bank[i][:], [bkk[i]], [opk])
                        for i in R_:
                            cx.mm(bank[i][:], rw[i][:], Xm[i][:], True, True, [rwk[i], Xk[i]], [bkk[i]])
                        for i in R_:
                            cx.cp("dve", op_[:, i, 1, :], bank[i][:], [bkk[i]], [opk])
                        cx.dma("sp", opk, sc["GA"][h, d, n0:n0 + NI].rearrange("n p m c -> p n m c"), op_[:], [opk], [])
            cx.S.barrier()
            cx.S.emit()
        if GDN_STAGES < 4:
            return

        with ExitStack() as st:
            chains = [(h, d) for h in range(4) for d in range(2)]
            bank = [cx.ps(st, f"n_sbk{i}", [128, 4, 128]) for i in range(8)]
            bkk = [f"n_sbk{i}" for i in range(8)]
            Sst = [cx.sb(st, f"n_S{i}", [128, 128], BF16) for i in range(8)]
            Sk = [f"n_S{i}" for i in range(8)]
            ga = [Rot(cx, st, f"n_ga{i}_", [128, 4, 128], BF16, 2) for i in range(8)]
            qt = [Rot(cx, st, f"n_qt{i}_", [128, 128], BF16, 2) for i in range(8)]
            vn = [cx.sb(st, f"n_vn{i}", [128, 128], BF16) for i in range(8)]
            vnk = [f"n_vn{i}" for i in range(8)]
            oi = [cx.sb(st, f"n_oi{i}", [128, 128], F32) for i in range(8)]
            oik = [f"n_oi{i}" for i in range(8)]
            oo = [Rot(cx, st, f"n_oo{i}_", [128, 128], F32, 2) for i in range(8)]
            par = [Rot(cx, st, f"n_par{i}_", [128, 128], F32, 2) for i in range(8)]
            nz = [Rot(cx, st, f"n_nz{i}_", [128, 128], BF16, 2) for i in range(8)]
            on = [cx.sb(st, f"n_on{i}", [128, 128], BF16) for i in range(8)]
            onk = [f"n_on{i}" for i in range(8)]
            sj = [cx.sb(st, f"n_sj{i}", [128, 128], BF16) for i in range(8)]
            ssq = [cx.sb(st, f"n_ss{i}", [128, 2], F32) for i in range(8)]
            ssk = [f"n_ss{i}" for i in range(8)]
            yy = [Rot(cx, st, f"n_yy{i}_", [128, 128], BF16, 2) for i in range(8)]
            for i in range(8):
                cx.memset("pool", Sst[i][:], 0.0, [Sk[i]])
            for s in range(min(NT, GDN_SB_STEPS)):
                second = s >= HALF
                cur = []
                for ci, (h, d) in enumerate(chains):
                    n = s if d == 0 else NT - 1 - s
                    par2 = s % 2
                    ga_, gak = ga[ci].next()
                    cx.dma("sp", f"n_ga_{par2}", ga_[:], sc["GA"][h, d, n], [], [gak])
                    q_, qk_ = qt[ci].next()
                    cx.dma("sp", f"n_qt_{par2}", q_[:], sc["GQT"][h, :, n * 128:(n + 1) * 128], [], [qk_])
                    ex = None
                    if second:
                        p_, pk_ = par[ci].next()
                        cx.dma("sp", f"n_par_{par2}", p_[:], sc["GO"][h, n * 128:(n + 1) * 128, :], [("GO", h, n)], [pk_])
                        z_, zk_ = nz[ci].next()
                        cx.dma("sp", f"n_nz_{par2}", z_[:], sc["FM"][FM_NZ + h, :, OFF + n * 128:OFF + (n + 1) * 128], [], [zk_])
                        ex = (p_, pk_, z_, zk_)
                    cur.append((h, d, n, ga_, gak, q_, qk_, ex))
                for step in range(2):
                    for ci, (h, d, n, ga_, gak, q_, qk_, ex) in enumerate(cur):
                        c = step if d == 0 else 1 - step
                        cr = slice(c * 64, (c + 1) * 64)
                        col = d * 4 + h
                        B = bank[ci]
                        cx.mm(B[cr, 0, :], ga_[:, 1, cr], Sst[ci][:], True, True, [gak, Sk[ci]], [bkk[ci]])
                        cx.mm(B[cr, 1, :], q_[:, cr], Sst[ci][:], True, True, [qk_, Sk[ci]], [bkk[ci]])
                    for ci, (h, d, n, ga_, gak, q_, qk_, ex) in enumerate(cur):
                        c = step if d == 0 else 1 - step
                        cr = slice(c * 64, (c + 1) * 64)
                        cx.tt("dve", vn[ci][cr, :], ga_[cr, 0, :], bank[ci][cr, 0, :], ALU.subtract, [gak, bkk[ci]], [vnk[ci]])
                    for ci, (h, d, n, ga_, gak, q_, qk_, ex) in enumerate(cur):
                        c = step if d == 0 else 1 - step
                        cr = slice(c * 64, (c + 1) * 64)
                        B = bank[ci]
                        cx.mm(B[cr, 2, :], ga_[cr, 2, cr], vn[ci][cr, :], True, True, [gak, vnk[ci]], [bkk[ci]])
                        cx.mm(B[:, 3, :], ga_[cr, 3, :], vn[ci][cr, :], True, True, [gak, vnk[ci]], [bkk[ci]])
                    for ci, (h, d, n, ga_, gak, q_, qk_, ex) in enumerate(cur):
                        c = step if d == 0 else 1 - step
                        col = d * 4 + h
                        dec = gt["dec0"] if c == 0 else gt["dec1"]
                        cx.stt("dve", Sst[ci][:], Sst[ci][:], dec[:, n, col:col + 1], bank[ci][:, 3, :], ALU.mult, ALU.add,
                               [Sk[ci], f"n_dec{c}", bkk[ci]], [Sk[ci]])
                for ci, (h, d, n, ga_, gak, q_, qk_, ex) in enumerate(cur):
                    col = d * 4 + h
                    cx.ts("dve", oi[ci][:], bank[ci][:, 1, :], gt["eB"][:, n, col:col + 1], None, ALU.mult, None, [bkk[ci], "n_eB"], [oik[ci]])
                for ci, (h, d, n, ga_, gak, q_, qk_, ex) in enumerate(cur):
                    o_, ok_ = oo[ci].next()
                    cx.tt("dve", o_[:], bank[ci][:, 2, :], oi[ci][:], ALU.add, [bkk[ci], oik[ci]], [ok_])
                    if not second:
                        cx.dma("sp", f"n_oo_{s % 2}", sc["GO"][h, n * 128:(n + 1) * 128, :], o_[:], [ok_], [("GO", h, n)])
                    else:
                        p_, pk_, z_, zk_ = ex
                        cx.tt(GDN_AUX, o_[:], o_[:], p_[:], ALU.add, [ok_, pk_], [ok_])
                        cx.act(sj[ci][:], o_[:], AF.Square, [ok_], [ssk[ci]], accum_out=ssq[ci][:, 0:1])
                        cx.rstd(ssq[ci][:, 1:2], ssq[ci][:, 0:1], 1.0 / 128, ssk[ci])
                        cx.ts("dve", on[ci][:], o_[:], ssq[ci][:, 1:2], None, ALU.mult, None, [ok_, ssk[ci]], [onk[ci]])
                        cx.mm(bank[ci][:, 0, :], on[ci][:], g.identb[:], True, True, [onk[ci], "identb"], [bkk[ci]])
                        y_, yk_ = yy[ci].next()
                        cx.stt("dve", y_[:], bank[ci][:, 0, :], nwc[:, 0:1], z_[:], ALU.mult, ALU.mult, [bkk[ci], "n_nwc", zk_], [yk_])
                        cx.dma("sp", f"n_yy_{s % 2}", sc["YT"][2, h, :, n * 128:(n + 1) * 128], y_[:], [yk_], [])
            cx.phase_end()


def phase_post(cx, g, dr, layer, sc, x_in, x_out):
    nc = cx.nc
    with ExitStack() as st:
        wb = cx.sb(st, "o_wb", [128, 3, 4, D], BF16)
        wo = cx.sb(st, "o_wo", [128, KT, D], BF16)
        for n in range(3):
            cx.dma("pool", "o_wb", wb[:, n], dr["w_branch"][layer, n].rearrange("(k p) d -> p k d", p=128), [], ["o_wb"])
        cx.dma("pool", "o_wo", wo[:], dr["w_out"][layer].rearrange("(k p) d -> p k d", p=128), [], ["o_wo"])
        yT = Rot(cx, st, "o_yT", [128, 12, 512], BF16, 2)
        gT = Rot(cx, st, "o_gT", [128, 24, 512], BF16, 2)
        mT = Rot(cx, st, "o_mT", [128, KT, 512], BF16, 2)
        tmp = Rot(cx, st, "o_tmp", [128, 512], F32, 4)
        acc = Rot(cx, st, "o_acc", [128, 512], F32, 2)
        pb = Rot(cx, st, "o_pb", [128, 512], F32, 4, psum=True)
        po = Rot(cx, st, "o_po", [128, 512], F32, 2, psum=True)
        xr = Rot(cx, st, "o_xr", [128, D], F32, 2)
        yo = Rot(cx, st, "o_yo", [128, D], F32, 2)
        fs = Rot(cx, st, "o_fs", [128, 2], F32, 2)
        for ch in range(T // 512):
            t0 = ch * 512
            y_, yk = yT.next()
            cx.dma("sp", yk, y_[:].rearrange("p (n k) t -> p n k t", n=3),
                   sc["YT"][:, :, :, t0:t0 + 512].rearrange("n k p t -> p n k t"), [], [yk])
            g_, gk = gT.next()
            cx.dma("sp", gk, g_[:], sc["FM"][FM_MG:FM_MG + 24, :, OFF + t0:OFF + t0 + 512].rearrange("a p t -> p a t"), [], [gk])
            m_, mk_ = mT.next()
            for dt_ in range(KT):
                a_, ak = acc.next()
                for n in range(3):
                    p_, pk = pb.next()
                    for k in range(4):
                        cx.mm(p_[:], wb[:, n, k, dt_ * 128:(dt_ + 1) * 128], y_[:, n * 4 + k, :], k == 0, k == 3, ["o_wb", yk], [pk])
                    gsl = g_[:, n * 8 + dt_, :]
                    if n == 0:
                        cx.tt("dve", a_[:], p_[:], gsl, ALU.mult, [pk, gk], [ak])
                    else:
                        t_, tk = tmp.next()
                        cx.tt("dve", t_[:], p_[:], gsl, ALU.mult, [pk, gk], [tk])
                        if n == 1:
                            cx.tt("pool", a_[:], a_[:], t_[:], ALU.add, [ak, tk], [ak])
                        else:
                            cx.tt("pool", m_[:, dt_, :], a_[:], t_[:], ALU.add, [ak, tk], [mk_])
            for s in range(4):
                rows = slice(t0 + s * 128, t0 + (s + 1) * 128)
                x_, xk = xr.next()
                cx.dma("sp", xk, x_[:], x_in[rows, :], [], [xk])
                o_, ok = yo.next()
                f_, fk = fs.next()
                ps_ = []
                for half in range(2):
                    p_, pk = po.next()
                    for k in range(KT):
                        cx.mm(p_[:], m_[:, k, s * 128:(s + 1) * 128], wo[:, k, half * 512:(half + 1) * 512], k == 0, k == KT - 1,
                              [mk_, "o_wo"], [pk])
                    ps_.append((p_, pk))
                    cx.cp("act", o_[:, half * 512:(half + 1) * 512], p_[:], [pk], [ok])
                cx.act(xr_junk(cx, st)[:], o_[:], AF.Square, [ok], [fk], accum_out=f_[:, 0:1])
                cx.rstd(f_[:, 1:2], f_[:, 0:1], 1.0 / D, fk)
                cx.stt("dve", o_[:], o_[:], f_[:, 1:2], g.mod[:, 2, :], ALU.mult, ALU.mult, [ok, fk, ("mod", 2)], [ok])
                cx.tt("pool", o_[:], o_[:], x_[:], ALU.add, [ok, xk], [ok])
                cx.dma("sp", ok, x_out[rows, :], o_[:], [ok], [])
        cx.phase_end()


_JUNK = {}


def xr_junk(cx, st):
    k = id(st)
    if k not in _JUNK:
        _JUNK.clear()
        _JUNK[k] = cx.sb(st, "junk", [128, D], BF16)
    return _JUNK[k]


N_CORES = 8
DEPTH = 2
PHASE_LIMIT = 99
PHASE_SKIP = ()


def build_program():
    nc = bass.Bass("TRN2", target_bir_lowering=False)
    dr = {}

    def inp(name, shape, dt=F32):
        dr[name] = nc.dram_tensor(name, list(shape), dt, kind="ExternalInput").ap()

    inp("x", [T, D]); inp("cT", [128, KT]); inp("positions", [1, T], I32)
    inp("adaln_w", [DEPTH, D, 6 * D]); inp("adaln_b", [DEPTH, 6 * D]); inp("norm_w", [DEPTH, 4, D])
    inp("w_fm", [DEPTH, D, NFMG * 512]); inp("w_tm", [DEPTH, D, NTM])
    inp("gla_gate_up", [DEPTH, 2, 16, 256]); inp("gla_gate_bias", [DEPTH, 2, 256]); inp("gla_norm", [DEPTH, 128])
    inp("diff_lambda", [DEPTH, 4, 64]); inp("diff_norm", [DEPTH, 128])
    inp("gdn_conv", [DEPTH, 4, 1536]); inp("gdn_A_log", [DEPTH, 2, 4]); inp("gdn_dt_bias", [DEPTH, 2, 4]); inp("gdn_norm", [DEPTH, 128])
    inp("w_branch", [DEPTH, 3, 512, D]); inp("w_out", [DEPTH, D, D])
    inp("ffn_w1", [1, D, D_FF]); inp("ffn_w3", [1, D, D_FF]); inp("ffn_w2", [1, D_FF, D])
    inp("router_w", [1, D, N_EXP]); inp("moe_w1", [1, N_EXP, D, D_EXP]); inp("moe_w3", [1, N_EXP, D, D_EXP]); inp("moe_w2", [1, N_EXP, D_EXP, D])
    for nm in ("ident", "mask_f", "mask_b", "sm_f", "sm_b", "m_same", "m_c0", "m_c1"):
        inp(nm, [128, 128])
    inp("reset64", [128, 512]); inp("rot_inv", [128, 1]); inp("rot_sgn", [128, 1])
    inp("tokidx", [128, (T // 2) // 128], I32)
    out = nc.dram_tensor("out", [T // 2, D], F32, kind="ExternalOutput").ap()

    sc = {}

    def scr(name, shape, dt):
        sc[name] = nc.dram_tensor(name, list(shape), dt, kind="Internal").ap()

    NT = T // 128
    PAGE_EL = 268435456 // 2
    arena = nc.dram_tensor("arenaA", [PAGE_EL], BF16, kind="Internal").ap()
    off = [0]

    def carve(name, shape, pattern, **kw):
        n = int(np.prod(shape))
        sc[name] = arena[off[0]:off[0] + n].rearrange(pattern, **kw)
        off[0] += n
        assert off[0] <= PAGE_EL

    carve("FM", [NFM, 128, TP], "(a p t) -> a p t", a=NFM, p=128)
    carve("TM", [T, NTM], "(t c) -> t c", c=NTM)
    carve("YT", [3, 4, 128, T], "(n k p t) -> n k p t", n=3, k=4, p=128)
    carve("GQT", [4, 128, T], "(h p t) -> h p t", h=4, p=128)
    carve("GKT", [4, 128, T], "(h p t) -> h p t", h=4, p=128)
    carve("GKM", [4, T, 128], "(h t c) -> h t c", h=4, c=128)
    carve("GVM", [4, T, 128], "(h t c) -> h t c", h=4, c=128)
    scr("GA", [4, 2, NT, 128, 4, 128], BF16)
    scr("GL", [T, 16], F32); scr("OFW", [4, 128, T], F32); scr("GO", [4, T, 128], F32)
    scr("XA", [T, D], F32); scr("XB", [T, D], F32)

    with ExitStack() as st:
        cx = Ctx(nc, st)
        g = setup_globals(cx, st, dr)
        nph = 0
        for layer in range(DEPTH):
            x_in = dr["x"] if layer == 0 else sc["XB"]
            x_out = sc["XB"] if layer == 0 else out
            if layer % 2 == 0:
                ex = [(dr["ffn_w1"][layer // 2], dr["ffn_w3"][layer // 2], dr["ffn_w2"][layer // 2])]
                rw = None
            else:
                ex = [(dr["moe_w1"][layer // 2, e], dr["moe_w3"][layer // 2, e], dr["moe_w2"][layer // 2, e]) for e in range(N_EXP)]
                rw = dr["router_w"][layer // 2]
            phases = [
                lambda: phase_mod(cx, g, dr, layer),
                lambda: phase_proj(cx, g, dr, x_in, layer, sc),
                lambda: phase_diff(cx, g, dr, layer, sc),
                lambda: phase_gla(cx, g, dr, layer, sc),
                lambda: phase_gdn(cx, g, dr, layer, sc),
                lambda: phase_post(cx, g, dr, layer, sc, x_in, sc["XA"]),
                (lambda: phase_ffn(cx, g, dr, sc["XA"], x_out, ex, rw)) if layer < DEPTH - 1 else
                (lambda: phase_ffn(cx, g, dr, sc["XA"], x_out, ex, rw, tok_range=(0, T // 2), tokidx=dr["tokidx"])),
            ]
            for ph in phases:
                if nph < PHASE_LIMIT and nph not in PHASE_SKIP:
                    ph()
                nph += 1
    return nc


def kernel(x, c, positions, adaln_w, adaln_b, norm_w, w_in, gla_gate_up, gla_gate_bias, gla_norm,
           diff_lambda, diff_norm, gdn_conv, gdn_A_log, gdn_dt_bias, gdn_norm, w_branch, w_out,
           ffn_w1, ffn_w3, ffn_w2, router_w, moe_w1, moe_w3, moe_w2):
    f = lambda a: np.ascontiguousarray(np.asarray(a, dtype=np.float32))
    x = f(x); c = f(c)
    positions = np.ascontiguousarray(np.asarray(positions).astype(np.int32))
    w_in = f(w_in)
    packed = [pack_w_in(w_in[l]) for l in range(DEPTH)]
    w_fm = np.stack([p[0] for p in packed])
    w_tm = np.stack([p[1] for p in packed])
    mf, mb, rs64 = chunk_masks()
    sm_f, sm_b, m_same, m_c0, m_c1 = gdn_masks()
    inv, sgn = rot_consts()
    shared = {
        "adaln_w": f(adaln_w), "adaln_b": f(adaln_b), "norm_w": f(norm_w), "w_fm": w_fm, "w_tm": w_tm,
        "gla_gate_up": f(gla_gate_up), "gla_gate_bias": f(gla_gate_bias), "gla_norm": f(gla_norm),
        "diff_lambda": f(diff_lambda), "diff_norm": f(diff_norm),
        "gdn_conv": f(gdn_conv), "gdn_A_log": f(gdn_A_log), "gdn_dt_bias": f(gdn_dt_bias), "gdn_norm": f(gdn_norm),
        "w_branch": f(w_branch), "w_out": f(w_out), "ffn_w1": f(ffn_w1), "ffn_w3": f(ffn_w3), "ffn_w2": f(ffn_w2),
        "router_w": f(router_w), "moe_w1": f(moe_w1), "moe_w3": f(moe_w3), "moe_w2": f(moe_w2),
        "ident": np.eye(128, dtype=np.float32), "mask_f": mf, "mask_b": mb, "sm_f": sm_f, "sm_b": sm_b,
        "m_same": m_same, "m_c0": m_c0, "m_c1": m_c1, "reset64": rs64, "rot_inv": inv, "rot_sgn": sgn,
    }
    in_maps = []
    for core in range(N_CORES):
        b = core % 4
        m = dict(shared)
        m["x"] = x[b]
        m["cT"] = np.ascontiguousarray(c[b].reshape(KT, 128).T)
        m["positions"] = positions[b:b + 1]
        half = core // 4
        m["tokidx"] = (half * (T // 2) + np.arange((T // 2) // 128, dtype=np.int32)[None, :] * 128
                       + np.arange(128, dtype=np.int32)[:, None]).astype(np.int32)
        in_maps.append(m)
    nc = build_program()
    res = run_bass_kernel_spmd(nc, in_maps, core_ids=list(range(N_CORES)))
    full = np.empty((4, T, D), np.float32)
    for core in range(N_CORES):
        b, half = core % 4, core // 4
        full[b, half * (T // 2):(half + 1) * (T // 2)] = np.asarray(res.results[core]["out"])
    return full
```
